# Optimizing a Trainium2 kernel written in Bass

```python
import jax
import jax.numpy as jnp
from jax import lax
import numpy as np

D_MODEL = 1024
BATCH = 4
SEQ = 8192
DEPTH = 4

GRID_W = 64
CTX_LEN = 256
HEAD_DIM = 64
N_HEADS_A = 8
N_KV_A = 2
N_HEADS_B = 8
N_KV_B = 2
G_A = N_HEADS_A // N_KV_A
G_B = N_HEADS_B // N_KV_B
QA = N_HEADS_A * HEAD_DIM
KVA = N_KV_A * HEAD_DIM
QB = N_HEADS_B * HEAD_DIM
KVB = N_KV_B * HEAD_DIM
IN_PROJ_WIDTH = QA + 2 * KVA + QB + 2 * KVB
IN_SPLITS = (QA, QA + KVA, QA + 2 * KVA, QA + 2 * KVA + QB, QA + 2 * KVA + QB + KVB)
MIX_OUT_WIDTH = QA + QB
WINDOW = 128
Q_BLOCK = 128
ROPE_THETA = 10000.0
ATTN_SCALE = HEAD_DIM ** -0.5
CONV_WIDTH = 3
D_FF = 2816
N_EXPERTS = 8
TOP_K = 2
D_FF_EXPERT = 3584
N_ADA = 6
EPS = 1e-6
N_ATTN_LAYERS = (DEPTH + 1) // 2
N_CONV_LAYERS = DEPTH // 2

kernel_name = 'hybrid_dit_gqa_window_shortconv_moe'


def rms_norm(x, g):
    xf = x.astype(jnp.float32)
    y = xf * lax.rsqrt(jnp.mean(xf * xf, axis=-1, keepdims=True) + EPS)
    return (y * g.astype(jnp.float32)).astype(x.dtype)


def axial_rope_tables(n_tokens):
    rows = n_tokens // GRID_W
    row, col = jnp.meshgrid(jnp.arange(rows, dtype=jnp.float32), jnp.arange(GRID_W, dtype=jnp.float32), indexing='ij')
    half = HEAD_DIM // 2
    inv_freq = ROPE_THETA ** (-jnp.arange(0, half, 2, dtype=jnp.float32) / half)
    ang = jnp.concatenate([row.reshape(-1, 1) * inv_freq, col.reshape(-1, 1) * inv_freq], axis=-1)
    return jnp.cos(ang), jnp.sin(ang)


def apply_rope(x, cos, sin):
    shape = (cos.shape[0],) + (1,) * (x.ndim - 3) + (cos.shape[1],)
    cs, sn = cos.reshape(shape), sin.reshape(shape)
    xr = x.astype(jnp.float32).reshape(x.shape[:-1] + (HEAD_DIM // 2, 2))
    x0, x1 = xr[..., 0], xr[..., 1]
    out = jnp.stack([x0 * cs - x1 * sn, x0 * sn + x1 * cs], axis=-1)
    return out.reshape(x.shape).astype(x.dtype)


def softmax_with_sink(s, sink):
    sk = sink.astype(jnp.float32)[:, :, None, None]
    m = jnp.maximum(jnp.max(s, axis=-1, keepdims=True), sk)
    p = jnp.exp(s - m)
    return p / (jnp.sum(p, axis=-1, keepdims=True) + jnp.exp(sk - m))


def project_heads(h, w_in, qn_a, kn_a, qn_b, kn_b):
    bsz, n, _ = h.shape
    qa, ka, va, qb, kb, vb = jnp.split(h @ w_in, IN_SPLITS, axis=-1)
    qa = rms_norm(qa.reshape(bsz, n, N_KV_A, G_A, HEAD_DIM), qn_a)
    ka = rms_norm(ka.reshape(bsz, n, N_KV_A, HEAD_DIM), kn_a)
    va = va.reshape(bsz, n, N_KV_A, HEAD_DIM)
    qb = rms_norm(qb.reshape(bsz, n, N_KV_B, G_B, HEAD_DIM), qn_b)
    kb = rms_norm(kb.reshape(bsz, n, N_KV_B, HEAD_DIM), kn_b)
    vb = vb.reshape(bsz, n, N_KV_B, HEAD_DIM)
    return qa, ka, va, qb, kb, vb


def context_attention(q, k, v, sink):
    s = jnp.einsum('bqhgd,bkhd->bhgqk', q, k).astype(jnp.float32) * ATTN_SCALE
    p = jax.nn.softmax(s, axis=-1) if sink is None else softmax_with_sink(s, sink)
    return jnp.einsum('bhgqk,bkhd->bqhgd', p.astype(v.dtype), v)


def global_attention(q, k_lat, v_lat, k_ctx, v_ctx):
    bsz, n = q.shape[:2]
    nb = n // Q_BLOCK
    k = jnp.concatenate([k_ctx, k_lat], axis=1)
    v = jnp.concatenate([v_ctx, v_lat], axis=1)
    qb = jnp.moveaxis(q.reshape((bsz, nb, Q_BLOCK) + q.shape[2:]), 1, 0)

    def block(q_blk):
        s = jnp.einsum('bqhgd,bkhd->bhgqk', q_blk, k).astype(jnp.float32) * ATTN_SCALE
        p = jax.nn.softmax(s, axis=-1)
        return jnp.einsum('bhgqk,bkhd->bqhgd', p.astype(v.dtype), v)

    out = lax.map(block, qb)
    return jnp.moveaxis(out, 0, 1).reshape(q.shape)


def window_attention(q, k_lat, v_lat, k_ctx, v_ctx, sink):
    bsz, n = q.shape[:2]
    nb = n // Q_BLOCK
    span = Q_BLOCK + 2 * WINDOW
    pad = ((0, 0), (WINDOW, WINDOW), (0, 0), (0, 0))
    kp = jnp.pad(k_lat, pad)
    vp = jnp.pad(v_lat, pad)
    qb = jnp.moveaxis(q.reshape((bsz, nb, Q_BLOCK) + q.shape[2:]), 1, 0)
    rel = jnp.arange(Q_BLOCK)[:, None] - jnp.arange(span)[None, :] + WINDOW
    band = jnp.abs(rel) <= WINDOW
    s_ctx_all = None

    def block(args):
        bi, q_blk = args
        start = bi * Q_BLOCK
        kb = lax.dynamic_slice_in_dim(kp, start, span, axis=1)
        vb = lax.dynamic_slice_in_dim(vp, start, span, axis=1)
        kpos = start - WINDOW + jnp.arange(span)
        valid = band & ((kpos >= 0) & (kpos < n))[None, :]
        s_loc = jnp.einsum('bqhgd,bkhd->bhgqk', q_blk, kb).astype(jnp.float32) * ATTN_SCALE
        s_loc = jnp.where(valid, s_loc, -jnp.inf)
        s_ctx = jnp.einsum('bqhgd,bkhd->bhgqk', q_blk, k_ctx).astype(jnp.float32) * ATTN_SCALE
        p = softmax_with_sink(jnp.concatenate([s_ctx, s_loc], axis=-1), sink)
        vv = jnp.concatenate([v_ctx, vb], axis=1)
        return jnp.einsum('bhgqk,bkhd->bqhgd', p.astype(vv.dtype), vv)

    out = lax.map(block, (jnp.arange(nb), qb))
    return jnp.moveaxis(out, 0, 1).reshape(q.shape)


def attn_mixer(h_lat, h_ctx, w_in, w_out, qn_a, kn_a, qn_b, kn_b, sink, cos, sin, with_ctx_out):
    bsz, n = h_lat.shape[:2]
    qa_l, ka_l, va_l, qb_l, kb_l, vb_l = project_heads(h_lat, w_in, qn_a, kn_a, qn_b, kn_b)
    qa_c, ka_c, va_c, qb_c, kb_c, vb_c = project_heads(h_ctx, w_in, qn_a, kn_a, qn_b, kn_b)
    qa_l, ka_l, qb_l, kb_l = [apply_rope(t, cos, sin) for t in (qa_l, ka_l, qb_l, kb_l)]
    sink = sink.reshape(N_KV_B, G_B)
    o_a = global_attention(qa_l, ka_l, va_l, ka_c, va_c).reshape(bsz, n, QA)
    o_b = window_attention(qb_l, kb_l, vb_l, kb_c, vb_c, sink).reshape(bsz, n, QB)
    out_lat = jnp.concatenate([o_a, o_b], axis=-1) @ w_out
    if not with_ctx_out:
        return out_lat, None
    n_ctx = h_ctx.shape[1]
    oa_c = context_attention(qa_c, ka_c, va_c, None).reshape(bsz, n_ctx, QA)
    ob_c = context_attention(qb_c, kb_c, vb_c, sink).reshape(bsz, n_ctx, QB)
    out_ctx = jnp.concatenate([oa_c, ob_c], axis=-1) @ w_out
    return out_lat, out_ctx


def short_conv_mixer(h, w_in, conv_w, w_out):
    n = h.shape[1]
    b_gate, c_gate, u = jnp.split(h @ w_in, 3, axis=-1)
    z = c_gate * u
    r = CONV_WIDTH // 2
    zp = jnp.pad(z, ((0, 0), (r, r), (0, 0)))
    conv = zp[:, 0:n] * conv_w[0]
    for j in range(1, CONV_WIDTH):
        conv = conv + zp[:, j:j + n] * conv_w[j]
    return (b_gate * conv) @ w_out


def swiglu(h, w_gate, w_up, w_down):
    return (jax.nn.silu(h @ w_gate) * (h @ w_up)) @ w_down


def moe_swiglu(h, router_w, w_gate, w_up, w_down):
    logits = (h @ router_w).astype(jnp.float32)
    top_vals, top_idx = lax.top_k(logits, TOP_K)
    weights = jax.nn.softmax(top_vals, axis=-1)
    gates = jnp.sum(jax.nn.one_hot(top_idx, N_EXPERTS, dtype=jnp.float32) * weights[..., None], axis=-2)
    gates = gates.astype(h.dtype)
    out = gates[..., 0:1] * swiglu(h, w_gate[0], w_up[0], w_down[0])
    for e in range(1, N_EXPERTS):
        out = out + gates[..., e:e + 1] * swiglu(h, w_gate[e], w_up[e], w_down[e])
    return out


def setup_inputs(seed: int = 0) -> dict:
    key = jax.random.key(seed)
    ks = jax.random.split(key, 25)
    f32 = jnp.float32
    D = D_MODEL
    NE, NO = N_ATTN_LAYERS, N_CONV_LAYERS

    def nrm(k, shape, scale):
        return jax.random.normal(k, shape, f32) * scale

    return {
        'x': nrm(ks[0], (BATCH, SEQ, D), 1.0),
        'c': nrm(ks[1], (BATCH, D), 1.0),
        'ctx': nrm(ks[2], (BATCH, CTX_LEN, D), 1.0),
        'c_ctx': nrm(ks[3], (D,), 1.0),
        'ada_w': nrm(ks[4], (DEPTH, D, N_ADA * D), 0.5 * D ** -0.5),
        'ada_b': nrm(ks[5], (DEPTH, N_ADA * D), 0.01),
        'norm1_g': 1.0 + nrm(ks[6], (DEPTH, D), 0.05),
        'norm2_g': 1.0 + nrm(ks[7], (DEPTH, D), 0.05),
        'attn_w_in': nrm(ks[8], (NE, D, IN_PROJ_WIDTH), D ** -0.5),
        'attn_w_out': nrm(ks[9], (NE, MIX_OUT_WIDTH, D), MIX_OUT_WIDTH ** -0.5),
        'qnorm_a': 1.0 + nrm(ks[10], (NE, HEAD_DIM), 0.05),
        'knorm_a': 1.0 + nrm(ks[11], (NE, HEAD_DIM), 0.05),
        'qnorm_b': 1.0 + nrm(ks[12], (NE, HEAD_DIM), 0.05),
        'knorm_b': 1.0 + nrm(ks[13], (NE, HEAD_DIM), 0.05),
        'sink_b': nrm(ks[14], (NE, N_HEADS_B), 0.5),
        'ffn_w_gate': nrm(ks[15], (NE, D, D_FF), D ** -0.5),
        'ffn_w_up': nrm(ks[16], (NE, D, D_FF), D ** -0.5),
        'ffn_w_down': nrm(ks[17], (NE, D_FF, D), D_FF ** -0.5),
        'conv_w_in': nrm(ks[18], (NO, D, 3 * D), D ** -0.5),
        'conv_w': nrm(ks[19], (NO, CONV_WIDTH, D), CONV_WIDTH ** -0.5),
        'conv_w_out': nrm(ks[20], (NO, D, D), D ** -0.5),
        'router_w': nrm(ks[21], (NO, D, N_EXPERTS), D ** -0.5),
        'moe_w_gate': nrm(ks[22], (NO, N_EXPERTS, D, D_FF_EXPERT), D ** -0.5),
        'moe_w_up': nrm(ks[23], (NO, N_EXPERTS, D, D_FF_EXPERT), D ** -0.5),
        'moe_w_down': nrm(ks[24], (NO, N_EXPERTS, D_FF_EXPERT, D), D_FF_EXPERT ** -0.5),
    }


def reference(x, c, ctx, c_ctx, ada_w, ada_b, norm1_g, norm2_g, attn_w_in, attn_w_out, qnorm_a, knorm_a, qnorm_b, knorm_b, sink_b, ffn_w_gate, ffn_w_up, ffn_w_down, conv_w_in, conv_w, conv_w_out, router_w, moe_w_gate, moe_w_up, moe_w_down):
    n_tok = x.shape[1]
    cos, sin = axial_rope_tables(n_tok)
    silu_c = jax.nn.silu(c)
    silu_cc = jax.nn.silu(c_ctx)
    xc = ctx
    for layer in range(DEPTH):
        i = layer // 2
        ctx_needed = any(j % 2 == 0 for j in range(layer + 1, DEPTH))
        sh1, sc1, g1, sh2, sc2, g2 = jnp.split((silu_c @ ada_w[layer] + ada_b[layer])[:, None, :], N_ADA, axis=-1)
        csh1, csc1, cg1, csh2, csc2, cg2 = jnp.split(silu_cc @ ada_w[layer] + ada_b[layer], N_ADA, axis=-1)

        h = rms_norm(x, norm1_g[layer]) * (1 + sc1) + sh1
        if layer % 2 == 0:
            hc = rms_norm(xc, norm1_g[layer]) * (1 + csc1) + csh1
            mix, mix_c = attn_mixer(h, hc, attn_w_in[i], attn_w_out[i], qnorm_a[i], knorm_a[i], qnorm_b[i], knorm_b[i], sink_b[i], cos, sin, ctx_needed)
        else:
            mix = short_conv_mixer(h, conv_w_in[i], conv_w[i], conv_w_out[i])
            if ctx_needed:
                hc = rms_norm(xc, norm1_g[layer]) * (1 + csc1) + csh1
                mix_c = short_conv_mixer(hc, conv_w_in[i], conv_w[i], conv_w_out[i])
        x = x + g1 * mix
        if ctx_needed:
            xc = xc + cg1 * mix_c

        if layer % 2 == 0:
            ffn = lambda t: swiglu(t, ffn_w_gate[i], ffn_w_up[i], ffn_w_down[i])
        else:
            ffn = lambda t: moe_swiglu(t, router_w[i], moe_w_gate[i], moe_w_up[i], moe_w_down[i])
        h = rms_norm(x, norm2_g[layer]) * (1 + sc2) + sh2
        x = x + g2 * ffn(h)
        if ctx_needed:
            hc = rms_norm(xc, norm2_g[layer]) * (1 + csc2) + csh2
            xc = xc + cg2 * ffn(hc)
    return x
```

```python
import os
import numpy as np
from contextlib import ExitStack
import concourse.bass as bass
import concourse.mybir as mybir
from concourse.bass_utils import run_bass_kernel_spmd

F32 = mybir.dt.float32
BF16 = mybir.dt.bfloat16
ACT = mybir.ActivationFunctionType
ALU = mybir.AluOpType
AX = mybir.AxisListType

D = 1024
KC = 8
SEQ = 8192
HALF = 4096
CTX = 256
NT = 512
DFF = 2816
DFFE = 3584
NE = 8
EPS = 1e-6
SCALE = 0.125
ENG = ("pe", "act", "dve", "pool", "sp")
DMAQ = "pool"


class Buf:
    __slots__ = ("name", "last_w", "readers", "dma_sem_idx")

    def __init__(self, name):
        self.name = name
        self.last_w = None
        self.readers = []
        self.dma_sem_idx = None


class Op:
    __slots__ = ("eng", "fn", "deps", "is_dma", "sem", "signal", "count", "idx")


class _Rec:
    def __init__(self):
        self.call = None

    def __getattr__(self, name):
        def f(*a, **k):
            self.call = (name, a, k)
            return None
        return f


class Sched:
    def __init__(self, same_engine_sync=True):
        self.ops = []
        self.same_engine_sync = same_engine_sync
        self.n_dma_sems = 0
        self.last_eng = {}
        self.dma_since_bar = []

    def buf(self, name):
        return Buf(name)

    def bufs(self, name, n):
        return [Buf(f"{name}{i}") for i in range(n)]

    def op(self, eng, fn, reads=(), writes=(), dma_home=None, extra_deps=()):
        o = Op()
        o.eng = eng
        rec = _Rec()
        fn(rec)
        o.fn = rec.call
        assert o.fn is not None
        o.is_dma = dma_home is not None
        o.idx = len(self.ops)
        o.signal = False
        o.count = None
        deps = set(extra_deps)
        for b in reads:
            if b.last_w is not None:
                deps.add(b.last_w)
        for b in writes:
            if b.last_w is not None:
                deps.add(b.last_w)
            for r in b.readers:
                deps.add(r)
        fdeps = []
        for d in deps:
            dop = self.ops[d]
            if (not dop.is_dma) and (not o.is_dma) and dop.eng == eng:
                if eng == "pe" or not self.same_engine_sync:
                    continue
            fdeps.append(d)
        o.deps = fdeps
        if o.is_dma:
            if dma_home.dma_sem_idx is None:
                dma_home.dma_sem_idx = self.n_dma_sems
                self.n_dma_sems += 1
            o.sem = ("dma", dma_home.dma_sem_idx)
            self.dma_since_bar.append(o.idx)
        else:
            o.sem = ("eng", eng)
            self.last_eng[eng] = o.idx
        for b in reads:
            b.readers.append(o.idx)
        for b in writes:
            b.last_w = o.idx
            b.readers = []
        self.ops.append(o)
        return o

    def barrier(self):
        deps = list(self.last_eng.values()) + list(self.dma_since_bar)
        self.dma_since_bar = []
        for e in ENG:
            o = Op()
            o.eng = e
            o.fn = None
            o.is_dma = False
            o.idx = len(self.ops)
            o.signal = False
            o.count = None
            o.sem = ("eng", e)
            o.deps = [d for d in deps if not (self.ops[d].eng == e and not self.ops[d].is_dma and e == "pe")]
            self.ops.append(o)

    def emit(self, nc, final_wait_ops=()):
        ops = self.ops
        for o in ops:
            for d in o.deps:
                ops[d].signal = True
        for o in final_wait_ops:
            o.signal = True
        counters = {}
        for o in ops:
            if o.fn is None:
                o.signal = False
                continue
            if o.signal or o.is_dma:
                inc = 16 if o.is_dma else 1
                counters[o.sem] = counters.get(o.sem, 0) + inc
                o.count = counters[o.sem]
                o.signal = True
        with ExitStack() as es:
            sems = {}
            for key in counters:
                sems[key] = es.enter_context(nc.semaphore(f"s_{key[0]}_{key[1]}"))
            block = es.enter_context(nc.Block())
            per_eng = {e: [o for o in ops if o.eng == e] for e in ENG}
            n_waits = [0]

            def make(engname):
                def body(engobj):
                    waited = {}
                    for o in per_eng[engname]:
                        need = {}
                        for d in o.deps:
                            dop = ops[d]
                            if dop.count is None:
                                continue
                            if dop.count > need.get(dop.sem, 0):
                                need[dop.sem] = dop.count
                        for sk, v in need.items():
                            if waited.get(sk, 0) >= v:
                                continue
                            engobj.wait_ge(sems[sk], v)
                            n_waits[0] += 1
                            waited[sk] = v
                        if o.fn is None:
                            continue
                        ins = getattr(engobj, o.fn[0])(*o.fn[1], **o.fn[2])
                        if o.signal:
                            ins.then_inc(sems[o.sem], 16 if o.is_dma else 1)
                    if engname == "sp":
                        for fo in final_wait_ops:
                            engobj.wait_ge(sems[fo.sem], fo.count)
                return body

            block.tensor(make("pe"))
            block.scalar(make("act"))
            block.vector(make("dve"))
            block.gpsimd(make("pool"))
            block.sync(make("sp"))
        self.stats = dict(n_ops=len(ops), n_sems=len(counters), n_waits=n_waits[0],
                          per_eng={e: len(per_eng[e]) for e in ENG})
        return self.stats


VEC = {}
_c = 0
for _n, _w in [("c", 8), ("cc", 8), ("n1", 8), ("n2", 8), ("adab", 48), ("qna", 1), ("kna", 1),
               ("qnb", 1), ("knb", 1), ("sink", 8), ("vlo", 1), ("vhi", 1), ("cw", 24)]:
    VEC[_n] = _c
    _c += _w
NVEC = _c


class KB:
    def __init__(self):
        self.nc = bass.Bass("TRN2", target_bir_lowering=False)
        self.S = Sched()
        self.es = ExitStack()
        self.dram_in = {}
        self.uid = 0
        self.dump = None

    def din(self, name, shape, dt=F32):
        t = self.nc.dram_tensor(name, list(shape), dt, kind="ExternalInput").ap()
        self.dram_in[name] = t
        return t

    def dout(self, name, shape, dt=F32):
        return self.nc.dram_tensor(name, list(shape), dt, kind="ExternalOutput").ap()

    def dscratch(self, name, shape, dt=F32):
        return self.nc.dram_tensor(name, list(shape), dt).ap()

    def sb(self, es, name, shape, dt):
        self.uid += 1
        return es.enter_context(self.nc.sbuf_tensor(f"sb{self.uid}_{name}", list(shape), dt))

    def ps(self, es, name, shape, dt=F32):
        self.uid += 1
        return es.enter_context(self.nc.psum_tensor(f"ps{self.uid}_{name}", list(shape), dt))

    def name(self, p):
        self.uid += 1
        return f"{p}{self.uid}"


def chunked(ap):
    return ap.rearrange("(k p) n -> p k n", p=128)


class Common:
    def __init__(self, kb, layer_vec, ada_w, consts, es=None):
        self.kb = kb
        nc, S = kb.nc, kb.S
        es = es if es is not None else kb.es
        self.vec = kb.sb(es, "vec", [128, NVEC], F32)
        self.b_vec = S.buf("vec")
        S.op(DMAQ, lambda e: e.dma_start(out=self.vec[:], in_=layer_vec), writes=[self.b_vec], dma_home=self.b_vec)
        self.cst_bf = kb.sb(es, "cst_bf", [128, 3, 128], BF16)
        self.b_cst = S.buf("cst")
        S.op("pool", lambda e: e.dma_start(out=self.cst_bf[:], in_=consts["cbf"]), writes=[self.b_cst], dma_home=self.b_cst)
        self.cst_f = kb.sb(es, "cst_f", [128, 2, 128], F32)
        self.b_cstf = S.buf("cstf")
        S.op(DMAQ, lambda e: e.dma_start(out=self.cst_f[:], in_=consts["cf"]), writes=[self.b_cstf], dma_home=self.b_cstf)
        self.onesm = self.cst_bf[:, 0, :]
        self.bones = self.cst_bf[:, 1, :]
        self.rotm = self.cst_bf[:, 2, :]
        self.ident = self.cst_f[:, 0, :]
        self.ones_f = self.cst_f[:, 1, :]
        self.eps_t = kb.sb(es, "eps_t", [128, 1], F32)
        self.b_eps = S.buf("eps")
        S.op("dve", lambda e: e.memset(self.eps_t[:], EPS), writes=[self.b_eps])
        self.eps_col = self.eps_t[:, 0:1]
        self.mod = kb.sb(es, "mod", [128, 2, 48], F32)
        self.geff = kb.sb(es, "geff", [128, 2, 2, 8], F32)
        self.b_mod = S.buf("mod")
        self._build_mod(ada_w)

    def _build_mod(self, ada_w):
        kb = self.kb
        nc, S = kb.nc, kb.S
        with ExitStack() as es:
            sc = kb.sb(es, "silu_c", [128, 8, 2], BF16)
            sc32 = kb.sb(es, "silu_c32", [128, 2, 8], F32)
            b_sc = S.buf("silu_c")
            wsl = [kb.sb(es, f"adaw{i}", [128, 8, 1024], BF16) for i in range(2)]
            b_w = S.bufs("adaw", 2)
            mps = kb.ps(es, "mod_ps", [128, 48, 2], F32)
            b_mps = S.buf("mod_ps")
            vec = self.vec
            S.op("act", lambda e: e.activation(out=sc32[:, 0, :], in_=vec[:, VEC["c"]:VEC["c"] + 8], func=ACT.Silu),
                 reads=[self.b_vec], writes=[b_sc])
            S.op("act", lambda e: e.activation(out=sc32[:, 1, :], in_=vec[:, VEC["cc"]:VEC["cc"] + 8], func=ACT.Silu),
                 reads=[self.b_vec], writes=[b_sc])
            for ci in range(2):
                S.op("dve", lambda e, ci=ci: e.tensor_copy(out=sc[:, :, ci], in_=sc32[:, ci, :]), reads=[b_sc], writes=[b_sc])
            for j in range(6):
                sl = j % 2
                S.op("pool", lambda e, j=j, sl=sl: e.dma_start(out=wsl[sl][:], in_=chunked(ada_w[:, j * 1024:(j + 1) * 1024])),
                     writes=[b_w[sl]], dma_home=b_w[sl])
                for oc in range(8):
                    for kc in range(8):
                        S.op("pe", lambda e, j=j, sl=sl, oc=oc, kc=kc: e.matmul(
                            mps[:, j * 8 + oc, :], lhsT=wsl[sl][:, kc, oc * 128:(oc + 1) * 128], rhs=sc[:, kc, :],
                            start=(kc == 0), stop=(kc == 7)), reads=[b_w[sl], b_sc], writes=[b_mps])
            ab = VEC["adab"]
            for ci in range(2):
                S.op("dve", lambda e, ci=ci: e.tensor_tensor(out=self.mod[:, ci, :], in0=mps[:, :, ci], in1=vec[:, ab:ab + 48], op=ALU.add),
                     reads=[b_mps, self.b_vec], writes=[self.b_mod])
            for ci in range(2):
                for ni in range(2):
                    gcol = VEC["n1"] if ni == 0 else VEC["n2"]
                    scj = 1 if ni == 0 else 4
                    S.op("dve", lambda e, ci=ci, ni=ni, gcol=gcol, scj=scj: e.scalar_tensor_tensor(
                        out=self.geff[:, ci, ni, :], in0=self.mod[:, ci, scj * 8:scj * 8 + 8], scalar=1.0,
                        in1=vec[:, gcol:gcol + 8], op0=ALU.add, op1=ALU.mult),
                        reads=[self.b_mod, self.b_vec], writes=[self.b_mod])
        S.barrier()

    def sh(self, ci, ni, kc):
        j = 0 if ni == 0 else 3
        return self.mod[:, ci, j * 8 + kc:j * 8 + kc + 1]

    def gate(self, ci, ni, kc):
        j = 2 if ni == 0 else 5
        return self.mod[:, ci, j * 8 + kc:j * 8 + kc + 1]

    def ge(self, ci, ni, kc):
        return self.geff[:, ci, ni, kc:kc + 1]


class NormWork:
    def __init__(self, kb, es, tag):
        S = kb.S
        self.sq = [kb.sb(es, f"{tag}_sq{i}", [128, NT], BF16) for i in range(2)]
        self.b_sq = S.bufs(f"{tag}_sq", 2)
        self.sd = kb.sb(es, f"{tag}_sd", [128, NT], F32)
        self.rstd = kb.sb(es, f"{tag}_rstd", [128, NT], F32)
        self.b_sd = S.buf(f"{tag}_sd")
        self.b_rstd = S.buf(f"{tag}_rstd")
        self.tmp = [kb.sb(es, f"{tag}_tmp{i}", [128, NT], F32) for i in range(2)]
        self.b_tmp = S.bufs(f"{tag}_tmp", 2)


def norm_mod(kb, cm, nw, xt, b_xt, N, ci, ni, ms_ps, b_ms, h_out, b_h, inplace=False):
    S = kb.S
    for kc in range(KC):
        sl = kc % 2
        S.op("act", lambda e, kc=kc, sl=sl: e.activation(out=nw.sq[sl][:, :N], in_=xt[:, kc, :N], func=ACT.Square),
             reads=[b_xt], writes=[nw.b_sq[sl]])
        S.op("pe", lambda e, kc=kc, sl=sl: e.matmul(ms_ps[:, :N], lhsT=cm.onesm, rhs=nw.sq[sl][:, :N], start=(kc == 0), stop=(kc == KC - 1)),
             reads=[nw.b_sq[sl], cm.b_cst], writes=[b_ms])
    S.op("act", lambda e: e.activation(out=nw.sd[:, :N], in_=ms_ps[:, :N], func=ACT.Sqrt, bias=cm.eps_col, scale=1.0),
         reads=[b_ms, cm.b_eps], writes=[nw.b_sd])
    S.op("dve", lambda e: e.reciprocal(out=nw.rstd[:, :N], in_=nw.sd[:, :N]), reads=[nw.b_sd], writes=[nw.b_rstd])
    for kc in range(KC):
        sl = kc % 2
        if inplace:
            S.op("dve", lambda e, kc=kc: e.scalar_tensor_tensor(out=xt[:, kc, :N], in0=xt[:, kc, :N], scalar=cm.ge(ci, ni, kc),
                                                                 in1=nw.rstd[:, :N], op0=ALU.mult, op1=ALU.mult),
                 reads=[nw.b_rstd, cm.b_mod], writes=[b_xt])
            S.op("act", lambda e, kc=kc: e.activation(out=xt[:, kc, :N], in_=xt[:, kc, :N], func=ACT.Identity, bias=cm.sh(ci, ni, kc), scale=1.0),
                 reads=[cm.b_mod], writes=[b_xt])
            S.op("pool", lambda e, kc=kc: e.tensor_copy(out=h_out(kc), in_=xt[:, kc, :N]), reads=[b_xt], writes=[b_h])
        else:
            S.op("dve", lambda e, kc=kc, sl=sl: e.scalar_tensor_tensor(out=nw.tmp[sl][:, :N], in0=xt[:, kc, :N], scalar=cm.ge(ci, ni, kc),
                                                                        in1=nw.rstd[:, :N], op0=ALU.mult, op1=ALU.mult),
                 reads=[b_xt, nw.b_rstd, cm.b_mod], writes=[nw.b_tmp[sl]])
            S.op("act", lambda e, kc=kc, sl=sl: e.activation(out=h_out(kc), in_=nw.tmp[sl][:, :N], func=ACT.Identity, bias=cm.sh(ci, ni, kc), scale=1.0),
                 reads=[nw.b_tmp[sl], cm.b_mod], writes=[b_h])


def ffn_phase(kb, cm, tiles, experts, dff, router_w=None):
    nc, S = kb.nc, kb.S
    moe = router_w is not None
    FS = 256
    nsl = dff // FS
    groups = []
    cur, tot = [], 0
    for t in tiles:
        if tot + t["N"] > 2304:
            groups.append(cur)
            cur, tot = [], 0
        cur.append(t)
        tot += t["N"]
    if cur:
        groups.append(cur)
    TMAX = max(sum(t["N"] for t in g) for g in groups)
    out_ops = []
    with ExitStack() as es:
        h2 = kb.sb(es, "f_h2", [128, KC, TMAX], BF16)
        yacc = kb.sb(es, "f_yacc", [128, KC, TMAX], F32)
        wgu = [kb.sb(es, f"f_wgu{i}", [128, 2, KC, FS], BF16) for i in range(2)]
        wdn = [kb.sb(es, f"f_wdn{i}", [128, FS // 128, D], BF16) for i in range(2)]
        b_wg = S.bufs("f_wg", 2)
        b_wu = S.bufs("f_wu", 2)
        b_wd = S.bufs("f_wd", 2)
        xt = kb.sb(es, "f_xt", [128, KC, NT], F32)
        b_xt = S.buf("f_xt")
        nw = NormWork(kb, es, "f_nw")
        sg = [kb.sb(es, f"f_sg{i}", [128, NT], F32) for i in range(2)]
        b_sg = S.bufs("f_sg", 2)
        tt = [kb.sb(es, f"f_tt{i}", [128, NT], F32) for i in range(2)]
        b_tt = S.bufs("f_tt", 2)
        abuf = [kb.sb(es, f"f_a{i}", [128, 2, NT], BF16) for i in range(2)]
        b_a = [S.bufs(f"f_a{i}_", 2) for i in range(2)]
        banks = [kb.ps(es, f"f_ps{i}", [128, NT], F32) for i in range(8)]
        b_bank = S.bufs("f_bank", 8)
        if moe:
            rw = kb.sb(es, "f_rw", [128, KC, NE], F32)
            b_rw = S.buf("f_rw")
            S.op(DMAQ, lambda e: e.dma_start(out=rw[:], in_=chunked(router_w)), writes=[b_rw], dma_home=b_rw)
            gT = kb.sb(es, "f_gT", [NE, TMAX], F32)
            Gb = kb.sb(es, "f_Gb", [128, TMAX], F32)
            sel = kb.sb(es, "f_sel", [NE, NE, 128], F32)
            b_sel = S.buf("f_sel")
            S.op(DMAQ, lambda e: e.dma_start(out=sel[:], in_=kb.dram_in["selm"]), writes=[b_sel], dma_home=b_sel)
            rt = {n: kb.sb(es, f"f_rt_{n}", [128, w], F32) for n, w in
                  [("lg", 8), ("m1", 1), ("eq", 8), ("lg2", 8), ("m2", 1), ("sel", 8), ("nm1", 1), ("ex", 8), ("w", 8), ("ss", 1), ("rs", 1), ("g", 8)]}
            b_rt = S.buf("f_rt")

        for g in groups:
            offs = []
            o = 0
            for t in g:
                offs.append(o)
                o += t["N"]
            b_h2 = S.bufs("f_h2_", len(g))
            b_y = S.bufs("f_y_", len(g))
            b_gT = S.bufs("f_gT_", len(g))
            b_Gb = S.bufs("f_Gb_", len(g))
            for ti, t in enumerate(g):
                N, off, ci = t["N"], offs[ti], t["ci"]
                S.op(DMAQ, lambda e, t=t, N=N: e.dma_start(out=xt[:, :, :N], in_=chunked(t["src"])), writes=[b_xt], dma_home=b_xt)
                norm_mod(kb, cm, nw, xt, b_xt, N, ci, 1, banks[0], b_bank[0],
                         lambda kc, off=off, N=N: h2[:, kc, off:off + N], b_h2[ti], inplace=moe)
                if moe:
                    for tb in range(N // 128):
                        lgp = banks[1]
                        for kc in range(KC):
                            S.op("pe", lambda e, kc=kc, tb=tb: e.matmul(lgp[:, 0:NE], lhsT=xt[:, kc, tb * 128:(tb + 1) * 128], rhs=rw[:, kc, :],
                                                                       start=(kc == 0), stop=(kc == KC - 1)),
                                 reads=[b_xt, b_rw], writes=[b_bank[1]])
                        R = rt
                        S.op("dve", lambda e: e.tensor_copy(out=R["lg"][:], in_=lgp[:, 0:NE]), reads=[b_bank[1]], writes=[b_rt])
                        S.op("dve", lambda e: e.tensor_reduce(out=R["m1"][:], in_=R["lg"][:], axis=AX.X, op=ALU.max), reads=[b_rt], writes=[b_rt])
                        S.op("dve", lambda e: e.tensor_scalar(out=R["eq"][:], in0=R["lg"][:], scalar1=R["m1"][:], scalar2=None, op0=ALU.is_ge), reads=[b_rt], writes=[b_rt])
                        S.op("dve", lambda e: e.scalar_tensor_tensor(out=R["lg2"][:], in0=R["eq"][:], scalar=-1e30, in1=R["lg"][:], op0=ALU.mult, op1=ALU.add), reads=[b_rt], writes=[b_rt])
                        S.op("dve", lambda e: e.tensor_reduce(out=R["m2"][:], in_=R["lg2"][:], axis=AX.X, op=ALU.max), reads=[b_rt], writes=[b_rt])
                        S.op("dve", lambda e: e.tensor_scalar(out=R["sel"][:], in0=R["lg"][:], scalar1=R["m2"][:], scalar2=None, op0=ALU.is_ge), reads=[b_rt], writes=[b_rt])
                        S.op("dve", lambda e: e.tensor_scalar(out=R["nm1"][:], in0=R["m1"][:], scalar1=-1.0, scalar2=None, op0=ALU.mult), reads=[b_rt], writes=[b_rt])
                        S.op("act", lambda e: e.activation(out=R["ex"][:], in_=R["lg"][:], func=ACT.Exp, bias=R["nm1"][:], scale=1.0), reads=[b_rt], writes=[b_rt])
                        S.op("dve", lambda e: e.tensor_tensor(out=R["w"][:], in0=R["ex"][:], in1=R["sel"][:], op=ALU.mult), reads=[b_rt], writes=[b_rt])
                        S.op("dve", lambda e: e.tensor_reduce(out=R["ss"][:], in_=R["w"][:], axis=AX.X, op=ALU.add), reads=[b_rt], writes=[b_rt])
                        S.op("dve", lambda e: e.reciprocal(out=R["rs"][:], in_=R["ss"][:]), reads=[b_rt], writes=[b_rt])
                        S.op("dve", lambda e: e.tensor_scalar(out=R["g"][:], in0=R["w"][:], scalar1=R["rs"][:], scalar2=None, op0=ALU.mult), reads=[b_rt], writes=[b_rt])
                        S.op("pe", lambda e: e.transpose(out=banks[2][0:NE, 0:128], in_=R["g"][:], identity=cm.ident), reads=[b_rt, cm.b_cstf], writes=[b_bank[2]])
                        S.op("dve", lambda e, off=off, tb=tb: e.tensor_copy(out=gT[:, off + tb * 128:off + (tb + 1) * 128], in_=banks[2][0:NE, 0:128]),
                             reads=[b_bank[2]], writes=[b_gT[ti]])
            first = True
            step = 0
            pend = None
            acount = 0
            for ei, (wg_d, wu_d, wd_d) in enumerate(experts):
                if moe:
                    for ti, t in enumerate(g):
                        N, off = t["N"], offs[ti]
                        S.op("pe", lambda e, ei=ei, off=off, N=N: e.matmul(banks[7][:, :N], lhsT=sel[:, ei, :], rhs=gT[:, off:off + N], start=True, stop=True),
                             reads=[b_sel, b_gT[ti]], writes=[b_bank[7]])
                        S.op("act", lambda e, off=off, N=N: e.activation(out=Gb[:, off:off + N], in_=banks[7][:, :N], func=ACT.Copy),
                             reads=[b_bank[7]], writes=[b_Gb[ti]])
                for s in range(nsl):
                    sl = step % 2
                    step += 1
                    f0 = s * FS
                    S.op("pool", lambda e, sl=sl, f0=f0, wg_d=wg_d: e.dma_start(out=wgu[sl][:, 0, :, :], in_=chunked(wg_d[:, f0:f0 + FS])),
                         writes=[b_wg[sl]], dma_home=b_wg[sl])
                    S.op("pool", lambda e, sl=sl, f0=f0, wu_d=wu_d: e.dma_start(out=wgu[sl][:, 1, :, :], in_=chunked(wu_d[:, f0:f0 + FS])),
                         writes=[b_wu[sl]], dma_home=b_wu[sl])
                    S.op("pool", lambda e, sl=sl, f0=f0, wd_d=wd_d: e.dma_start(out=wdn[sl][:], in_=chunked(wd_d[f0:f0 + FS, :])),
                         writes=[b_wd[sl]], dma_home=b_wd[sl])
                    for ti, t in enumerate(g):
                        N, off = t["N"], offs[ti]
                        asl = acount % 2
                        acount += 1

                        def down(half, sl=sl, asl=asl, N=N, off=off, ti=ti, first=first):
                            for oc in range(half * 4, half * 4 + 4):
                                bk = 4 + oc % 4
                                for fc in range(2):
                                    S.op("pe", lambda e, oc=oc, fc=fc, bk=bk: e.matmul(banks[bk][:, :N], lhsT=wdn[sl][:, fc, oc * 128:(oc + 1) * 128],
                                                                                    rhs=abuf[asl][:, fc, :N], start=(fc == 0), stop=(fc == 1)),
                                         reads=[b_wd[sl], b_a[asl][fc]], writes=[b_bank[bk]])
                                if first:
                                    S.op("dve", lambda e, oc=oc, bk=bk: e.tensor_copy(out=yacc[:, oc, off:off + N], in_=banks[bk][:, :N]),
                                         reads=[b_bank[bk]], writes=[b_y[ti]])
                                else:
                                    S.op("dve", lambda e, oc=oc, bk=bk: e.tensor_tensor(out=yacc[:, oc, off:off + N], in0=yacc[:, oc, off:off + N],
                                                                                     in1=banks[bk][:, :N], op=ALU.add),
                                         reads=[b_bank[bk]], writes=[b_y[ti]])

                        for fc in range(2):
                            gb, ub = fc, 2 + fc
                            for kc in range(KC):
                                S.op("pe", lambda e, kc=kc, fc=fc, gb=gb: e.matmul(banks[gb][:, :N], lhsT=wgu[sl][:, 0, kc, fc * 128:(fc + 1) * 128],
                                                                                rhs=h2[:, kc, off:off + N], start=(kc == 0), stop=(kc == KC - 1)),
                                     reads=[b_wg[sl], b_h2[ti]], writes=[b_bank[gb]])
                            for kc in range(KC):
                                S.op("pe", lambda e, kc=kc, fc=fc, ub=ub: e.matmul(banks[ub][:, :N], lhsT=wgu[sl][:, 1, kc, fc * 128:(fc + 1) * 128],
                                                                                rhs=h2[:, kc, off:off + N], start=(kc == 0), stop=(kc == KC - 1)),
                                     reads=[b_wu[sl], b_h2[ti]], writes=[b_bank[ub]])
                            if pend is not None:
                                pend(fc)
                            S.op("act", lambda e, fc=fc, gb=gb: e.activation(out=sg[fc][:, :N], in_=banks[gb][:, :N], func=ACT.Silu),
                                 reads=[b_bank[gb]], writes=[b_sg[fc]])
                            if moe:
                                S.op("dve", lambda e, fc=fc, ub=ub: e.tensor_tensor(out=tt[fc][:, :N], in0=sg[fc][:, :N], in1=banks[ub][:, :N], op=ALU.mult),
                                     reads=[b_sg[fc], b_bank[ub]], writes=[b_tt[fc]])
                                S.op("dve", lambda e, fc=fc, asl=asl: e.tensor_tensor(out=abuf[asl][:, fc, :N], in0=tt[fc][:, :N], in1=Gb[:, off:off + N], op=ALU.mult),
                                     reads=[b_tt[fc], b_Gb[ti]], writes=[b_a[asl][fc]])
                            else:
                                S.op("dve", lambda e, fc=fc, ub=ub, asl=asl: e.tensor_tensor(out=abuf[asl][:, fc, :N], in0=sg[fc][:, :N], in1=banks[ub][:, :N], op=ALU.mult),
                                     reads=[b_sg[fc], b_bank[ub]], writes=[b_a[asl][fc]])
                        pend = down
                    first = False
            if pend is not None:
                pend(0)
                pend(1)
                pend = None
            for ti, t in enumerate(g):
                N, off, ci = t["N"], offs[ti], t["ci"]
                S.op(DMAQ, lambda e, t=t, N=N: e.dma_start(out=xt[:, :, :N], in_=chunked(t["src"])), writes=[b_xt], dma_home=b_xt)
                for kc in range(KC):
                    S.op("dve", lambda e, kc=kc, N=N, off=off, ci=ci: e.scalar_tensor_tensor(
                        out=xt[:, kc, :N], in0=yacc[:, kc, off:off + N], scalar=cm.gate(ci, 1, kc), in1=xt[:, kc, :N], op0=ALU.mult, op1=ALU.add),
                        reads=[b_y[ti], cm.b_mod], writes=[b_xt])
                oo = S.op(DMAQ, lambda e, t=t, N=N: e.dma_start(out=chunked(t["dst"]), in_=xt[:, :, :N]), reads=[b_xt], dma_home=b_xt)
                out_ops.append(oo)
    S.barrier()
    return out_ops


def conv_phase(kb, cm, tiles, cw_in, cw_out):
    nc, S = kb.nc, kb.S
    with ExitStack() as es:
        win = kb.sb(es, "c_win", [128, KC, 3 * D], BF16)
        wout = kb.sb(es, "c_wout", [128, KC, D], BF16)
        b_win = S.bufs("c_win", 3)
        b_wout = S.buf("c_wout")
        for j in range(3):
            for hh in range(2):
                c0 = j * D + hh * 512
                S.op("pool", lambda e, c0=c0: e.dma_start(out=win[:, :, c0:c0 + 512], in_=chunked(cw_in[:, c0:c0 + 512])),
                     writes=[b_win[j]], dma_home=S.buf("c_win_d"))
        S.op("pool", lambda e: e.dma_start(out=wout[:], in_=chunked(cw_out)), writes=[b_wout], dma_home=b_wout)
        xt = kb.sb(es, "c_xt", [128, KC, NT], F32)
        b_xt = S.buf("c_xt")
        h = kb.sb(es, "c_h", [128, KC, NT], BF16)
        b_h = S.buf("c_h")
        mb = kb.sb(es, "c_m", [128, KC, NT], BF16)
        b_m = S.bufs("c_m", KC)
        nw = NormWork(kb, es, "c_nw")
        bsb = [kb.sb(es, f"c_b{i}", [128, NT], BF16) for i in range(2)]
        csb = [kb.sb(es, f"c_c{i}", [128, NT], F32) for i in range(2)]
        zsb = [kb.sb(es, f"c_z{i}", [128, NT], F32) for i in range(2)]
        acc = [kb.sb(es, f"c_acc{i}", [128, NT], F32) for i in range(2)]
        b_bsb, b_csb, b_zsb, b_acc = S.bufs("c_b", 2), S.bufs("c_c", 2), S.bufs("c_z", 2), S.bufs("c_acc", 2)
        banks = [kb.ps(es, f"c_ps{i}", [128, NT], F32) for i in range(8)]
        b_bank = S.bufs("c_bank", 8)
        vec = cm.vec
        cw = VEC["cw"]
        for t in tiles:
            N, ci = t["N"], t["ci"]
            M = N + 2
            S.op(DMAQ, lambda e, t=t, M=M: e.dma_start(out=xt[:, :, :M], in_=chunked(t["src"])), writes=[b_xt], dma_home=b_xt)
            norm_mod(kb, cm, nw, xt, b_xt, M, ci, 0, banks[6], b_bank[6], lambda kc, M=M: h[:, kc, :M], b_h)
            for kc in range(KC):
                sl = kc % 2
                pb, pc, pu = banks[sl * 3], banks[sl * 3 + 1], banks[sl * 3 + 2]
                bb, bc, bu = b_bank[sl * 3], b_bank[sl * 3 + 1], b_bank[sl * 3 + 2]
                for j, (pp, bpp) in enumerate([(pb, bb), (pc, bc), (pu, bu)]):
                    col = j * D + kc * 128
                    for k2 in range(KC):
                        S.op("pe", lambda e, pp=pp, col=col, k2=k2, M=M: e.matmul(pp[:, :M], lhsT=win[:, k2, col:col + 128], rhs=h[:, k2, :M],
                                                                              start=(k2 == 0), stop=(k2 == KC - 1)),
                             reads=[b_win[j], b_h], writes=[bpp])
                S.op("act", lambda e, sl=sl, pb=pb, M=M: e.activation(out=bsb[sl][:, :M], in_=pb[:, :M], func=ACT.Copy), reads=[bb], writes=[b_bsb[sl]])
                S.op("act", lambda e, sl=sl, pc=pc, M=M: e.activation(out=csb[sl][:, :M], in_=pc[:, :M], func=ACT.Copy), reads=[bc], writes=[b_csb[sl]])
                S.op("dve", lambda e, sl=sl, pu=pu, M=M: e.tensor_tensor(out=zsb[sl][:, :M], in0=csb[sl][:, :M], in1=pu[:, :M], op=ALU.mult),
                     reads=[b_csb[sl], bu], writes=[b_zsb[sl]])
                for side, col in (("lo", 0), ("hi", M - 1)):
                    mode = t[side]
                    if mode == "flag":
                        fcol = VEC["vlo"] if side == "lo" else VEC["vhi"]
                        S.op("dve", lambda e, sl=sl, col=col, fcol=fcol: e.tensor_scalar(out=zsb[sl][:, col:col + 1], in0=zsb[sl][:, col:col + 1],
                                                                                      scalar1=vec[:, fcol:fcol + 1], scalar2=None, op0=ALU.mult),
                             reads=[cm.b_vec], writes=[b_zsb[sl]])
                    elif mode == "zero":
                        S.op("dve", lambda e, sl=sl, col=col: e.memset(zsb[sl][:, col:col + 1], 0.0), writes=[b_zsb[sl]])
                w0 = vec[:, cw + kc:cw + kc + 1]
                w1 = vec[:, cw + 8 + kc:cw + 8 + kc + 1]
                w2 = vec[:, cw + 16 + kc:cw + 16 + kc + 1]
                S.op("dve", lambda e, sl=sl, w0=w0, N=N: e.tensor_scalar(out=acc[sl][:, :N], in0=zsb[sl][:, 0:N], scalar1=w0, scalar2=None, op0=ALU.mult),
                     reads=[b_zsb[sl], cm.b_vec], writes=[b_acc[sl]])
                S.op("dve", lambda e, sl=sl, w1=w1, N=N: e.scalar_tensor_tensor(out=acc[sl][:, :N], in0=zsb[sl][:, 1:N + 1], scalar=w1, in1=acc[sl][:, :N],
                                                                            op0=ALU.mult, op1=ALU.add),
                     reads=[b_zsb[sl]], writes=[b_acc[sl]])
                S.op("dve", lambda e, sl=sl, w2=w2, N=N: e.scalar_tensor_tensor(out=acc[sl][:, :N], in0=zsb[sl][:, 2:N + 2], scalar=w2, in1=acc[sl][:, :N],
                                                                            op0=ALU.mult, op1=ALU.add),
                     reads=[b_zsb[sl]], writes=[b_acc[sl]])
                S.op("pool", lambda e, sl=sl, kc=kc, N=N: e.tensor_tensor(out=mb[:, kc, :N], in0=acc[sl][:, :N], in1=bsb[sl][:, 1:N + 1], op=ALU.mult),
                     reads=[b_acc[sl], b_bsb[sl]], writes=[b_m[kc]])
            for oc in range(KC):
                pk = 6 + oc % 2
                for k2 in range(KC):
                    S.op("pe", lambda e, pk=pk, oc=oc, k2=k2, N=N: e.matmul(banks[pk][:, :N], lhsT=wout[:, k2, oc * 128:(oc + 1) * 128], rhs=mb[:, k2, :N],
                                                                        start=(k2 == 0), stop=(k2 == KC - 1)),
                         reads=[b_wout, b_m[k2]], writes=[b_bank[pk]])
                S.op("dve", lambda e, pk=pk, oc=oc, N=N, ci=ci: e.scalar_tensor_tensor(out=xt[:, oc, 1:N + 1], in0=banks[pk][:, :N], scalar=cm.gate(ci, 0, oc),
                                                                                   in1=xt[:, oc, 1:N + 1], op0=ALU.mult, op1=ALU.add),
                     reads=[b_bank[pk], cm.b_mod], writes=[b_xt])
            S.op(DMAQ, lambda e, t=t, N=N: e.dma_start(out=chunked(t["dst"]), in_=xt[:, :, 1:N + 1]), reads=[b_xt], dma_home=b_xt)
    S.barrier()


def conv_tiles(xpad, xm, ci, ntok, lo, hi):
    tiles = []
    t0 = 0
    while t0 < ntok:
        N = min(510, ntok - t0)
        tiles.append(dict(src=xpad[:, t0:t0 + N + 2], dst=xm[:, t0:t0 + N], N=N, ci=ci,
                          lo=(lo if t0 == 0 else None), hi=(hi if t0 + N == ntok else None)))
        t0 += N
    return tiles


def ffn_tiles(src, dst, ci, ntok):
    return [dict(src=src[:, t0:min(t0 + NT, ntok)], dst=dst[:, t0:min(t0 + NT, ntok)], N=min(NT, ntok - t0), ci=ci) for t0 in range(0, ntok, NT)]


def build_layer_B(with_ctx, n_exp=NE):
    global DMAQ
    DMAQ = "sp"
    kb = KB()
    xpad = kb.din("xpad", [D, HALF + 2])
    vecd = kb.din("vec", [128, NVEC])
    ada_w = kb.din("ada_w", [D, 6 * D])
    consts = dict(cbf=kb.din("cbf", [128, 3, 128]), cf=kb.din("cf", [128, 2, 128]))
    cw_in = kb.din("cw_in", [D, 3 * D])
    cw_out = kb.din("cw_out", [D, D])
    router_w = kb.din("router_w", [D, NE])
    kb.din("selm", [NE, NE, 128])
    mwg = kb.din("mwg", [n_exp, D, DFFE])
    mwu = kb.din("mwu", [n_exp, D, DFFE])
    mwd = kb.din("mwd", [n_exp, DFFE, D])
    yo = kb.dout("yo", [D, HALF])
    xm = kb.dscratch("xm", [D, HALF])
    if with_ctx:
        xcpad = kb.din("xcpad", [D, CTX + 2])
        yc = kb.dout("yc", [D, CTX])
        xcm = kb.dscratch("xcm", [D, CTX])
    cm = Common(kb, vecd, ada_w, consts)
    ct = conv_tiles(xpad, xm, 0, HALF, "flag", "flag")
    ft = ffn_tiles(xm, yo, 0, HALF)
    if with_ctx:
        ct += conv_tiles(xcpad, xcm, 1, CTX, "zero", "zero")
        ft = ft[:4] + ffn_tiles(xcm, yc, 1, CTX) + ft[4:]
    conv_phase(kb, cm, ct, cw_in, cw_out)
    experts = [(mwg[e], mwu[e], mwd[e]) for e in range(n_exp)]
    outs = ffn_phase(kb, cm, ft, experts, DFFE, router_w=router_w)
    stats = kb.S.emit(kb.nc, final_wait_ops=outs)
    kb.es.close()
    return kb.nc, stats


def colpack(v):
    return np.ascontiguousarray(v.reshape(-1, 128).T)


def make_consts():
    cbf = np.zeros((128, 3, 128), np.float32)
    cbf[:, 0, :] = 1.0 / D
    for hh in range(2):
        cbf[hh * 64:(hh + 1) * 64, 1, hh * 64:(hh + 1) * 64] = 1.0 / 64
    for i in range(64):
        cbf[2 * i + 1, 2, 2 * i] = -1.0
        cbf[2 * i, 2, 2 * i + 1] = 1.0
    cf = np.zeros((128, 2, 128), np.float32)
    cf[:, 0, :] = np.eye(128, dtype=np.float32)
    cf[:, 1, :] = 1.0
    selm = np.zeros((NE, NE, 128), np.float32)
    for e in range(NE):
        selm[e, e, :] = 1.0
    return cbf, cf, selm


def make_vec(inp, layer, b, half):
    i = layer // 2
    v = np.zeros((128, NVEC), np.float32)
    v[:, VEC["c"]:VEC["c"] + 8] = colpack(inp["c"][b])
    v[:, VEC["cc"]:VEC["cc"] + 8] = colpack(inp["c_ctx"])
    v[:, VEC["n1"]:VEC["n1"] + 8] = colpack(inp["norm1_g"][layer])
    v[:, VEC["n2"]:VEC["n2"] + 8] = colpack(inp["norm2_g"][layer])
    v[:, VEC["adab"]:VEC["adab"] + 48] = colpack(inp["ada_b"][layer])
    if layer % 2 == 0:
        for n, k in (("qna", "qnorm_a"), ("kna", "knorm_a"), ("qnb", "qnorm_b"), ("knb", "knorm_b")):
            v[:, VEC[n]] = np.tile(inp[k][i], 2)
        v[:, VEC["sink"]:VEC["sink"] + 8] = inp["sink_b"][i][None, :]
    else:
        for j in range(3):
            v[:, VEC["cw"] + 8 * j:VEC["cw"] + 8 * j + 8] = colpack(inp["conv_w"][i][j])
    v[:, VEC["vlo"]] = float(half)
    v[:, VEC["vhi"]] = float(1 - half)
    return v


W_QA, W_QB, W_KA, W_KB, W_V = 0, 512, 1024, 1152, 1280
NWIN = 1536
NTL = SEQ // NT
NB = 2 + SEQ // 128


class QKWork:
    def __init__(self, kb, es):
        S = kb.S
        self.sq = kb.sb(es, "qk_sq", [128, NT], BF16)
        self.sd = kb.sb(es, "qk_sd", [128, NT], F32)
        self.r = kb.sb(es, "qk_r", [128, NT], F32)
        self.qn = kb.sb(es, "qk_qn", [128, NT], BF16)
        self.t1 = kb.sb(es, "qk_t1", [128, NT], F32)
        self.t2 = kb.sb(es, "qk_t2", [128, NT], F32)
        self.b = {n: S.buf("qk_" + n) for n in ("sq", "sd", "r", "qn", "t1", "t2")}


def qk_norm_rope(kb, cm, qw, ps, b_ps, N, gcol, cs, b_cs, ms_ps, b_ms, rot_ps, b_rot, out_ap, b_out):
    S = kb.S
    vec = cm.vec
    S.op("act", lambda e: e.activation(out=qw.sq[:, :N], in_=ps[:, :N], func=ACT.Square), reads=[b_ps], writes=[qw.b["sq"]])
    S.op("pe", lambda e: e.matmul(ms_ps[:, :N], lhsT=cm.bones, rhs=qw.sq[:, :N], start=True, stop=True), reads=[qw.b["sq"], cm.b_cst], writes=[b_ms])
    S.op("act", lambda e: e.activation(out=qw.sd[:, :N], in_=ms_ps[:, :N], func=ACT.Sqrt, bias=cm.eps_col, scale=1.0), reads=[b_ms, cm.b_eps], writes=[qw.b["sd"]])
    S.op("dve", lambda e: e.reciprocal(out=qw.r[:, :N], in_=qw.sd[:, :N]), reads=[qw.b["sd"]], writes=[qw.b["r"]])
    if cs is None:
        S.op("dve", lambda e: e.scalar_tensor_tensor(out=out_ap, in0=ps[:, :N], scalar=vec[:, gcol:gcol + 1], in1=qw.r[:, :N], op0=ALU.mult, op1=ALU.mult),
             reads=[b_ps, qw.b["r"], cm.b_vec], writes=[b_out])
        return
    cos, sin = cs
    S.op("dve", lambda e: e.scalar_tensor_tensor(out=qw.qn[:, :N], in0=ps[:, :N], scalar=vec[:, gcol:gcol + 1], in1=qw.r[:, :N], op0=ALU.mult, op1=ALU.mult),
         reads=[b_ps, qw.b["r"], cm.b_vec], writes=[qw.b["qn"]])
    S.op("pe", lambda e: e.matmul(rot_ps[:, :N], lhsT=cm.rotm, rhs=qw.qn[:, :N], start=True, stop=True), reads=[qw.b["qn"], cm.b_cst], writes=[b_rot])
    S.op("dve", lambda e: e.tensor_tensor(out=qw.t1[:, :N], in0=qw.qn[:, :N], in1=cos[:, :N], op=ALU.mult), reads=[qw.b["qn"], b_cs], writes=[qw.b["t1"]])
    S.op("dve", lambda e: e.tensor_tensor(out=qw.t2[:, :N], in0=rot_ps[:, :N], in1=sin[:, :N], op=ALU.mult), reads=[b_rot, b_cs], writes=[qw.b["t2"]])
    S.op("pool", lambda e: e.tensor_tensor(out=out_ap, in0=qw.t1[:, :N], in1=qw.t2[:, :N], op=ALU.add), reads=[qw.b["t1"], qw.b["t2"]], writes=[b_out])


def attn_phase(kb, cm, w_in_d, w_out_d, cosd, sind, bandd, xs, xc, xm, xcm, with_ctx_out):
    nc, S = kb.nc, kb.S
    vec = cm.vec
    with ExitStack() as es:
        KA = kb.sb(es, "KA", [128, NB * 128], BF16)
        KBc = kb.sb(es, "KB", [128, NB * 128], BF16)
        VA = kb.sb(es, "VA", [128, NB, 2, 65], BF16)
        VB = kb.sb(es, "VB", [128, NB, 2, 65], BF16)
        b_KA, b_VA, b_KB, b_VB = S.buf("KA"), S.buf("VA"), S.buf("KB"), S.buf("VB")
        win = kb.sb(es, "a_win", [128, KC, NWIN], BF16)
        wout = kb.sb(es, "a_wout", [128, KC, D], BF16)
        b_win, b_wout = S.buf("a_win"), S.buf("a_wout")
        for j in range(3):
            c0 = j * 512
            S.op("pool", lambda e: e.dma_start(out=win[:, :, c0:c0 + 512], in_=chunked(w_in_d[:, c0:c0 + 512])), writes=[b_win], dma_home=S.buf("a_win_d"))
        S.op("pool", lambda e: e.dma_start(out=wout[:], in_=chunked(w_out_d)), writes=[b_wout], dma_home=b_wout)
        band = kb.sb(es, "a_band", [128, 384], BF16)
        b_band = S.buf("a_band")
        S.op("pool", lambda e: e.dma_start(out=band[:], in_=bandd), writes=[b_band], dma_home=b_band)
        esink = kb.sb(es, "a_esink", [128, 8], F32)
        b_esink = S.buf("a_esink")
        S.op("act", lambda e: e.activation(out=esink[:], in_=vec[:, VEC["sink"]:VEC["sink"] + 8], func=ACT.Exp), reads=[cm.b_vec], writes=[b_esink])
        xt = kb.sb(es, "a_xt", [128, KC, NT], F32)
        b_xt = S.buf("a_xt")
        h = kb.sb(es, "a_h", [128, KC, NT], BF16)
        b_h = S.buf("a_h")
        Q = kb.sb(es, "a_Q", [128, 8, NT], BF16)
        b_Q = S.bufs("a_Q", 8)
        oT = kb.sb(es, "a_oT", [128, 8, NT], BF16)
        b_oT = S.bufs("a_oT", 16)
        nw = NormWork(kb, es, "a_nw")
        qw = QKWork(kb, es)
        cst = kb.sb(es, "a_cos", [128, NT], F32)
        snt = kb.sb(es, "a_sin", [128, NT], F32)
        b_cs = S.buf("a_cs")
        b_cos_d, b_sin_d = S.buf("a_cos_d"), S.buf("a_sin_d")
        psb = [kb.sb(es, f"a_p{i}", [128, NT], BF16) for i in range(3)]
        b_psb = S.bufs("a_p", 3)
        rec = kb.sb(es, "a_rec", [128, NT], F32)
        bcs = kb.sb(es, "a_bc", [64, NT], F32)
        b_rec, b_bcs = S.buf("a_rec"), S.buf("a_bcs")
        banks = [kb.ps(es, f"a_ps{i}", [128, NT], F32) for i in range(8)]
        b_bank = S.bufs("a_bank", 8)

        S.op("dve", lambda e: e.memset(VA[:, :, :, 64:65], 1.0), writes=[b_VA])
        S.op("dve", lambda e: e.memset(VB[:, :, :, 64:65], 1.0), writes=[b_VB])

        def load_tile(src, N, ci, pos0):
            S.op(DMAQ, lambda e: e.dma_start(out=xt[:, :, :N], in_=chunked(src)), writes=[b_xt], dma_home=b_xt)
            if pos0 is not None:
                S.op(DMAQ, lambda e: e.dma_start(out=cst[:, :N], in_=cosd[:, pos0:pos0 + N]), writes=[b_cs], dma_home=b_cos_d)
                S.op(DMAQ, lambda e: e.dma_start(out=snt[:, :N], in_=sind[:, pos0:pos0 + N]), writes=[b_cs], dma_home=b_sin_d)
            norm_mod(kb, cm, nw, xt, b_xt, N, ci, 0, banks[5], b_bank[5], lambda kc: h[:, kc, :N], b_h)

        def project(col, N):
            for kc in range(KC):
                S.op("pe", lambda e: e.matmul(banks[5][:, :N], lhsT=win[:, kc, col:col + 128], rhs=h[:, kc, :N], start=(kc == 0), stop=(kc == KC - 1)),
                     reads=[b_win, b_h], writes=[b_bank[5]])

        def kv_build(src, N, ci, pos0, blk0):
            load_tile(src, N, ci, pos0)
            cs = None if pos0 is None else (cst, snt)
            project(W_KA, N)
            qk_norm_rope(kb, cm, qw, banks[5], b_bank[5], N, VEC["kna"], cs, b_cs, banks[6], b_bank[6], banks[7], b_bank[7],
                         KA[:, blk0 * 128:blk0 * 128 + N], b_KA)
            project(W_KB, N)
            qk_norm_rope(kb, cm, qw, banks[5], b_bank[5], N, VEC["knb"], cs, b_cs, banks[6], b_bank[6], banks[7], b_bank[7],
                         KBc[:, blk0 * 128:blk0 * 128 + N], b_KB)
            for tb in range(N // 128):
                vp = banks[4]
                for kc in range(KC):
                    S.op("pe", lambda e: e.matmul(vp[:, 0:256], lhsT=h[:, kc, tb * 128:(tb + 1) * 128], rhs=win[:, kc, W_V:W_V + 256], start=(kc == 0), stop=(kc == KC - 1)),
                         reads=[b_h, b_win], writes=[b_bank[4]])
                S.op("dve", lambda e: e.tensor_copy(out=VA[:, blk0 + tb, :, 0:64], in_=vp[:, 0:128].rearrange("p (a b) -> p a b", a=2)),
                     reads=[b_bank[4]], writes=[b_VA])
                S.op("dve", lambda e: e.tensor_copy(out=VB[:, blk0 + tb, :, 0:64], in_=vp[:, 128:256].rearrange("p (a b) -> p a b", a=2)),
                     reads=[b_bank[4]], writes=[b_VB])

        kv_build(xc, CTX, 1, None, 0)
        for it in range(NTL):
            kv_build(xs[:, it * NT:(it + 1) * NT], NT, 0, it * NT, 2 + 4 * it)

        def finalize(o_ps, b_o, N, chunk, half, sink_h):
            if sink_h is not None:
                S.op("dve", lambda e: e.tensor_scalar(out=rec[64:65, :N], in0=o_ps[64:65, :N], scalar1=esink[64:65, sink_h:sink_h + 1], scalar2=None, op0=ALU.add),
                     reads=[b_o, b_esink], writes=[b_rec])
                S.op("dve", lambda e: e.reciprocal(out=rec[64:65, :N], in_=rec[64:65, :N]), reads=[b_rec], writes=[b_rec])
            else:
                S.op("dve", lambda e: e.reciprocal(out=rec[64:65, :N], in_=o_ps[64:65, :N]), reads=[b_o], writes=[b_rec])
            S.op("pe", lambda e: e.matmul(banks[7][0:64, :N], lhsT=cm.cst_f[64:65, 1, 0:64], rhs=rec[64:65, :N], start=True, stop=True),
                 reads=[b_rec, cm.b_cstf], writes=[b_bank[7]])
            S.op("dve", lambda e: e.tensor_copy(out=bcs[:, :N], in_=banks[7][0:64, :N]), reads=[b_bank[7]], writes=[b_bcs])
            p0 = half * 64
            S.op("dve", lambda e: e.tensor_tensor(out=oT[p0:p0 + 64, chunk, :N], in0=o_ps[0:64, :N], in1=bcs[:, :N], op=ALU.mult),
                 reads=[b_o, b_bcs], writes=[b_oT[2 * chunk + half]])

        scnt = [0]

        def attend(o_ps, b_o, Kc, b_K, Vc, b_V, kv, qc, N, blocks):
            nb = len(blocks)
            for bi, (blk, q0, q1, m0) in enumerate(blocks):
                si = scnt[0] % 3
                scnt[0] += 1
                S.op("pe", lambda e: e.matmul(banks[si][:, q0:q1], lhsT=Kc[kv * 64:(kv + 1) * 64, blk * 128:(blk + 1) * 128],
                                              rhs=Q[kv * 64:(kv + 1) * 64, qc, q0:q1], start=True, stop=True),
                     reads=[b_K, b_Q[qc]], writes=[b_bank[si]])
                S.op("act", lambda e: e.activation(out=psb[si][:, q0:q1], in_=banks[si][:, q0:q1], func=ACT.Exp, scale=SCALE),
                     reads=[b_bank[si]], writes=[b_psb[si]])
                if m0 is not None:
                    S.op("dve", lambda e: e.tensor_tensor(out=psb[si][:, q0:q1], in0=psb[si][:, q0:q1], in1=band[:, m0:m0 + (q1 - q0)], op=ALU.mult),
                         reads=[b_band], writes=[b_psb[si]])
                S.op("pe", lambda e: e.matmul(o_ps[0:65, q0:q1], lhsT=Vc[:, blk, kv, 0:65], rhs=psb[si][:, q0:q1], start=(bi == 0), stop=(bi == nb - 1),
                                              skip_group_check=True),
                     reads=[b_V, b_psb[si]], writes=[b_o])

        def q_tile(src, dst, N, ci, pos0, it):
            load_tile(src, N, ci, pos0)
            cs = None if pos0 is None else (cst, snt)
            for c in range(8):
                project((W_QA if c < 4 else W_QB) + (c % 4) * 128, N)
                qk_norm_rope(kb, cm, qw, banks[5], b_bank[5], N, VEC["qna"] if c < 4 else VEC["qnb"], cs, b_cs, banks[6], b_bank[6], banks[7], b_bank[7],
                             Q[:, c, :N], b_Q[c])
            hcnt = 0
            for grp in range(2):
                for hd in range(8):
                    kv, c = hd // 4, hd % 4
                    ob = 3 + hcnt % 2
                    hcnt += 1
                    if grp == 0:
                        blocks = [(0, 0, N, None), (1, 0, N, None)] if it is None else [(b, 0, N, None) for b in range(NB)]
                        attend(banks[ob], b_bank[ob], KA, b_KA, VA, b_VA, kv, c, N, blocks)
                        finalize(banks[ob], b_bank[ob], N, c, kv, None)
                    else:
                        blocks = [(0, 0, N, None), (1, 0, N, None)]
                        if it is not None:
                            for j in range(6):
                                lb = it * 4 - 1 + j
                                if lb < 0 or lb >= SEQ // 128:
                                    continue
                                q0 = max(0, 128 * (j - 2))
                                q1 = min(512, 128 * (j - 2) + 384)
                                blocks.append((2 + lb, q0, q1, q0 - 128 * (j - 2)))
                        attend(banks[ob], b_bank[ob], KBc, b_KB, VB, b_VB, kv, 4 + c, N, blocks)
                        finalize(banks[ob], b_bank[ob], N, 4 + c, kv, hd)
            for oc in range(KC):
                pk = 5 + oc % 2
                for c in range(8):
                    S.op("pe", lambda e: e.matmul(banks[pk][:, :N], lhsT=wout[:, c, oc * 128:(oc + 1) * 128], rhs=oT[:, c, :N], start=(c == 0), stop=(c == 7)),
                         reads=[b_wout, b_oT[2 * c], b_oT[2 * c + 1]], writes=[b_bank[pk]])
                S.op("dve", lambda e: e.scalar_tensor_tensor(out=xt[:, oc, :N], in0=banks[pk][:, :N], scalar=cm.gate(ci, 0, oc), in1=xt[:, oc, :N], op0=ALU.mult, op1=ALU.add),
                     reads=[b_bank[pk], cm.b_mod], writes=[b_xt])
            S.op(DMAQ, lambda e: e.dma_start(out=chunked(dst), in_=xt[:, :, :N]), reads=[b_xt], dma_home=b_xt)

        if with_ctx_out:
            q_tile(xc, xcm, CTX, 1, None, None)
        for it in range(NTL):
            q_tile(xs[:, it * NT:(it + 1) * NT], xm[:, it * NT:(it + 1) * NT], NT, 0, it * NT, it)
    S.barrier()


def with_ctx_tiles(ft, fc):
    return ft[:4] + fc + ft[4:]


def build_fused(n_layers=4, n_exp=NE):
    global DMAQ
    DMAQ = "sp"
    kb = KB()
    x0 = kb.din("x0", [D, SEQ])
    xc0 = kb.din("xc0", [D, CTX])
    vecd = kb.din("vec", [4, 128, NVEC])
    ada_w = kb.din("ada_w", [4, D, 6 * D])
    consts = dict(cbf=kb.din("cbf", [128, 3, 128]), cf=kb.din("cf", [128, 2, 128]))
    kb.din("selm", [NE, NE, 128])
    w_in = kb.din("w_in", [2, D, NWIN])
    w_out = kb.din("w_out", [2, D, D])
    cosd = kb.din("rcos", [128, SEQ])
    sind = kb.din("rsin", [128, SEQ])
    bandd = kb.din("bandm", [128, 384])
    fwg = kb.din("fwg", [2, D, DFF])
    fwu = kb.din("fwu", [2, D, DFF])
    fwd = kb.din("fwd", [2, DFF, D])
    cw_in = kb.din("cw_in", [2, D, 3 * D])
    cw_out = kb.din("cw_out", [2, D, D])
    router_w = kb.din("router_w", [2, D, NE])
    mwg = kb.din("mwg", [2, n_exp, D, DFFE])
    mwu = kb.din("mwu", [2, n_exp, D, DFFE])
    mwd = kb.din("mwd", [2, n_exp, DFFE, D])
    yo = kb.dout("yo", [D, SEQ])
    xm = kb.dscratch("xm", [D, SEQ])
    xcm = kb.dscratch("xcm", [D, CTX])
    xp1 = kb.dscratch("xp1", [D, SEQ + 2])
    xcp1 = kb.dscratch("xcp1", [D, CTX + 2])
    x2 = kb.dscratch("x2", [D, SEQ])
    xc2 = kb.dscratch("xc2", [D, CTX])
    xp3 = kb.dscratch("xp3", [D, SEQ + 2])
    outs = []
    zt = kb.sb(kb.es, "zero_col", [128, KC, 1], F32)
    b_zt = kb.S.buf("zero_col")
    kb.S.op("dve", lambda e: e.memset(zt[:], 0.0), writes=[b_zt])
    for buf, n in ((xp1, SEQ), (xcp1, CTX), (xp3, SEQ)):
        for col in (0, n + 1):
            kb.S.op(DMAQ, lambda e: e.dma_start(out=chunked(buf[:, col:col + 1]), in_=zt[:], allow_slow_non_contiguous=True), reads=[b_zt], dma_home=kb.S.buf("zc_d"))
    for layer in range(n_layers):
        i = layer // 2
        with ExitStack() as es:
            cm = Common(kb, vecd[layer], ada_w[layer], consts, es=es)
            if layer == 0:
                attn_phase(kb, cm, w_in[0], w_out[0], cosd, sind, bandd, x0, xc0, xm, xcm, True)
                ft = with_ctx_tiles(ffn_tiles(xm, xp1[:, 1:SEQ + 1], 0, SEQ), ffn_tiles(xcm, xcp1[:, 1:CTX + 1], 1, CTX))
                outs = ffn_phase(kb, cm, ft, [(fwg[0], fwu[0], fwd[0])], DFF)
            elif layer == 1:
                ct = conv_tiles(xp1, xm, 0, SEQ, "zero", "zero") + conv_tiles(xcp1, xcm, 1, CTX, "zero", "zero")
                conv_phase(kb, cm, ct, cw_in[0], cw_out[0])
                ft = with_ctx_tiles(ffn_tiles(xm, x2, 0, SEQ), ffn_tiles(xcm, xc2, 1, CTX))
                outs = ffn_phase(kb, cm, ft, [(mwg[0, e], mwu[0, e], mwd[0, e]) for e in range(n_exp)], DFFE, router_w=router_w[0])
            elif layer == 2:
                attn_phase(kb, cm, w_in[1], w_out[1], cosd, sind, bandd, x2, xc2, xm, None, False)
                outs = ffn_phase(kb, cm, ffn_tiles(xm, xp3[:, 1:SEQ + 1], 0, SEQ), [(fwg[1], fwu[1], fwd[1])], DFF)
            else:
                conv_phase(kb, cm, conv_tiles(xp3, xm, 0, SEQ, "zero", "zero"), cw_in[1], cw_out[1])
                outs = ffn_phase(kb, cm, ffn_tiles(xm, yo, 0, SEQ), [(mwg[1, e], mwu[1, e], mwd[1, e]) for e in range(n_exp)], DFFE, router_w=router_w[1])
    if n_layers < 4:
        pass
    stats = kb.S.emit(kb.nc, final_wait_ops=outs)
    kb.es.close()
    return kb.nc, stats


def rope_tables():
    half = 32
    inv = (10000.0 ** (-np.arange(0, half, 2, dtype=np.float32) / half)).astype(np.float32)
    pos = np.arange(SEQ)
    row = (pos // 64).astype(np.float32)
    col = (pos % 64).astype(np.float32)
    ang = np.concatenate([row[:, None] * inv[None, :], col[:, None] * inv[None, :]], axis=-1)
    cos = np.cos(ang).astype(np.float32)
    sin = np.sin(ang).astype(np.float32)
    pidx = (np.arange(128) % 64) // 2
    return np.ascontiguousarray(cos[:, pidx].T), np.ascontiguousarray(sin[:, pidx].T)


def band_mask():
    kk = np.arange(128)[:, None]
    u = np.arange(384)[None, :] - 128
    return ((u >= kk - 128) & (u <= kk + 128)).astype(np.float32)


def perm_w_in(w):
    cols = []
    for base in (0, 768):
        for c in range(4):
            cols += [w[:, base + c * 64:base + (c + 1) * 64], w[:, base + (4 + c) * 64:base + (5 + c) * 64]]
    cols += [w[:, 512:640], w[:, 1280:1408], w[:, 640:768], w[:, 1408:1536]]
    return np.ascontiguousarray(np.concatenate(cols, axis=1))


def perm_w_out(w):
    rows = []
    for base in (0, 512):
        for c in range(4):
            rows += [w[base + c * 64:base + (c + 1) * 64], w[base + (4 + c) * 64:base + (5 + c) * 64]]
    return np.ascontiguousarray(np.concatenate(rows, axis=0))


_NC = {}


def make_inputs(inp, b):
    cbf, cf, selm = make_consts()
    cos, sin = rope_tables()
    return dict(
        x0=np.ascontiguousarray(inp["x"][b].T), xc0=np.ascontiguousarray(inp["ctx"][b].T),
        vec=np.stack([make_vec(inp, l, b, 0) for l in range(4)]), ada_w=inp["ada_w"], cbf=cbf, cf=cf, selm=selm,
        w_in=np.stack([perm_w_in(inp["attn_w_in"][i]) for i in range(2)]), w_out=np.stack([perm_w_out(inp["attn_w_out"][i]) for i in range(2)]),
        rcos=cos, rsin=sin, bandm=band_mask(), fwg=inp["ffn_w_gate"], fwu=inp["ffn_w_up"], fwd=inp["ffn_w_down"],
        cw_in=inp["conv_w_in"], cw_out=inp["conv_w_out"], router_w=inp["router_w"],
        mwg=inp["moe_w_gate"], mwu=inp["moe_w_up"], mwd=inp["moe_w_down"])


def kernel(**inp):
    inp = {k: np.asarray(v) for k, v in inp.items()}
    B = inp["x"].shape[0]
    if "nc" not in _NC:
        _NC["nc"] = build_fused()[0]
    per_b = [make_inputs(inp, b) for b in range(B)]
    ins = [per_b[c // 2] for c in range(8)]
    res = run_bass_kernel_spmd(_NC["nc"], ins, core_ids=list(range(8)))
    return np.stack([np.ascontiguousarray(res.results[2 * b]["yo"].T) for b in range(B)]).astype(np.float32)
```

```python
import os
import numpy as np
from contextlib import ExitStack
import concourse.bass as bass
import concourse.mybir as mybir
from concourse.bass_utils import run_bass_kernel_spmd

F32 = mybir.dt.float32
BF16 = mybir.dt.bfloat16
ACT = mybir.ActivationFunctionType
ALU = mybir.AluOpType
AX = mybir.AxisListType

D = 1024
KC = 8
SEQ = 8192
HALF = 4096
CTX = 256
NT = 512
DFF = 2816
DFFE = 3584
NE = 8
EPS = 1e-6
SCALE = 0.125
ENG = ("pe", "act", "dve", "pool", "sp")
DMAQ = "pool"


class Buf:
    __slots__ = ("name", "last_w", "readers", "dma_sem_idx")

    def __init__(self, name):
        self.name = name
        self.last_w = None
        self.readers = []
        self.dma_sem_idx = None


class Op:
    __slots__ = ("eng", "fn", "deps", "is_dma", "sem", "signal", "count", "idx")


class _Rec:
    def __init__(self):
        self.call = None

    def __getattr__(self, name):
        def f(*a, **k):
            self.call = (name, a, k)
            return None
        return f


class Sched:
    def __init__(self, same_engine_sync=True):
        self.ops = []
        self.same_engine_sync = same_engine_sync
        self.n_dma_sems = 0
        self.last_eng = {}
        self.dma_since_bar = []

    def buf(self, name):
        return Buf(name)

    def bufs(self, name, n):
        return [Buf(f"{name}{i}") for i in range(n)]

    def op(self, eng, fn, reads=(), writes=(), dma_home=None, extra_deps=()):
        o = Op()
        o.eng = eng
        rec = _Rec()
        fn(rec)
        o.fn = rec.call
        assert o.fn is not None
        o.is_dma = dma_home is not None
        o.idx = len(self.ops)
        o.signal = False
        o.count = None
        deps = set(extra_deps)
        for b in reads:
            if b.last_w is not None:
                deps.add(b.last_w)
        for b in writes:
            if b.last_w is not None:
                deps.add(b.last_w)
            for r in b.readers:
                deps.add(r)
        fdeps = []
        for d in deps:
            dop = self.ops[d]
            if (not dop.is_dma) and (not o.is_dma) and dop.eng == eng:
                if eng == "pe" or not self.same_engine_sync:
                    continue
            fdeps.append(d)
        o.deps = fdeps
        if o.is_dma:
            if dma_home.dma_sem_idx is None:
                dma_home.dma_sem_idx = self.n_dma_sems
                self.n_dma_sems += 1
            o.sem = ("dma", dma_home.dma_sem_idx)
            self.dma_since_bar.append(o.idx)
        else:
            o.sem = ("eng", eng)
            self.last_eng[eng] = o.idx
        for b in reads:
            b.readers.append(o.idx)
        for b in writes:
            b.last_w = o.idx
            b.readers = []
        self.ops.append(o)
        return o

    def barrier(self):
        deps = list(self.last_eng.values()) + list(self.dma_since_bar)
        self.dma_since_bar = []
        for e in ENG:
            o = Op()
            o.eng = e
            o.fn = None
            o.is_dma = False
            o.idx = len(self.ops)
            o.signal = False
            o.count = None
            o.sem = ("eng", e)
            o.deps = [d for d in deps if not (self.ops[d].eng == e and not self.ops[d].is_dma and e == "pe")]
            self.ops.append(o)

    def emit(self, nc, final_wait_ops=()):
        ops = self.ops
        for o in ops:
            for d in o.deps:
                ops[d].signal = True
        for o in final_wait_ops:
            o.signal = True
        counters = {}
        for o in ops:
            if o.fn is None:
                o.signal = False
                continue
            if o.signal or o.is_dma:
                inc = 16 if o.is_dma else 1
                counters[o.sem] = counters.get(o.sem, 0) + inc
                o.count = counters[o.sem]
                o.signal = True
        with ExitStack() as es:
            sems = {}
            for key in counters:
                sems[key] = es.enter_context(nc.semaphore(f"s_{key[0]}_{key[1]}"))
            block = es.enter_context(nc.Block())
            per_eng = {e: [o for o in ops if o.eng == e] for e in ENG}
            n_waits = [0]

            def make(engname):
                def body(engobj):
                    waited = {}
                    for o in per_eng[engname]:
                        need = {}
                        for d in o.deps:
                            dop = ops[d]
                            if dop.count is None:
                                continue
                            if dop.count > need.get(dop.sem, 0):
                                need[dop.sem] = dop.count
                        for sk, v in need.items():
                            if waited.get(sk, 0) >= v:
                                continue
                            engobj.wait_ge(sems[sk], v)
                            n_waits[0] += 1
                            waited[sk] = v
                        if o.fn is None:
                            continue
                        ins = getattr(engobj, o.fn[0])(*o.fn[1], **o.fn[2])
                        if o.signal:
                            ins.then_inc(sems[o.sem], 16 if o.is_dma else 1)
                    if engname == "sp":
                        for fo in final_wait_ops:
                            engobj.wait_ge(sems[fo.sem], fo.count)
                return body

            block.tensor(make("pe"))
            block.scalar(make("act"))
            block.vector(make("dve"))
            block.gpsimd(make("pool"))
            block.sync(make("sp"))
        self.stats = dict(n_ops=len(ops), n_sems=len(counters), n_waits=n_waits[0],
                          per_eng={e: len(per_eng[e]) for e in ENG})
        return self.stats


VEC = {}
_c = 0
for _n, _w in [("c", 8), ("cc", 8), ("n1", 8), ("n2", 8), ("adab", 48), ("qna", 1), ("kna", 1),
               ("qnb", 1), ("knb", 1), ("sink", 8), ("vlo", 1), ("vhi", 1), ("cw", 24)]:
    VEC[_n] = _c
    _c += _w
NVEC = _c


class KB:
    def __init__(self):
        self.nc = bass.Bass("TRN2", target_bir_lowering=False)
        self.S = Sched()
        self.es = ExitStack()
        self.dram_in = {}
        self.uid = 0
        self.dump = None

    def din(self, name, shape, dt=F32):
        t = self.nc.dram_tensor(name, list(shape), dt, kind="ExternalInput").ap()
        self.dram_in[name] = t
        return t

    def dout(self, name, shape, dt=F32):
        return self.nc.dram_tensor(name, list(shape), dt, kind="ExternalOutput").ap()

    def dscratch(self, name, shape, dt=F32):
        return self.nc.dram_tensor(name, list(shape), dt).ap()

    def sb(self, es, name, shape, dt):
        self.uid += 1
        return es.enter_context(self.nc.sbuf_tensor(f"sb{self.uid}_{name}", list(shape), dt))

    def ps(self, es, name, shape, dt=F32):
        self.uid += 1
        return es.enter_context(self.nc.psum_tensor(f"ps{self.uid}_{name}", list(shape), dt))

    def name(self, p):
        self.uid += 1
        return f"{p}{self.uid}"


def chunked(ap):
    return ap.rearrange("(k p) n -> p k n", p=128)


class Common:
    def __init__(self, kb, layer_vec, ada_w, consts, es=None):
        self.kb = kb
        nc, S = kb.nc, kb.S
        es = es if es is not None else kb.es
        self.vec = kb.sb(es, "vec", [128, NVEC], F32)
        self.b_vec = S.buf("vec")
        S.op(DMAQ, lambda e: e.dma_start(out=self.vec[:], in_=layer_vec), writes=[self.b_vec], dma_home=self.b_vec)
        self.cst_bf = kb.sb(es, "cst_bf", [128, 3, 128], BF16)
        self.b_cst = S.buf("cst")
        S.op("pool", lambda e: e.dma_start(out=self.cst_bf[:], in_=consts["cbf"]), writes=[self.b_cst], dma_home=self.b_cst)
        self.cst_f = kb.sb(es, "cst_f", [128, 2, 128], F32)
        self.b_cstf = S.buf("cstf")
        S.op(DMAQ, lambda e: e.dma_start(out=self.cst_f[:], in_=consts["cf"]), writes=[self.b_cstf], dma_home=self.b_cstf)
        self.onesm = self.cst_bf[:, 0, :]
        self.bones = self.cst_bf[:, 1, :]
        self.rotm = self.cst_bf[:, 2, :]
        self.ident = self.cst_f[:, 0, :]
        self.ones_f = self.cst_f[:, 1, :]
        self.eps_t = kb.sb(es, "eps_t", [128, 1], F32)
        self.b_eps = S.buf("eps")
        S.op("dve", lambda e: e.memset(self.eps_t[:], EPS), writes=[self.b_eps])
        self.eps_col = self.eps_t[:, 0:1]
        self.mod = kb.sb(es, "mod", [128, 2, 48], F32)
        self.geff = kb.sb(es, "geff", [128, 2, 2, 8], F32)
        self.b_mod = S.buf("mod")
        self._build_mod(ada_w)

    def _build_mod(self, ada_w):
        kb = self.kb
        nc, S = kb.nc, kb.S
        with ExitStack() as es:
            sc = kb.sb(es, "silu_c", [128, 8, 2], BF16)
            sc32 = kb.sb(es, "silu_c32", [128, 2, 8], F32)
            b_sc = S.buf("silu_c")
            wsl = [kb.sb(es, f"adaw{i}", [128, 8, 1024], BF16) for i in range(2)]
            b_w = S.bufs("adaw", 2)
            mps = kb.ps(es, "mod_ps", [128, 48, 2], F32)
            b_mps = S.buf("mod_ps")
            vec = self.vec
            S.op("act", lambda e: e.activation(out=sc32[:, 0, :], in_=vec[:, VEC["c"]:VEC["c"] + 8], func=ACT.Silu),
                 reads=[self.b_vec], writes=[b_sc])
            S.op("act", lambda e: e.activation(out=sc32[:, 1, :], in_=vec[:, VEC["cc"]:VEC["cc"] + 8], func=ACT.Silu),
                 reads=[self.b_vec], writes=[b_sc])
            for ci in range(2):
                S.op("dve", lambda e, ci=ci: e.tensor_copy(out=sc[:, :, ci], in_=sc32[:, ci, :]), reads=[b_sc], writes=[b_sc])
            for j in range(6):
                sl = j % 2
                S.op("pool", lambda e, j=j, sl=sl: e.dma_start(out=wsl[sl][:], in_=chunked(ada_w[:, j * 1024:(j + 1) * 1024])),
                     writes=[b_w[sl]], dma_home=b_w[sl])
                for oc in range(8):
                    for kc in range(8):
                        S.op("pe", lambda e, j=j, sl=sl, oc=oc, kc=kc: e.matmul(
                            mps[:, j * 8 + oc, :], lhsT=wsl[sl][:, kc, oc * 128:(oc + 1) * 128], rhs=sc[:, kc, :],
                            start=(kc == 0), stop=(kc == 7)), reads=[b_w[sl], b_sc], writes=[b_mps])
            ab = VEC["adab"]
            for ci in range(2):
                S.op("dve", lambda e, ci=ci: e.tensor_tensor(out=self.mod[:, ci, :], in0=mps[:, :, ci], in1=vec[:, ab:ab + 48], op=ALU.add),
                     reads=[b_mps, self.b_vec], writes=[self.b_mod])
            for ci in range(2):
                for ni in range(2):
                    gcol = VEC["n1"] if ni == 0 else VEC["n2"]
                    scj = 1 if ni == 0 else 4
                    S.op("dve", lambda e, ci=ci, ni=ni, gcol=gcol, scj=scj: e.scalar_tensor_tensor(
                        out=self.geff[:, ci, ni, :], in0=self.mod[:, ci, scj * 8:scj * 8 + 8], scalar=1.0,
                        in1=vec[:, gcol:gcol + 8], op0=ALU.add, op1=ALU.mult),
                        reads=[self.b_mod, self.b_vec], writes=[self.b_mod])
        S.barrier()

    def sh(self, ci, ni, kc):
        j = 0 if ni == 0 else 3
        return self.mod[:, ci, j * 8 + kc:j * 8 + kc + 1]

    def gate(self, ci, ni, kc):
        j = 2 if ni == 0 else 5
        return self.mod[:, ci, j * 8 + kc:j * 8 + kc + 1]

    def ge(self, ci, ni, kc):
        return self.geff[:, ci, ni, kc:kc + 1]


class NormWork:
    def __init__(self, kb, es, tag):
        S = kb.S
        self.sq = [kb.sb(es, f"{tag}_sq{i}", [128, NT], BF16) for i in range(2)]
        self.b_sq = S.bufs(f"{tag}_sq", 2)
        self.sd = kb.sb(es, f"{tag}_sd", [128, NT], F32)
        self.rstd = kb.sb(es, f"{tag}_rstd", [128, NT], F32)
        self.b_sd = S.buf(f"{tag}_sd")
        self.b_rstd = S.buf(f"{tag}_rstd")
        self.tmp = [kb.sb(es, f"{tag}_tmp{i}", [128, NT], F32) for i in range(2)]
        self.b_tmp = S.bufs(f"{tag}_tmp", 2)


def norm_mod(kb, cm, nw, xt, b_xt, N, ci, ni, ms_ps, b_ms, h_out, b_h, inplace=False):
    S = kb.S
    for kc in range(KC):
        sl = kc % 2
        S.op("act", lambda e, kc=kc, sl=sl: e.activation(out=nw.sq[sl][:, :N], in_=xt[:, kc, :N], func=ACT.Square),
             reads=[b_xt], writes=[nw.b_sq[sl]])
        S.op("pe", lambda e, kc=kc, sl=sl: e.matmul(ms_ps[:, :N], lhsT=cm.onesm, rhs=nw.sq[sl][:, :N], start=(kc == 0), stop=(kc == KC - 1)),
             reads=[nw.b_sq[sl], cm.b_cst], writes=[b_ms])
    S.op("act", lambda e: e.activation(out=nw.sd[:, :N], in_=ms_ps[:, :N], func=ACT.Sqrt, bias=cm.eps_col, scale=1.0),
         reads=[b_ms, cm.b_eps], writes=[nw.b_sd])
    S.op("dve", lambda e: e.reciprocal(out=nw.rstd[:, :N], in_=nw.sd[:, :N]), reads=[nw.b_sd], writes=[nw.b_rstd])
    for kc in range(KC):
        sl = kc % 2
        if inplace:
            S.op("dve", lambda e, kc=kc: e.scalar_tensor_tensor(out=xt[:, kc, :N], in0=xt[:, kc, :N], scalar=cm.ge(ci, ni, kc),
                                                                 in1=nw.rstd[:, :N], op0=ALU.mult, op1=ALU.mult),
                 reads=[nw.b_rstd, cm.b_mod], writes=[b_xt])
            S.op("act", lambda e, kc=kc: e.activation(out=xt[:, kc, :N], in_=xt[:, kc, :N], func=ACT.Identity, bias=cm.sh(ci, ni, kc), scale=1.0),
                 reads=[cm.b_mod], writes=[b_xt])
            S.op("pool", lambda e, kc=kc: e.tensor_copy(out=h_out(kc), in_=xt[:, kc, :N]), reads=[b_xt], writes=[b_h])
        else:
            S.op("dve", lambda e, kc=kc, sl=sl: e.scalar_tensor_tensor(out=nw.tmp[sl][:, :N], in0=xt[:, kc, :N], scalar=cm.ge(ci, ni, kc),
                                                                        in1=nw.rstd[:, :N], op0=ALU.mult, op1=ALU.mult),
                 reads=[b_xt, nw.b_rstd, cm.b_mod], writes=[nw.b_tmp[sl]])
            S.op("act", lambda e, kc=kc, sl=sl: e.activation(out=h_out(kc), in_=nw.tmp[sl][:, :N], func=ACT.Identity, bias=cm.sh(ci, ni, kc), scale=1.0),
                 reads=[nw.b_tmp[sl], cm.b_mod], writes=[b_h])


def ffn_phase(kb, cm, tiles, experts, dff, router_w=None):
    nc, S = kb.nc, kb.S
    moe = router_w is not None
    FS = 256
    nsl = dff // FS
    groups = []
    cur, tot = [], 0
    for t in tiles:
        if tot + t["N"] > 2304:
            groups.append(cur)
            cur, tot = [], 0
        cur.append(t)
        tot += t["N"]
    if cur:
        groups.append(cur)
    TMAX = max(sum(t["N"] for t in g) for g in groups)
    out_ops = []
    with ExitStack() as es:
        h2 = kb.sb(es, "f_h2", [128, KC, TMAX], BF16)
        yacc = kb.sb(es, "f_yacc", [128, KC, TMAX], F32)
        wgu = [kb.sb(es, f"f_wgu{i}", [128, 2, KC, FS], BF16) for i in range(2)]
        wdn = [kb.sb(es, f"f_wdn{i}", [128, FS // 128, D], BF16) for i in range(2)]
        b_wg = S.bufs("f_wg", 2)
        b_wu = S.bufs("f_wu", 2)
        b_wd = S.bufs("f_wd", 2)
        xt = kb.sb(es, "f_xt", [128, KC, NT], F32)
        b_xt = S.buf("f_xt")
        nw = NormWork(kb, es, "f_nw")
        sg = [kb.sb(es, f"f_sg{i}", [128, NT], F32) for i in range(2)]
        b_sg = S.bufs("f_sg", 2)
        tt = [kb.sb(es, f"f_tt{i}", [128, NT], F32) for i in range(2)]
        b_tt = S.bufs("f_tt", 2)
        abuf = [kb.sb(es, f"f_a{i}", [128, 2, NT], BF16) for i in range(2)]
        b_a = [S.bufs(f"f_a{i}_", 2) for i in range(2)]
        banks = [kb.ps(es, f"f_ps{i}", [128, NT], F32) for i in range(8)]
        b_bank = S.bufs("f_bank", 8)
        if moe:
            rw = kb.sb(es, "f_rw", [128, KC, NE], F32)
            b_rw = S.buf("f_rw")
            S.op(DMAQ, lambda e: e.dma_start(out=rw[:], in_=chunked(router_w)), writes=[b_rw], dma_home=b_rw)
            gT = kb.sb(es, "f_gT", [NE, TMAX], F32)
            Gb = kb.sb(es, "f_Gb", [128, TMAX], F32)
            sel = kb.sb(es, "f_sel", [NE, NE, 128], F32)
            b_sel = S.buf("f_sel")
            S.op(DMAQ, lambda e: e.dma_start(out=sel[:], in_=kb.dram_in["selm"]), writes=[b_sel], dma_home=b_sel)
            rt = {n: kb.sb(es, f"f_rt_{n}", [128, w], F32) for n, w in
                  [("lg", 8), ("m1", 1), ("eq", 8), ("lg2", 8), ("m2", 1), ("sel", 8), ("nm1", 1), ("ex", 8), ("w", 8), ("ss", 1), ("rs", 1), ("g", 8)]}
            b_rt = S.buf("f_rt")

        for g in groups:
            offs = []
            o = 0
            for t in g:
                offs.append(o)
                o += t["N"]
            b_h2 = S.bufs("f_h2_", len(g))
            b_y = S.bufs("f_y_", len(g))
            b_gT = S.bufs("f_gT_", len(g))
            b_Gb = S.bufs("f_Gb_", len(g))
            for ti, t in enumerate(g):
                N, off, ci = t["N"], offs[ti], t["ci"]
                S.op(DMAQ, lambda e, t=t, N=N: e.dma_start(out=xt[:, :, :N], in_=chunked(t["src"])), writes=[b_xt], dma_home=b_xt)
                norm_mod(kb, cm, nw, xt, b_xt, N, ci, 1, banks[0], b_bank[0],
                         lambda kc, off=off, N=N: h2[:, kc, off:off + N], b_h2[ti], inplace=moe)
                if moe:
                    for tb in range(N // 128):
                        lgp = banks[1]
                        for kc in range(KC):
                            S.op("pe", lambda e, kc=kc, tb=tb: e.matmul(lgp[:, 0:NE], lhsT=xt[:, kc, tb * 128:(tb + 1) * 128], rhs=rw[:, kc, :],
                                                                       start=(kc == 0), stop=(kc == KC - 1)),
                                 reads=[b_xt, b_rw], writes=[b_bank[1]])
                        R = rt
                        S.op("dve", lambda e: e.tensor_copy(out=R["lg"][:], in_=lgp[:, 0:NE]), reads=[b_bank[1]], writes=[b_rt])
                        S.op("dve", lambda e: e.tensor_reduce(out=R["m1"][:], in_=R["lg"][:], axis=AX.X, op=ALU.max), reads=[b_rt], writes=[b_rt])
                        S.op("dve", lambda e: e.tensor_scalar(out=R["eq"][:], in0=R["lg"][:], scalar1=R["m1"][:], scalar2=None, op0=ALU.is_ge), reads=[b_rt], writes=[b_rt])
                        S.op("dve", lambda e: e.scalar_tensor_tensor(out=R["lg2"][:], in0=R["eq"][:], scalar=-1e30, in1=R["lg"][:], op0=ALU.mult, op1=ALU.add), reads=[b_rt], writes=[b_rt])
                        S.op("dve", lambda e: e.tensor_reduce(out=R["m2"][:], in_=R["lg2"][:], axis=AX.X, op=ALU.max), reads=[b_rt], writes=[b_rt])
                        S.op("dve", lambda e: e.tensor_scalar(out=R["sel"][:], in0=R["lg"][:], scalar1=R["m2"][:], scalar2=None, op0=ALU.is_ge), reads=[b_rt], writes=[b_rt])
                        S.op("dve", lambda e: e.tensor_scalar(out=R["nm1"][:], in0=R["m1"][:], scalar1=-1.0, scalar2=None, op0=ALU.mult), reads=[b_rt], writes=[b_rt])
                        S.op("act", lambda e: e.activation(out=R["ex"][:], in_=R["lg"][:], func=ACT.Exp, bias=R["nm1"][:], scale=1.0), reads=[b_rt], writes=[b_rt])
                        S.op("dve", lambda e: e.tensor_tensor(out=R["w"][:], in0=R["ex"][:], in1=R["sel"][:], op=ALU.mult), reads=[b_rt], writes=[b_rt])
                        S.op("dve", lambda e: e.tensor_reduce(out=R["ss"][:], in_=R["w"][:], axis=AX.X, op=ALU.add), reads=[b_rt], writes=[b_rt])
                        S.op("dve", lambda e: e.reciprocal(out=R["rs"][:], in_=R["ss"][:]), reads=[b_rt], writes=[b_rt])
                        S.op("dve", lambda e: e.tensor_scalar(out=R["g"][:], in0=R["w"][:], scalar1=R["rs"][:], scalar2=None, op0=ALU.mult), reads=[b_rt], writes=[b_rt])
                        S.op("pe", lambda e: e.transpose(out=banks[2][0:NE, 0:128], in_=R["g"][:], identity=cm.ident), reads=[b_rt, cm.b_cstf], writes=[b_bank[2]])
                        S.op("dve", lambda e, off=off, tb=tb: e.tensor_copy(out=gT[:, off + tb * 128:off + (tb + 1) * 128], in_=banks[2][0:NE, 0:128]),
                             reads=[b_bank[2]], writes=[b_gT[ti]])
            first = True
            step = 0
            pend = None
            acount = 0
            for ei, (wg_d, wu_d, wd_d) in enumerate(experts):
                if moe:
                    for ti, t in enumerate(g):
                        N, off = t["N"], offs[ti]
                        S.op("pe", lambda e, ei=ei, off=off, N=N: e.matmul(banks[7][:, :N], lhsT=sel[:, ei, :], rhs=gT[:, off:off + N], start=True, stop=True),
                             reads=[b_sel, b_gT[ti]], writes=[b_bank[7]])
                        S.op("act", lambda e, off=off, N=N: e.activation(out=Gb[:, off:off + N], in_=banks[7][:, :N], func=ACT.Copy),
                             reads=[b_bank[7]], writes=[b_Gb[ti]])
                for s in range(nsl):
                    sl = step % 2
                    step += 1
                    f0 = s * FS
                    S.op("pool", lambda e, sl=sl, f0=f0, wg_d=wg_d: e.dma_start(out=wgu[sl][:, 0, :, :], in_=chunked(wg_d[:, f0:f0 + FS])),
                         writes=[b_wg[sl]], dma_home=b_wg[sl])
                    S.op("pool", lambda e, sl=sl, f0=f0, wu_d=wu_d: e.dma_start(out=wgu[sl][:, 1, :, :], in_=chunked(wu_d[:, f0:f0 + FS])),
                         writes=[b_wu[sl]], dma_home=b_wu[sl])
                    S.op("pool", lambda e, sl=sl, f0=f0, wd_d=wd_d: e.dma_start(out=wdn[sl][:], in_=chunked(wd_d[f0:f0 + FS, :])),
                         writes=[b_wd[sl]], dma_home=b_wd[sl])
                    for ti, t in enumerate(g):
                        N, off = t["N"], offs[ti]
                        asl = acount % 2
                        acount += 1

                        def down(half, sl=sl, asl=asl, N=N, off=off, ti=ti, first=first):
                            for oc in range(half * 4, half * 4 + 4):
                                bk = 4 + oc % 4
                                for fc in range(2):
                                    S.op("pe", lambda e, oc=oc, fc=fc, bk=bk: e.matmul(banks[bk][:, :N], lhsT=wdn[sl][:, fc, oc * 128:(oc + 1) * 128],
                                                                                    rhs=abuf[asl][:, fc, :N], start=(fc == 0), stop=(fc == 1)),
                                         reads=[b_wd[sl], b_a[asl][fc]], writes=[b_bank[bk]])
                                if first:
                                    S.op("dve", lambda e, oc=oc, bk=bk: e.tensor_copy(out=yacc[:, oc, off:off + N], in_=banks[bk][:, :N]),
                                         reads=[b_bank[bk]], writes=[b_y[ti]])
                                else:
                                    S.op("dve", lambda e, oc=oc, bk=bk: e.tensor_tensor(out=yacc[:, oc, off:off + N], in0=yacc[:, oc, off:off + N],
                                                                                     in1=banks[bk][:, :N], op=ALU.add),
                                         reads=[b_bank[bk]], writes=[b_y[ti]])

                        for fc in range(2):
                            gb, ub = fc, 2 + fc
                            for kc in range(KC):
                                S.op("pe", lambda e, kc=kc, fc=fc, gb=gb: e.matmul(banks[gb][:, :N], lhsT=wgu[sl][:, 0, kc, fc * 128:(fc + 1) * 128],
                                                                                rhs=h2[:, kc, off:off + N], start=(kc == 0), stop=(kc == KC - 1)),
                                     reads=[b_wg[sl], b_h2[ti]], writes=[b_bank[gb]])
                            for kc in range(KC):
                                S.op("pe", lambda e, kc=kc, fc=fc, ub=ub: e.matmul(banks[ub][:, :N], lhsT=wgu[sl][:, 1, kc, fc * 128:(fc + 1) * 128],
                                                                                rhs=h2[:, kc, off:off + N], start=(kc == 0), stop=(kc == KC - 1)),
                                     reads=[b_wu[sl], b_h2[ti]], writes=[b_bank[ub]])
                            if pend is not None:
                                pend(fc)
                            S.op("act", lambda e, fc=fc, gb=gb: e.activation(out=sg[fc][:, :N], in_=banks[gb][:, :N], func=ACT.Silu),
                                 reads=[b_bank[gb]], writes=[b_sg[fc]])
                            if moe:
                                S.op("dve", lambda e, fc=fc, ub=ub: e.tensor_tensor(out=tt[fc][:, :N], in0=sg[fc][:, :N], in1=banks[ub][:, :N], op=ALU.mult),
                                     reads=[b_sg[fc], b_bank[ub]], writes=[b_tt[fc]])
                                S.op("dve", lambda e, fc=fc, asl=asl: e.tensor_tensor(out=abuf[asl][:, fc, :N], in0=tt[fc][:, :N], in1=Gb[:, off:off + N], op=ALU.mult),
                                     reads=[b_tt[fc], b_Gb[ti]], writes=[b_a[asl][fc]])
                            else:
                                S.op("dve", lambda e, fc=fc, ub=ub, asl=asl: e.tensor_tensor(out=abuf[asl][:, fc, :N], in0=sg[fc][:, :N], in1=banks[ub][:, :N], op=ALU.mult),
                                     reads=[b_sg[fc], b_bank[ub]], writes=[b_a[asl][fc]])
                        pend = down
                    first = False
            if pend is not None:
                pend(0)
                pend(1)
                pend = None
            for ti, t in enumerate(g):
                N, off, ci = t["N"], offs[ti], t["ci"]
                S.op(DMAQ, lambda e, t=t, N=N: e.dma_start(out=xt[:, :, :N], in_=chunked(t["src"])), writes=[b_xt], dma_home=b_xt)
                for kc in range(KC):
                    S.op("dve", lambda e, kc=kc, N=N, off=off, ci=ci: e.scalar_tensor_tensor(
                        out=xt[:, kc, :N], in0=yacc[:, kc, off:off + N], scalar=cm.gate(ci, 1, kc), in1=xt[:, kc, :N], op0=ALU.mult, op1=ALU.add),
                        reads=[b_y[ti], cm.b_mod], writes=[b_xt])
                oo = S.op(DMAQ, lambda e, t=t, N=N: e.dma_start(out=chunked(t["dst"]), in_=xt[:, :, :N]), reads=[b_xt], dma_home=b_xt)
                out_ops.append(oo)
    S.barrier()
    return out_ops


def conv_phase(kb, cm, tiles, cw_in, cw_out):
    nc, S = kb.nc, kb.S
    with ExitStack() as es:
        win = kb.sb(es, "c_win", [128, KC, 3 * D], BF16)
        wout = kb.sb(es, "c_wout", [128, KC, D], BF16)
        b_win = S.bufs("c_win", 3)
        b_wout = S.buf("c_wout")
        for j in range(3):
            for hh in range(2):
                c0 = j * D + hh * 512
                S.op("pool", lambda e, c0=c0: e.dma_start(out=win[:, :, c0:c0 + 512], in_=chunked(cw_in[:, c0:c0 + 512])),
                     writes=[b_win[j]], dma_home=S.buf("c_win_d"))
        S.op("pool", lambda e: e.dma_start(out=wout[:], in_=chunked(cw_out)), writes=[b_wout], dma_home=b_wout)
        xt = kb.sb(es, "c_xt", [128, KC, NT], F32)
        b_xt = S.buf("c_xt")
        h = kb.sb(es, "c_h", [128, KC, NT], BF16)
        b_h = S.buf("c_h")
        mb = kb.sb(es, "c_m", [128, KC, NT], BF16)
        b_m = S.bufs("c_m", KC)
        nw = NormWork(kb, es, "c_nw")
        bsb = [kb.sb(es, f"c_b{i}", [128, NT], BF16) for i in range(2)]
        csb = [kb.sb(es, f"c_c{i}", [128, NT], F32) for i in range(2)]
        zsb = [kb.sb(es, f"c_z{i}", [128, NT], F32) for i in range(2)]
        acc = [kb.sb(es, f"c_acc{i}", [128, NT], F32) for i in range(2)]
        b_bsb, b_csb, b_zsb, b_acc = S.bufs("c_b", 2), S.bufs("c_c", 2), S.bufs("c_z", 2), S.bufs("c_acc", 2)
        banks = [kb.ps(es, f"c_ps{i}", [128, NT], F32) for i in range(8)]
        b_bank = S.bufs("c_bank", 8)
        vec = cm.vec
        cw = VEC["cw"]
        for t in tiles:
            N, ci = t["N"], t["ci"]
            M = N + 2
            S.op(DMAQ, lambda e, t=t, M=M: e.dma_start(out=xt[:, :, :M], in_=chunked(t["src"])), writes=[b_xt], dma_home=b_xt)
            norm_mod(kb, cm, nw, xt, b_xt, M, ci, 0, banks[6], b_bank[6], lambda kc, M=M: h[:, kc, :M], b_h)
            for kc in range(KC):
                sl = kc % 2
                pb, pc, pu = banks[sl * 3], banks[sl * 3 + 1], banks[sl * 3 + 2]
                bb, bc, bu = b_bank[sl * 3], b_bank[sl * 3 + 1], b_bank[sl * 3 + 2]
                for j, (pp, bpp) in enumerate([(pb, bb), (pc, bc), (pu, bu)]):
                    col = j * D + kc * 128
                    for k2 in range(KC):
                        S.op("pe", lambda e, pp=pp, col=col, k2=k2, M=M: e.matmul(pp[:, :M], lhsT=win[:, k2, col:col + 128], rhs=h[:, k2, :M],
                                                                              start=(k2 == 0), stop=(k2 == KC - 1)),
                             reads=[b_win[j], b_h], writes=[bpp])
                S.op("act", lambda e, sl=sl, pb=pb, M=M: e.activation(out=bsb[sl][:, :M], in_=pb[:, :M], func=ACT.Copy), reads=[bb], writes=[b_bsb[sl]])
                S.op("act", lambda e, sl=sl, pc=pc, M=M: e.activation(out=csb[sl][:, :M], in_=pc[:, :M], func=ACT.Copy), reads=[bc], writes=[b_csb[sl]])
                S.op("dve", lambda e, sl=sl, pu=pu, M=M: e.tensor_tensor(out=zsb[sl][:, :M], in0=csb[sl][:, :M], in1=pu[:, :M], op=ALU.mult),
                     reads=[b_csb[sl], bu], writes=[b_zsb[sl]])
                for side, col in (("lo", 0), ("hi", M - 1)):
                    mode = t[side]
                    if mode == "flag":
                        fcol = VEC["vlo"] if side == "lo" else VEC["vhi"]
                        S.op("dve", lambda e, sl=sl, col=col, fcol=fcol: e.tensor_scalar(out=zsb[sl][:, col:col + 1], in0=zsb[sl][:, col:col + 1],
                                                                                      scalar1=vec[:, fcol:fcol + 1], scalar2=None, op0=ALU.mult),
                             reads=[cm.b_vec], writes=[b_zsb[sl]])
                    elif mode == "zero":
                        S.op("dve", lambda e, sl=sl, col=col: e.memset(zsb[sl][:, col:col + 1], 0.0), writes=[b_zsb[sl]])
                w0 = vec[:, cw + kc:cw + kc + 1]
                w1 = vec[:, cw + 8 + kc:cw + 8 + kc + 1]
                w2 = vec[:, cw + 16 + kc:cw + 16 + kc + 1]
                S.op("dve", lambda e, sl=sl, w0=w0, N=N: e.tensor_scalar(out=acc[sl][:, :N], in0=zsb[sl][:, 0:N], scalar1=w0, scalar2=None, op0=ALU.mult),
                     reads=[b_zsb[sl], cm.b_vec], writes=[b_acc[sl]])
                S.op("dve", lambda e, sl=sl, w1=w1, N=N: e.scalar_tensor_tensor(out=acc[sl][:, :N], in0=zsb[sl][:, 1:N + 1], scalar=w1, in1=acc[sl][:, :N],
                                                                            op0=ALU.mult, op1=ALU.add),
                     reads=[b_zsb[sl]], writes=[b_acc[sl]])
                S.op("dve", lambda e, sl=sl, w2=w2, N=N: e.scalar_tensor_tensor(out=acc[sl][:, :N], in0=zsb[sl][:, 2:N + 2], scalar=w2, in1=acc[sl][:, :N],
                                                                            op0=ALU.mult, op1=ALU.add),
                     reads=[b_zsb[sl]], writes=[b_acc[sl]])
                S.op("pool", lambda e, sl=sl, kc=kc, N=N: e.tensor_tensor(out=mb[:, kc, :N], in0=acc[sl][:, :N], in1=bsb[sl][:, 1:N + 1], op=ALU.mult),
                     reads=[b_acc[sl], b_bsb[sl]], writes=[b_m[kc]])
            for oc in range(KC):
                pk = 6 + oc % 2
                for k2 in range(KC):
                    S.op("pe", lambda e, pk=pk, oc=oc, k2=k2, N=N: e.matmul(banks[pk][:, :N], lhsT=wout[:, k2, oc * 128:(oc + 1) * 128], rhs=mb[:, k2, :N],
                                                                        start=(k2 == 0), stop=(k2 == KC - 1)),
                         reads=[b_wout, b_m[k2]], writes=[b_bank[pk]])
                S.op("dve", lambda e, pk=pk, oc=oc, N=N, ci=ci: e.scalar_tensor_tensor(out=xt[:, oc, 1:N + 1], in0=banks[pk][:, :N], scalar=cm.gate(ci, 0, oc),
                                                                                   in1=xt[:, oc, 1:N + 1], op0=ALU.mult, op1=ALU.add),
                     reads=[b_bank[pk], cm.b_mod], writes=[b_xt])
            S.op(DMAQ, lambda e, t=t, N=N: e.dma_start(out=chunked(t["dst"]), in_=xt[:, :, 1:N + 1]), reads=[b_xt], dma_home=b_xt)
    S.barrier()


def conv_tiles(xpad, xm, ci, ntok, lo, hi):
    tiles = []
    t0 = 0
    while t0 < ntok:
        N = min(510, ntok - t0)
        tiles.append(dict(src=xpad[:, t0:t0 + N + 2], dst=xm[:, t0:t0 + N], N=N, ci=ci,
                          lo=(lo if t0 == 0 else None), hi=(hi if t0 + N == ntok else None)))
        t0 += N
    return tiles


def ffn_tiles(src, dst, ci, ntok):
    return [dict(src=src[:, t0:min(t0 + NT, ntok)], dst=dst[:, t0:min(t0 + NT, ntok)], N=min(NT, ntok - t0), ci=ci) for t0 in range(0, ntok, NT)]


def build_layer_B(with_ctx, n_exp=NE):
    global DMAQ
    DMAQ = "sp"
    kb = KB()
    xpad = kb.din("xpad", [D, HALF + 2])
    vecd = kb.din("vec", [128, NVEC])
    ada_w = kb.din("ada_w", [D, 6 * D])
    consts = dict(cbf=kb.din("cbf", [128, 3, 128]), cf=kb.din("cf", [128, 2, 128]))
    cw_in = kb.din("cw_in", [D, 3 * D])
    cw_out = kb.din("cw_out", [D, D])
    router_w = kb.din("router_w", [D, NE])
    kb.din("selm", [NE, NE, 128])
    mwg = kb.din("mwg", [n_exp, D, DFFE])
    mwu = kb.din("mwu", [n_exp, D, DFFE])
    mwd = kb.din("mwd", [n_exp, DFFE, D])
    yo = kb.dout("yo", [D, HALF])
    xm = kb.dscratch("xm", [D, HALF])
    if with_ctx:
        xcpad = kb.din("xcpad", [D, CTX + 2])
        yc = kb.dout("yc", [D, CTX])
        xcm = kb.dscratch("xcm", [D, CTX])
    cm = Common(kb, vecd, ada_w, consts)
    ct = conv_tiles(xpad, xm, 0, HALF, "flag", "flag")
    ft = ffn_tiles(xm, yo, 0, HALF)
    if with_ctx:
        ct += conv_tiles(xcpad, xcm, 1, CTX, "zero", "zero")
        ft = ft[:4] + ffn_tiles(xcm, yc, 1, CTX) + ft[4:]
    conv_phase(kb, cm, ct, cw_in, cw_out)
    experts = [(mwg[e], mwu[e], mwd[e]) for e in range(n_exp)]
    outs = ffn_phase(kb, cm, ft, experts, DFFE, router_w=router_w)
    stats = kb.S.emit(kb.nc, final_wait_ops=outs)
    kb.es.close()
    return kb.nc, stats


def colpack(v):
    return np.ascontiguousarray(v.reshape(-1, 128).T)


def make_consts():
    cbf = np.zeros((128, 3, 128), np.float32)
    cbf[:, 0, :] = 1.0 / D
    for hh in range(2):
        cbf[hh * 64:(hh + 1) * 64, 1, hh * 64:(hh + 1) * 64] = 1.0 / 64
    for i in range(64):
        cbf[2 * i + 1, 2, 2 * i] = -1.0
        cbf[2 * i, 2, 2 * i + 1] = 1.0
    cf = np.zeros((128, 2, 128), np.float32)
    cf[:, 0, :] = np.eye(128, dtype=np.float32)
    cf[:, 1, :] = 1.0
    selm = np.zeros((NE, NE, 128), np.float32)
    for e in range(NE):
        selm[e, e, :] = 1.0
    return cbf, cf, selm


def make_vec(inp, layer, b, half):
    i = layer // 2
    v = np.zeros((128, NVEC), np.float32)
    v[:, VEC["c"]:VEC["c"] + 8] = colpack(inp["c"][b])
    v[:, VEC["cc"]:VEC["cc"] + 8] = colpack(inp["c_ctx"])
    v[:, VEC["n1"]:VEC["n1"] + 8] = colpack(inp["norm1_g"][layer])
    v[:, VEC["n2"]:VEC["n2"] + 8] = colpack(inp["norm2_g"][layer])
    v[:, VEC["adab"]:VEC["adab"] + 48] = colpack(inp["ada_b"][layer])
    if layer % 2 == 0:
        for n, k in (("qna", "qnorm_a"), ("kna", "knorm_a"), ("qnb", "qnorm_b"), ("knb", "knorm_b")):
            v[:, VEC[n]] = np.tile(inp[k][i], 2)
        v[:, VEC["sink"]:VEC["sink"] + 8] = inp["sink_b"][i][None, :]
    else:
        for j in range(3):
            v[:, VEC["cw"] + 8 * j:VEC["cw"] + 8 * j + 8] = colpack(inp["conv_w"][i][j])
    v[:, VEC["vlo"]] = float(half)
    v[:, VEC["vhi"]] = float(1 - half)
    return v


W_QA, W_QB, W_KA, W_KB, W_V = 0, 512, 1024, 1152, 1280
NWIN = 1536
NTL = SEQ // NT
NB = 2 + SEQ // 128


class QKWork:
    def __init__(self, kb, es):
        S = kb.S
        self.sq = kb.sb(es, "qk_sq", [128, NT], BF16)
        self.sd = kb.sb(es, "qk_sd", [128, NT], F32)
        self.r = kb.sb(es, "qk_r", [128, NT], F32)
        self.qn = kb.sb(es, "qk_qn", [128, NT], BF16)
        self.t1 = kb.sb(es, "qk_t1", [128, NT], F32)
        self.t2 = kb.sb(es, "qk_t2", [128, NT], F32)
        self.b = {n: S.buf("qk_" + n) for n in ("sq", "sd", "r", "qn", "t1", "t2")}


def qk_norm_rope(kb, cm, qw, ps, b_ps, N, gcol, cs, b_cs, ms_ps, b_ms, rot_ps, b_rot, out_ap, b_out):
    S = kb.S
    vec = cm.vec
    S.op("act", lambda e: e.activation(out=qw.sq[:, :N], in_=ps[:, :N], func=ACT.Square), reads=[b_ps], writes=[qw.b["sq"]])
    S.op("pe", lambda e: e.matmul(ms_ps[:, :N], lhsT=cm.bones, rhs=qw.sq[:, :N], start=True, stop=True), reads=[qw.b["sq"], cm.b_cst], writes=[b_ms])
    S.op("act", lambda e: e.activation(out=qw.sd[:, :N], in_=ms_ps[:, :N], func=ACT.Sqrt, bias=cm.eps_col, scale=1.0), reads=[b_ms, cm.b_eps], writes=[qw.b["sd"]])
    S.op("dve", lambda e: e.reciprocal(out=qw.r[:, :N], in_=qw.sd[:, :N]), reads=[qw.b["sd"]], writes=[qw.b["r"]])
    if cs is None:
        S.op("dve", lambda e: e.scalar_tensor_tensor(out=out_ap, in0=ps[:, :N], scalar=vec[:, gcol:gcol + 1], in1=qw.r[:, :N], op0=ALU.mult, op1=ALU.mult),
             reads=[b_ps, qw.b["r"], cm.b_vec], writes=[b_out])
        return
    cos, sin = cs
    S.op("dve", lambda e: e.scalar_tensor_tensor(out=qw.qn[:, :N], in0=ps[:, :N], scalar=vec[:, gcol:gcol + 1], in1=qw.r[:, :N], op0=ALU.mult, op1=ALU.mult),
         reads=[b_ps, qw.b["r"], cm.b_vec], writes=[qw.b["qn"]])
    S.op("pe", lambda e: e.matmul(rot_ps[:, :N], lhsT=cm.rotm, rhs=qw.qn[:, :N], start=True, stop=True), reads=[qw.b["qn"], cm.b_cst], writes=[b_rot])
    S.op("dve", lambda e: e.tensor_tensor(out=qw.t1[:, :N], in0=qw.qn[:, :N], in1=cos[:, :N], op=ALU.mult), reads=[qw.b["qn"], b_cs], writes=[qw.b["t1"]])
    S.op("dve", lambda e: e.tensor_tensor(out=qw.t2[:, :N], in0=rot_ps[:, :N], in1=sin[:, :N], op=ALU.mult), reads=[b_rot, b_cs], writes=[qw.b["t2"]])
    S.op("pool", lambda e: e.tensor_tensor(out=out_ap, in0=qw.t1[:, :N], in1=qw.t2[:, :N], op=ALU.add), reads=[qw.b["t1"], qw.b["t2"]], writes=[b_out])


def attn_phase(kb, cm, w_in_d, w_out_d, cosd, sind, bandd, xs, xc, xm, xcm, with_ctx_out):
    nc, S = kb.nc, kb.S
    vec = cm.vec
    with ExitStack() as es:
        KA = kb.sb(es, "KA", [128, NB * 128], BF16)
        KBc = kb.sb(es, "KB", [128, NB * 128], BF16)
        VA = kb.sb(es, "VA", [128, NB, 2, 65], BF16)
        VB = kb.sb(es, "VB", [128, NB, 2, 65], BF16)
        b_KA, b_VA, b_KB, b_VB = S.buf("KA"), S.buf("VA"), S.buf("KB"), S.buf("VB")
        win = kb.sb(es, "a_win", [128, KC, NWIN], BF16)
        wout = kb.sb(es, "a_wout", [128, KC, D], BF16)
        b_win, b_wout = S.buf("a_win"), S.buf("a_wout")
        for j in range(3):
            c0 = j * 512
            S.op("pool", lambda e: e.dma_start(out=win[:, :, c0:c0 + 512], in_=chunked(w_in_d[:, c0:c0 + 512])), writes=[b_win], dma_home=S.buf("a_win_d"))
        S.op("pool", lambda e: e.dma_start(out=wout[:], in_=chunked(w_out_d)), writes=[b_wout], dma_home=b_wout)
        band = kb.sb(es, "a_band", [128, 384], BF16)
        b_band = S.buf("a_band")
        S.op("pool", lambda e: e.dma_start(out=band[:], in_=bandd), writes=[b_band], dma_home=b_band)
        esink = kb.sb(es, "a_esink", [128, 8], F32)
        b_esink = S.buf("a_esink")
        S.op("act", lambda e: e.activation(out=esink[:], in_=vec[:, VEC["sink"]:VEC["sink"] + 8], func=ACT.Exp), reads=[cm.b_vec], writes=[b_esink])
        xt = kb.sb(es, "a_xt", [128, KC, NT], F32)
        b_xt = S.buf("a_xt")
        h = kb.sb(es, "a_h", [128, KC, NT], BF16)
        b_h = S.buf("a_h")
        Q = kb.sb(es, "a_Q", [128, 8, NT], BF16)
        b_Q = S.bufs("a_Q", 8)
        oT = kb.sb(es, "a_oT", [128, 8, NT], BF16)
        b_oT = S.bufs("a_oT", 16)
        nw = NormWork(kb, es, "a_nw")
        qw = QKWork(kb, es)
        cst = kb.sb(es, "a_cos", [128, NT], F32)
        snt = kb.sb(es, "a_sin", [128, NT], F32)
        b_cs = S.buf("a_cs")
        b_cos_d, b_sin_d = S.buf("a_cos_d"), S.buf("a_sin_d")
        psb = [kb.sb(es, f"a_p{i}", [128, NT], BF16) for i in range(3)]
        b_psb = S.bufs("a_p", 3)
        rec = kb.sb(es, "a_rec", [128, NT], F32)
        bcs = kb.sb(es, "a_bc", [64, NT], F32)
        b_rec, b_bcs = S.buf("a_rec"), S.buf("a_bcs")
        banks = [kb.ps(es, f"a_ps{i}", [128, NT], F32) for i in range(8)]
        b_bank = S.bufs("a_bank", 8)

        S.op("dve", lambda e: e.memset(VA[:, :, :, 64:65], 1.0), writes=[b_VA])
        S.op("dve", lambda e: e.memset(VB[:, :, :, 64:65], 1.0), writes=[b_VB])

        def load_tile(src, N, ci, pos0):
            S.op(DMAQ, lambda e: e.dma_start(out=xt[:, :, :N], in_=chunked(src)), writes=[b_xt], dma_home=b_xt)
            if pos0 is not None:
                S.op(DMAQ, lambda e: e.dma_start(out=cst[:, :N], in_=cosd[:, pos0:pos0 + N]), writes=[b_cs], dma_home=b_cos_d)
                S.op(DMAQ, lambda e: e.dma_start(out=snt[:, :N], in_=sind[:, pos0:pos0 + N]), writes=[b_cs], dma_home=b_sin_d)
            norm_mod(kb, cm, nw, xt, b_xt, N, ci, 0, banks[5], b_bank[5], lambda kc: h[:, kc, :N], b_h)

        def project(col, N):
            for kc in range(KC):
                S.op("pe", lambda e: e.matmul(banks[5][:, :N], lhsT=win[:, kc, col:col + 128], rhs=h[:, kc, :N], start=(kc == 0), stop=(kc == KC - 1)),
                     reads=[b_win, b_h], writes=[b_bank[5]])

        def kv_build(src, N, ci, pos0, blk0):
            load_tile(src, N, ci, pos0)
            cs = None if pos0 is None else (cst, snt)
            project(W_KA, N)
            qk_norm_rope(kb, cm, qw, banks[5], b_bank[5], N, VEC["kna"], cs, b_cs, banks[6], b_bank[6], banks[7], b_bank[7],
                         KA[:, blk0 * 128:blk0 * 128 + N], b_KA)
            project(W_KB, N)
            qk_norm_rope(kb, cm, qw, banks[5], b_bank[5], N, VEC["knb"], cs, b_cs, banks[6], b_bank[6], banks[7], b_bank[7],
                         KBc[:, blk0 * 128:blk0 * 128 + N], b_KB)
            for tb in range(N // 128):
                vp = banks[4]
                for kc in range(KC):
                    S.op("pe", lambda e: e.matmul(vp[:, 0:256], lhsT=h[:, kc, tb * 128:(tb + 1) * 128], rhs=win[:, kc, W_V:W_V + 256], start=(kc == 0), stop=(kc == KC - 1)),
                         reads=[b_h, b_win], writes=[b_bank[4]])
                S.op("dve", lambda e: e.tensor_copy(out=VA[:, blk0 + tb, :, 0:64], in_=vp[:, 0:128].rearrange("p (a b) -> p a b", a=2)),
                     reads=[b_bank[4]], writes=[b_VA])
                S.op("dve", lambda e: e.tensor_copy(out=VB[:, blk0 + tb, :, 0:64], in_=vp[:, 128:256].rearrange("p (a b) -> p a b", a=2)),
                     reads=[b_bank[4]], writes=[b_VB])

        kv_build(xc, CTX, 1, None, 0)
        for it in range(NTL):
            kv_build(xs[:, it * NT:(it + 1) * NT], NT, 0, it * NT, 2 + 4 * it)

        def finalize(o_ps, b_o, N, chunk, half, sink_h):
            if sink_h is not None:
                S.op("dve", lambda e: e.tensor_scalar(out=rec[64:65, :N], in0=o_ps[64:65, :N], scalar1=esink[64:65, sink_h:sink_h + 1], scalar2=None, op0=ALU.add),
                     reads=[b_o, b_esink], writes=[b_rec])
                S.op("dve", lambda e: e.reciprocal(out=rec[64:65, :N], in_=rec[64:65, :N]), reads=[b_rec], writes=[b_rec])
            else:
                S.op("dve", lambda e: e.reciprocal(out=rec[64:65, :N], in_=o_ps[64:65, :N]), reads=[b_o], writes=[b_rec])
            S.op("pe", lambda e: e.matmul(banks[7][0:64, :N], lhsT=cm.cst_f[64:65, 1, 0:64], rhs=rec[64:65, :N], start=True, stop=True),
                 reads=[b_rec, cm.b_cstf], writes=[b_bank[7]])
            S.op("dve", lambda e: e.tensor_copy(out=bcs[:, :N], in_=banks[7][0:64, :N]), reads=[b_bank[7]], writes=[b_bcs])
            p0 = half * 64
            S.op("dve", lambda e: e.tensor_tensor(out=oT[p0:p0 + 64, chunk, :N], in0=o_ps[0:64, :N], in1=bcs[:, :N], op=ALU.mult),
                 reads=[b_o, b_bcs], writes=[b_oT[2 * chunk + half]])

        scnt = [0]
        PIPE = 2

        def run_steps(steps):
            n = len(steps)
            sis = []
            for i in range(n + PIPE):
                if i < n:
                    st = steps[i]
                    si = scnt[0] % 3
                    scnt[0] += 1
                    sis.append(si)
                    kv, qc, blk, q0, q1, m0 = st["kv"], st["qc"], st["blk"], st["q0"], st["q1"], st["m0"]
                    Kc = st["Kc"]
                    S.op("pe", lambda e: e.matmul(banks[si][:, q0:q1], lhsT=Kc[kv * 64:(kv + 1) * 64, blk * 128:(blk + 1) * 128],
                                                  rhs=Q[kv * 64:(kv + 1) * 64, qc, q0:q1], start=True, stop=True),
                         reads=[st["b_K"], b_Q[qc]], writes=[b_bank[si]])
                    S.op("act", lambda e: e.activation(out=psb[si][:, q0:q1], in_=banks[si][:, q0:q1], func=ACT.Exp, scale=SCALE),
                         reads=[b_bank[si]], writes=[b_psb[si]])
                    if m0 is not None:
                        S.op("dve", lambda e: e.tensor_tensor(out=psb[si][:, q0:q1], in0=psb[si][:, q0:q1], in1=band[:, m0:m0 + (q1 - q0)], op=ALU.mult),
                             reads=[b_band], writes=[b_psb[si]])
                j = i - PIPE
                if j >= 0:
                    st = steps[j]
                    si = sis[j]
                    kv, blk, q0, q1 = st["kv"], st["blk"], st["q0"], st["q1"]
                    Vc, o_ps = st["Vc"], st["o_ps"]
                    S.op("pe", lambda e: e.matmul(o_ps[0:65, q0:q1], lhsT=Vc[:, blk, kv, 0:65], rhs=psb[si][:, q0:q1], start=st["start"], stop=st["stop"],
                                                  skip_group_check=True),
                         reads=[st["b_V"], b_psb[si]], writes=[st["b_o"]])
                    if st["fin"] is not None:
                        st["fin"]()

        def q_tile(src, dst, N, ci, pos0, it):
            load_tile(src, N, ci, pos0)
            cs = None if pos0 is None else (cst, snt)
            for c in range(8):
                project((W_QA if c < 4 else W_QB) + (c % 4) * 128, N)
                qk_norm_rope(kb, cm, qw, banks[5], b_bank[5], N, VEC["qna"] if c < 4 else VEC["qnb"], cs, b_cs, banks[6], b_bank[6], banks[7], b_bank[7],
                             Q[:, c, :N], b_Q[c])
            hcnt = 0
            steps = []
            for grp in range(2):
                for hd in range(8):
                    kv, c = hd // 4, hd % 4
                    ob = 3 + hcnt % 2
                    hcnt += 1
                    if grp == 0:
                        blocks = [(0, 0, N, None), (1, 0, N, None)] if it is None else [(b, 0, N, None) for b in range(NB)]
                        Kc, b_K, Vc, b_V, qc = KA, b_KA, VA, b_VA, c
                        fin = (lambda ob=ob, c=c, kv=kv: finalize(banks[ob], b_bank[ob], N, c, kv, None))
                    else:
                        blocks = [(0, 0, N, None), (1, 0, N, None)]
                        if it is not None:
                            for j in range(6):
                                lb = it * 4 - 1 + j
                                if lb < 0 or lb >= SEQ // 128:
                                    continue
                                q0 = max(0, 128 * (j - 2))
                                q1 = min(512, 128 * (j - 2) + 384)
                                blocks.append((2 + lb, q0, q1, q0 - 128 * (j - 2)))
                        Kc, b_K, Vc, b_V, qc = KBc, b_KB, VB, b_VB, 4 + c
                        fin = (lambda ob=ob, c=c, kv=kv, hd=hd: finalize(banks[ob], b_bank[ob], N, 4 + c, kv, hd))
                    nb = len(blocks)
                    for bi, (blk, q0, q1, m0) in enumerate(blocks):
                        steps.append(dict(Kc=Kc, b_K=b_K, Vc=Vc, b_V=b_V, kv=kv, qc=qc, blk=blk, q0=q0, q1=q1, m0=m0, o_ps=banks[ob], b_o=b_bank[ob],
                                          start=(bi == 0), stop=(bi == nb - 1), fin=(fin if bi == nb - 1 else None)))
            run_steps(steps)
            for oc in range(KC):
                pk = 5 + oc % 2
                for c in range(8):
                    S.op("pe", lambda e: e.matmul(banks[pk][:, :N], lhsT=wout[:, c, oc * 128:(oc + 1) * 128], rhs=oT[:, c, :N], start=(c == 0), stop=(c == 7)),
                         reads=[b_wout, b_oT[2 * c], b_oT[2 * c + 1]], writes=[b_bank[pk]])
                S.op("dve", lambda e: e.scalar_tensor_tensor(out=xt[:, oc, :N], in0=banks[pk][:, :N], scalar=cm.gate(ci, 0, oc), in1=xt[:, oc, :N], op0=ALU.mult, op1=ALU.add),
                     reads=[b_bank[pk], cm.b_mod], writes=[b_xt])
            S.op(DMAQ, lambda e: e.dma_start(out=chunked(dst), in_=xt[:, :, :N]), reads=[b_xt], dma_home=b_xt)

        if with_ctx_out:
            q_tile(xc, xcm, CTX, 1, None, None)
        for it in range(NTL):
            q_tile(xs[:, it * NT:(it + 1) * NT], xm[:, it * NT:(it + 1) * NT], NT, 0, it * NT, it)
    S.barrier()


def with_ctx_tiles(ft, fc):
    return ft[:4] + fc + ft[4:]


def build_fused(n_layers=4, n_exp=NE):
    global DMAQ
    DMAQ = "sp"
    kb = KB()
    x0 = kb.din("x0", [D, SEQ])
    xc0 = kb.din("xc0", [D, CTX])
    vecd = kb.din("vec", [4, 128, NVEC])
    ada_w = kb.din("ada_w", [4, D, 6 * D])
    consts = dict(cbf=kb.din("cbf", [128, 3, 128]), cf=kb.din("cf", [128, 2, 128]))
    kb.din("selm", [NE, NE, 128])
    w_in = kb.din("w_in", [2, D, NWIN])
    w_out = kb.din("w_out", [2, D, D])
    cosd = kb.din("rcos", [128, SEQ])
    sind = kb.din("rsin", [128, SEQ])
    bandd = kb.din("bandm", [128, 384])
    fwg = kb.din("fwg", [2, D, DFF])
    fwu = kb.din("fwu", [2, D, DFF])
    fwd = kb.din("fwd", [2, DFF, D])
    cw_in = kb.din("cw_in", [2, D, 3 * D])
    cw_out = kb.din("cw_out", [2, D, D])
    router_w = kb.din("router_w", [2, D, NE])
    mwg = kb.din("mwg", [2, n_exp, D, DFFE])
    mwu = kb.din("mwu", [2, n_exp, D, DFFE])
    mwd = kb.din("mwd", [2, n_exp, DFFE, D])
    yo = kb.dout("yo", [D, SEQ])
    xm = kb.dscratch("xm", [D, SEQ])
    xcm = kb.dscratch("xcm", [D, CTX])
    xp1 = kb.dscratch("xp1", [D, SEQ + 2])
    xcp1 = kb.dscratch("xcp1", [D, CTX + 2])
    x2 = kb.dscratch("x2", [D, SEQ])
    xc2 = kb.dscratch("xc2", [D, CTX])
    xp3 = kb.dscratch("xp3", [D, SEQ + 2])
    outs = []
    zt = kb.sb(kb.es, "zero_col", [128, KC, 1], F32)
    b_zt = kb.S.buf("zero_col")
    kb.S.op("dve", lambda e: e.memset(zt[:], 0.0), writes=[b_zt])
    for buf, n in ((xp1, SEQ), (xcp1, CTX), (xp3, SEQ)):
        for col in (0, n + 1):
            kb.S.op(DMAQ, lambda e: e.dma_start(out=chunked(buf[:, col:col + 1]), in_=zt[:], allow_slow_non_contiguous=True), reads=[b_zt], dma_home=kb.S.buf("zc_d"))
    for layer in range(n_layers):
        i = layer // 2
        with ExitStack() as es:
            cm = Common(kb, vecd[layer], ada_w[layer], consts, es=es)
            if layer == 0:
                attn_phase(kb, cm, w_in[0], w_out[0], cosd, sind, bandd, x0, xc0, xm, xcm, True)
                ft = with_ctx_tiles(ffn_tiles(xm, xp1[:, 1:SEQ + 1], 0, SEQ), ffn_tiles(xcm, xcp1[:, 1:CTX + 1], 1, CTX))
                outs = ffn_phase(kb, cm, ft, [(fwg[0], fwu[0], fwd[0])], DFF)
            elif layer == 1:
                ct = conv_tiles(xp1, xm, 0, SEQ, "zero", "zero") + conv_tiles(xcp1, xcm, 1, CTX, "zero", "zero")
                conv_phase(kb, cm, ct, cw_in[0], cw_out[0])
                ft = with_ctx_tiles(ffn_tiles(xm, x2, 0, SEQ), ffn_tiles(xcm, xc2, 1, CTX))
                outs = ffn_phase(kb, cm, ft, [(mwg[0, e], mwu[0, e], mwd[0, e]) for e in range(n_exp)], DFFE, router_w=router_w[0])
            elif layer == 2:
                attn_phase(kb, cm, w_in[1], w_out[1], cosd, sind, bandd, x2, xc2, xm, None, False)
                outs = ffn_phase(kb, cm, ffn_tiles(xm, xp3[:, 1:SEQ + 1], 0, SEQ), [(fwg[1], fwu[1], fwd[1])], DFF)
            else:
                conv_phase(kb, cm, conv_tiles(xp3, xm, 0, SEQ, "zero", "zero"), cw_in[1], cw_out[1])
                outs = ffn_phase(kb, cm, ffn_tiles(xm, yo, 0, SEQ), [(mwg[1, e], mwu[1, e], mwd[1, e]) for e in range(n_exp)], DFFE, router_w=router_w[1])
    if n_layers < 4:
        pass
    stats = kb.S.emit(kb.nc, final_wait_ops=outs)
    kb.es.close()
    return kb.nc, stats


def rope_tables():
    half = 32
    inv = (10000.0 ** (-np.arange(0, half, 2, dtype=np.float32) / half)).astype(np.float32)
    pos = np.arange(SEQ)
    row = (pos // 64).astype(np.float32)
    col = (pos % 64).astype(np.float32)
    ang = np.concatenate([row[:, None] * inv[None, :], col[:, None] * inv[None, :]], axis=-1)
    cos = np.cos(ang).astype(np.float32)
    sin = np.sin(ang).astype(np.float32)
    pidx = (np.arange(128) % 64) // 2
    return np.ascontiguousarray(cos[:, pidx].T), np.ascontiguousarray(sin[:, pidx].T)


def band_mask():
    kk = np.arange(128)[:, None]
    u = np.arange(384)[None, :] - 128
    return ((u >= kk - 128) & (u <= kk + 128)).astype(np.float32)


def perm_w_in(w):
    cols = []
    for base in (0, 768):
        for c in range(4):
            cols += [w[:, base + c * 64:base + (c + 1) * 64], w[:, base + (4 + c) * 64:base + (5 + c) * 64]]
    cols += [w[:, 512:640], w[:, 1280:1408], w[:, 640:768], w[:, 1408:1536]]
    return np.ascontiguousarray(np.concatenate(cols, axis=1))


def perm_w_out(w):
    rows = []
    for base in (0, 512):
        for c in range(4):
            rows += [w[base + c * 64:base + (c + 1) * 64], w[base + (4 + c) * 64:base + (5 + c) * 64]]
    return np.ascontiguousarray(np.concatenate(rows, axis=0))


_NC = {}


def make_inputs(inp, b):
    cbf, cf, selm = make_consts()
    cos, sin = rope_tables()
    return dict(
        x0=np.ascontiguousarray(inp["x"][b].T), xc0=np.ascontiguousarray(inp["ctx"][b].T),
        vec=np.stack([make_vec(inp, l, b, 0) for l in range(4)]), ada_w=inp["ada_w"], cbf=cbf, cf=cf, selm=selm,
        w_in=np.stack([perm_w_in(inp["attn_w_in"][i]) for i in range(2)]), w_out=np.stack([perm_w_out(inp["attn_w_out"][i]) for i in range(2)]),
        rcos=cos, rsin=sin, bandm=band_mask(), fwg=inp["ffn_w_gate"], fwu=inp["ffn_w_up"], fwd=inp["ffn_w_down"],
        cw_in=inp["conv_w_in"], cw_out=inp["conv_w_out"], router_w=inp["router_w"],
        mwg=inp["moe_w_gate"], mwu=inp["moe_w_up"], mwd=inp["moe_w_down"])


def kernel(**inp):
    inp = {k: np.asarray(v) for k, v in inp.items()}
    B = inp["x"].shape[0]
    if "nc" not in _NC:
        _NC["nc"] = build_fused()[0]
    per_b = [make_inputs(inp, b) for b in range(B)]
    ins = [per_b[c // 2] for c in range(8)]
    res = run_bass_kernel_spmd(_NC["nc"], ins, core_ids=list(range(8)))
    return np.stack([np.ascontiguousarray(res.results[2 * b]["yo"].T) for b in range(B)]).astype(np.float32)
```

```python
import os
import numpy as np
from contextlib import ExitStack
import concourse.bass as bass
import concourse.mybir as mybir
from concourse.bass_utils import run_bass_kernel_spmd

F32 = mybir.dt.float32
BF16 = mybir.dt.bfloat16
ACT = mybir.ActivationFunctionType
ALU = mybir.AluOpType
AX = mybir.AxisListType

D = 1024
KC = 8
SEQ = 8192
HALF = 4096
CTX = 256
NT = 512
DFF = 2816
DFFE = 3584
NE = 8
EPS = 1e-6
SCALE = 0.125
ENG = ("pe", "act", "dve", "pool", "sp")
DMAQ = "pool"


class Buf:
    __slots__ = ("name", "last_w", "readers", "dma_sem_idx")

    def __init__(self, name):
        self.name = name
        self.last_w = None
        self.readers = []
        self.dma_sem_idx = None


class Op:
    __slots__ = ("eng", "fn", "deps", "is_dma", "sem", "signal", "count", "idx")


class _Rec:
    def __init__(self):
        self.call = None

    def __getattr__(self, name):
        def f(*a, **k):
            self.call = (name, a, k)
            return None
        return f


class Sched:
    def __init__(self, same_engine_sync=True):
        self.ops = []
        self.same_engine_sync = same_engine_sync
        self.n_dma_sems = 0
        self.last_eng = {}
        self.dma_since_bar = []

    def buf(self, name):
        return Buf(name)

    def bufs(self, name, n):
        return [Buf(f"{name}{i}") for i in range(n)]

    def op(self, eng, fn, reads=(), writes=(), dma_home=None, extra_deps=()):
        o = Op()
        o.eng = eng
        rec = _Rec()
        fn(rec)
        o.fn = rec.call
        assert o.fn is not None
        o.is_dma = dma_home is not None
        o.idx = len(self.ops)
        o.signal = False
        o.count = None
        deps = set(extra_deps)
        for b in reads:
            if b.last_w is not None:
                deps.add(b.last_w)
        for b in writes:
            if b.last_w is not None:
                deps.add(b.last_w)
            for r in b.readers:
                deps.add(r)
        fdeps = []
        for d in deps:
            dop = self.ops[d]
            if (not dop.is_dma) and (not o.is_dma) and dop.eng == eng:
                if eng == "pe" or not self.same_engine_sync:
                    continue
            fdeps.append(d)
        o.deps = fdeps
        if o.is_dma:
            if dma_home.dma_sem_idx is None:
                dma_home.dma_sem_idx = self.n_dma_sems
                self.n_dma_sems += 1
            o.sem = ("dma", dma_home.dma_sem_idx)
            self.dma_since_bar.append(o.idx)
        else:
            o.sem = ("eng", eng)
            self.last_eng[eng] = o.idx
        for b in reads:
            b.readers.append(o.idx)
        for b in writes:
            b.last_w = o.idx
            b.readers = []
        self.ops.append(o)
        return o

    def barrier(self):
        deps = list(self.last_eng.values()) + list(self.dma_since_bar)
        self.dma_since_bar = []
        for e in ENG:
            o = Op()
            o.eng = e
            o.fn = None
            o.is_dma = False
            o.idx = len(self.ops)
            o.signal = False
            o.count = None
            o.sem = ("eng", e)
            o.deps = [d for d in deps if not (self.ops[d].eng == e and not self.ops[d].is_dma and e == "pe")]
            self.ops.append(o)

    def emit(self, nc, final_wait_ops=()):
        ops = self.ops
        for o in ops:
            for d in o.deps:
                ops[d].signal = True
        for o in final_wait_ops:
            o.signal = True
        counters = {}
        for o in ops:
            if o.fn is None:
                o.signal = False
                continue
            if o.signal or o.is_dma:
                inc = 16 if o.is_dma else 1
                counters[o.sem] = counters.get(o.sem, 0) + inc
                o.count = counters[o.sem]
                o.signal = True
        with ExitStack() as es:
            sems = {}
            for key in counters:
                sems[key] = es.enter_context(nc.semaphore(f"s_{key[0]}_{key[1]}"))
            block = es.enter_context(nc.Block())
            per_eng = {e: [o for o in ops if o.eng == e] for e in ENG}
            n_waits = [0]

            def make(engname):
                def body(engobj):
                    waited = {}
                    for o in per_eng[engname]:
                        need = {}
                        for d in o.deps:
                            dop = ops[d]
                            if dop.count is None:
                                continue
                            if dop.count > need.get(dop.sem, 0):
                                need[dop.sem] = dop.count
                        for sk, v in need.items():
                            if waited.get(sk, 0) >= v:
                                continue
                            engobj.wait_ge(sems[sk], v)
                            n_waits[0] += 1
                            waited[sk] = v
                        if o.fn is None:
                            continue
                        ins = getattr(engobj, o.fn[0])(*o.fn[1], **o.fn[2])
                        if o.signal:
                            ins.then_inc(sems[o.sem], 16 if o.is_dma else 1)
                    if engname == "sp":
                        for fo in final_wait_ops:
                            engobj.wait_ge(sems[fo.sem], fo.count)
                return body

            block.tensor(make("pe"))
            block.scalar(make("act"))
            block.vector(make("dve"))
            block.gpsimd(make("pool"))
            block.sync(make("sp"))
        self.stats = dict(n_ops=len(ops), n_sems=len(counters), n_waits=n_waits[0],
                          per_eng={e: len(per_eng[e]) for e in ENG})
        return self.stats


VEC = {}
_c = 0
for _n, _w in [("c", 8), ("cc", 8), ("n1", 8), ("n2", 8), ("adab", 48), ("qna", 1), ("kna", 1),
               ("qnb", 1), ("knb", 1), ("sink", 8), ("vlo", 1), ("vhi", 1), ("cw", 24)]:
    VEC[_n] = _c
    _c += _w
NVEC = _c


class KB:
    def __init__(self):
        self.nc = bass.Bass("TRN2", target_bir_lowering=False)
        self.S = Sched()
        self.es = ExitStack()
        self.dram_in = {}
        self.uid = 0
        self.dump = None

    def din(self, name, shape, dt=F32):
        t = self.nc.dram_tensor(name, list(shape), dt, kind="ExternalInput").ap()
        self.dram_in[name] = t
        return t

    def dout(self, name, shape, dt=F32):
        return self.nc.dram_tensor(name, list(shape), dt, kind="ExternalOutput").ap()

    def dscratch(self, name, shape, dt=F32):
        return self.nc.dram_tensor(name, list(shape), dt).ap()

    def sb(self, es, name, shape, dt):
        self.uid += 1
        return es.enter_context(self.nc.sbuf_tensor(f"sb{self.uid}_{name}", list(shape), dt))

    def ps(self, es, name, shape, dt=F32):
        self.uid += 1
        return es.enter_context(self.nc.psum_tensor(f"ps{self.uid}_{name}", list(shape), dt))

    def name(self, p):
        self.uid += 1
        return f"{p}{self.uid}"


def chunked(ap):
    return ap.rearrange("(k p) n -> p k n", p=128)


class Common:
    def __init__(self, kb, layer_vec, ada_w, consts, es=None):
        self.kb = kb
        nc, S = kb.nc, kb.S
        es = es if es is not None else kb.es
        self.vec = kb.sb(es, "vec", [128, NVEC], F32)
        self.b_vec = S.buf("vec")
        S.op(DMAQ, lambda e: e.dma_start(out=self.vec[:], in_=layer_vec), writes=[self.b_vec], dma_home=self.b_vec)
        self.cst_bf = kb.sb(es, "cst_bf", [128, 3, 128], BF16)
        self.b_cst = S.buf("cst")
        S.op("pool", lambda e: e.dma_start(out=self.cst_bf[:], in_=consts["cbf"]), writes=[self.b_cst], dma_home=self.b_cst)
        self.cst_f = kb.sb(es, "cst_f", [128, 2, 128], F32)
        self.b_cstf = S.buf("cstf")
        S.op(DMAQ, lambda e: e.dma_start(out=self.cst_f[:], in_=consts["cf"]), writes=[self.b_cstf], dma_home=self.b_cstf)
        self.onesm = self.cst_bf[:, 0, :]
        self.bones = self.cst_bf[:, 1, :]
        self.rotm = self.cst_bf[:, 2, :]
        self.ident = self.cst_f[:, 0, :]
        self.ones_f = self.cst_f[:, 1, :]
        self.eps_t = kb.sb(es, "eps_t", [128, 1], F32)
        self.b_eps = S.buf("eps")
        S.op("dve", lambda e: e.memset(self.eps_t[:], EPS), writes=[self.b_eps])
        self.eps_col = self.eps_t[:, 0:1]
        self.mod = kb.sb(es, "mod", [128, 2, 48], F32)
        self.geff = kb.sb(es, "geff", [128, 2, 2, 8], F32)
        self.b_mod = S.buf("mod")
        self._build_mod(ada_w)

    def _build_mod(self, ada_w):
        kb = self.kb
        nc, S = kb.nc, kb.S
        with ExitStack() as es:
            sc = kb.sb(es, "silu_c", [128, 8, 2], BF16)
            sc32 = kb.sb(es, "silu_c32", [128, 2, 8], F32)
            b_sc = S.buf("silu_c")
            wsl = [kb.sb(es, f"adaw{i}", [128, 8, 1024], BF16) for i in range(2)]
            b_w = S.bufs("adaw", 2)
            mps = kb.ps(es, "mod_ps", [128, 48, 2], F32)
            b_mps = S.buf("mod_ps")
            vec = self.vec
            S.op("act", lambda e: e.activation(out=sc32[:, 0, :], in_=vec[:, VEC["c"]:VEC["c"] + 8], func=ACT.Silu),
                 reads=[self.b_vec], writes=[b_sc])
            S.op("act", lambda e: e.activation(out=sc32[:, 1, :], in_=vec[:, VEC["cc"]:VEC["cc"] + 8], func=ACT.Silu),
                 reads=[self.b_vec], writes=[b_sc])
            for ci in range(2):
                S.op("dve", lambda e, ci=ci: e.tensor_copy(out=sc[:, :, ci], in_=sc32[:, ci, :]), reads=[b_sc], writes=[b_sc])
            for j in range(6):
                sl = j % 2
                S.op("pool", lambda e, j=j, sl=sl: e.dma_start(out=wsl[sl][:], in_=chunked(ada_w[:, j * 1024:(j + 1) * 1024])),
                     writes=[b_w[sl]], dma_home=b_w[sl])
                for oc in range(8):
                    for kc in range(8):
                        S.op("pe", lambda e, j=j, sl=sl, oc=oc, kc=kc: e.matmul(
                            mps[:, j * 8 + oc, :], lhsT=wsl[sl][:, kc, oc * 128:(oc + 1) * 128], rhs=sc[:, kc, :],
                            start=(kc == 0), stop=(kc == 7)), reads=[b_w[sl], b_sc], writes=[b_mps])
            ab = VEC["adab"]
            for ci in range(2):
                S.op("dve", lambda e, ci=ci: e.tensor_tensor(out=self.mod[:, ci, :], in0=mps[:, :, ci], in1=vec[:, ab:ab + 48], op=ALU.add),
                     reads=[b_mps, self.b_vec], writes=[self.b_mod])
            for ci in range(2):
                for ni in range(2):
                    gcol = VEC["n1"] if ni == 0 else VEC["n2"]
                    scj = 1 if ni == 0 else 4
                    S.op("dve", lambda e, ci=ci, ni=ni, gcol=gcol, scj=scj: e.scalar_tensor_tensor(
                        out=self.geff[:, ci, ni, :], in0=self.mod[:, ci, scj * 8:scj * 8 + 8], scalar=1.0,
                        in1=vec[:, gcol:gcol + 8], op0=ALU.add, op1=ALU.mult),
                        reads=[self.b_mod, self.b_vec], writes=[self.b_mod])
        S.barrier()

    def sh(self, ci, ni, kc):
        j = 0 if ni == 0 else 3
        return self.mod[:, ci, j * 8 + kc:j * 8 + kc + 1]

    def gate(self, ci, ni, kc):
        j = 2 if ni == 0 else 5
        return self.mod[:, ci, j * 8 + kc:j * 8 + kc + 1]

    def ge(self, ci, ni, kc):
        return self.geff[:, ci, ni, kc:kc + 1]


class NormWork:
    def __init__(self, kb, es, tag):
        S = kb.S
        self.sq = [kb.sb(es, f"{tag}_sq{i}", [128, NT], BF16) for i in range(2)]
        self.b_sq = S.bufs(f"{tag}_sq", 2)
        self.sd = kb.sb(es, f"{tag}_sd", [128, NT], F32)
        self.rstd = kb.sb(es, f"{tag}_rstd", [128, NT], F32)
        self.b_sd = S.buf(f"{tag}_sd")
        self.b_rstd = S.buf(f"{tag}_rstd")
        self.tmp = [kb.sb(es, f"{tag}_tmp{i}", [128, NT], F32) for i in range(2)]
        self.b_tmp = S.bufs(f"{tag}_tmp", 2)


def norm_mod(kb, cm, nw, xt, b_xt, N, ci, ni, ms_ps, b_ms, h_out, b_h, inplace=False):
    S = kb.S
    for kc in range(KC):
        sl = kc % 2
        S.op("act", lambda e, kc=kc, sl=sl: e.activation(out=nw.sq[sl][:, :N], in_=xt[:, kc, :N], func=ACT.Square),
             reads=[b_xt], writes=[nw.b_sq[sl]])
        S.op("pe", lambda e, kc=kc, sl=sl: e.matmul(ms_ps[:, :N], lhsT=cm.onesm, rhs=nw.sq[sl][:, :N], start=(kc == 0), stop=(kc == KC - 1)),
             reads=[nw.b_sq[sl], cm.b_cst], writes=[b_ms])
    S.op("act", lambda e: e.activation(out=nw.sd[:, :N], in_=ms_ps[:, :N], func=ACT.Sqrt, bias=cm.eps_col, scale=1.0),
         reads=[b_ms, cm.b_eps], writes=[nw.b_sd])
    S.op("dve", lambda e: e.reciprocal(out=nw.rstd[:, :N], in_=nw.sd[:, :N]), reads=[nw.b_sd], writes=[nw.b_rstd])
    for kc in range(KC):
        sl = kc % 2
        if inplace:
            S.op("dve", lambda e, kc=kc: e.scalar_tensor_tensor(out=xt[:, kc, :N], in0=xt[:, kc, :N], scalar=cm.ge(ci, ni, kc),
                                                                 in1=nw.rstd[:, :N], op0=ALU.mult, op1=ALU.mult),
                 reads=[nw.b_rstd, cm.b_mod], writes=[b_xt])
            S.op("act", lambda e, kc=kc: e.activation(out=xt[:, kc, :N], in_=xt[:, kc, :N], func=ACT.Identity, bias=cm.sh(ci, ni, kc), scale=1.0),
                 reads=[cm.b_mod], writes=[b_xt])
            S.op("pool", lambda e, kc=kc: e.tensor_copy(out=h_out(kc), in_=xt[:, kc, :N]), reads=[b_xt], writes=[b_h])
        else:
            S.op("dve", lambda e, kc=kc, sl=sl: e.scalar_tensor_tensor(out=nw.tmp[sl][:, :N], in0=xt[:, kc, :N], scalar=cm.ge(ci, ni, kc),
                                                                        in1=nw.rstd[:, :N], op0=ALU.mult, op1=ALU.mult),
                 reads=[b_xt, nw.b_rstd, cm.b_mod], writes=[nw.b_tmp[sl]])
            S.op("act", lambda e, kc=kc, sl=sl: e.activation(out=h_out(kc), in_=nw.tmp[sl][:, :N], func=ACT.Identity, bias=cm.sh(ci, ni, kc), scale=1.0),
                 reads=[nw.b_tmp[sl], cm.b_mod], writes=[b_h])


def ffn_phase(kb, cm, tiles, experts, dff, router_w=None):
    nc, S = kb.nc, kb.S
    moe = router_w is not None
    FS = 256
    nsl = dff // FS
    groups = []
    cur, tot = [], 0
    for t in tiles:
        if tot + t["N"] > 2304:
            groups.append(cur)
            cur, tot = [], 0
        cur.append(t)
        tot += t["N"]
    if cur:
        groups.append(cur)
    TMAX = max(sum(t["N"] for t in g) for g in groups)
    out_ops = []
    with ExitStack() as es:
        h2 = kb.sb(es, "f_h2", [128, KC, TMAX], BF16)
        yacc = kb.sb(es, "f_yacc", [128, KC, TMAX], F32)
        wgu = [kb.sb(es, f"f_wgu{i}", [128, 2, KC, FS], BF16) for i in range(2)]
        wdn = [kb.sb(es, f"f_wdn{i}", [128, FS // 128, D], BF16) for i in range(2)]
        b_wg = S.bufs("f_wg", 2)
        b_wu = S.bufs("f_wu", 2)
        b_wd = S.bufs("f_wd", 2)
        xt = kb.sb(es, "f_xt", [128, KC, NT], F32)
        b_xt = S.buf("f_xt")
        nw = NormWork(kb, es, "f_nw")
        sg = [kb.sb(es, f"f_sg{i}", [128, NT], F32) for i in range(2)]
        b_sg = S.bufs("f_sg", 2)
        tt = [kb.sb(es, f"f_tt{i}", [128, NT], F32) for i in range(2)]
        b_tt = S.bufs("f_tt", 2)
        abuf = [kb.sb(es, f"f_a{i}", [128, 2, NT], BF16) for i in range(2)]
        b_a = [S.bufs(f"f_a{i}_", 2) for i in range(2)]
        banks = [kb.ps(es, f"f_ps{i}", [128, NT], F32) for i in range(8)]
        b_bank = S.bufs("f_bank", 8)
        if moe:
            rw = kb.sb(es, "f_rw", [128, KC, NE], F32)
            b_rw = S.buf("f_rw")
            S.op(DMAQ, lambda e: e.dma_start(out=rw[:], in_=chunked(router_w)), writes=[b_rw], dma_home=b_rw)
            gT = kb.sb(es, "f_gT", [NE, TMAX], F32)
            Gb = kb.sb(es, "f_Gb", [128, TMAX], F32)
            sel = kb.sb(es, "f_sel", [NE, NE, 128], F32)
            b_sel = S.buf("f_sel")
            S.op(DMAQ, lambda e: e.dma_start(out=sel[:], in_=kb.dram_in["selm"]), writes=[b_sel], dma_home=b_sel)
            rt = {n: kb.sb(es, f"f_rt_{n}", [128, w], F32) for n, w in
                  [("lg", 8), ("m1", 1), ("eq", 8), ("lg2", 8), ("m2", 1), ("sel", 8), ("nm1", 1), ("ex", 8), ("w", 8), ("ss", 1), ("rs", 1), ("g", 8)]}
            b_rt = S.buf("f_rt")

        for g in groups:
            offs = []
            o = 0
            for t in g:
                offs.append(o)
                o += t["N"]
            b_h2 = S.bufs("f_h2_", len(g))
            b_y = S.bufs("f_y_", len(g))
            b_gT = S.bufs("f_gT_", len(g))
            b_Gb = S.bufs("f_Gb_", len(g))
            for ti, t in enumerate(g):
                N, off, ci = t["N"], offs[ti], t["ci"]
                S.op(DMAQ, lambda e, t=t, N=N: e.dma_start(out=xt[:, :, :N], in_=chunked(t["src"])), writes=[b_xt], dma_home=b_xt)
                norm_mod(kb, cm, nw, xt, b_xt, N, ci, 1, banks[0], b_bank[0],
                         lambda kc, off=off, N=N: h2[:, kc, off:off + N], b_h2[ti], inplace=moe)
                if moe:
                    for tb in range(N // 128):
                        lgp = banks[1]
                        for kc in range(KC):
                            S.op("pe", lambda e, kc=kc, tb=tb: e.matmul(lgp[:, 0:NE], lhsT=xt[:, kc, tb * 128:(tb + 1) * 128], rhs=rw[:, kc, :],
                                                                       start=(kc == 0), stop=(kc == KC - 1)),
                                 reads=[b_xt, b_rw], writes=[b_bank[1]])
                        R = rt
                        S.op("dve", lambda e: e.tensor_copy(out=R["lg"][:], in_=lgp[:, 0:NE]), reads=[b_bank[1]], writes=[b_rt])
                        S.op("dve", lambda e: e.tensor_reduce(out=R["m1"][:], in_=R["lg"][:], axis=AX.X, op=ALU.max), reads=[b_rt], writes=[b_rt])
                        S.op("dve", lambda e: e.tensor_scalar(out=R["eq"][:], in0=R["lg"][:], scalar1=R["m1"][:], scalar2=None, op0=ALU.is_ge), reads=[b_rt], writes=[b_rt])
                        S.op("dve", lambda e: e.scalar_tensor_tensor(out=R["lg2"][:], in0=R["eq"][:], scalar=-1e30, in1=R["lg"][:], op0=ALU.mult, op1=ALU.add), reads=[b_rt], writes=[b_rt])
                        S.op("dve", lambda e: e.tensor_reduce(out=R["m2"][:], in_=R["lg2"][:], axis=AX.X, op=ALU.max), reads=[b_rt], writes=[b_rt])
                        S.op("dve", lambda e: e.tensor_scalar(out=R["sel"][:], in0=R["lg"][:], scalar1=R["m2"][:], scalar2=None, op0=ALU.is_ge), reads=[b_rt], writes=[b_rt])
                        S.op("dve", lambda e: e.tensor_scalar(out=R["nm1"][:], in0=R["m1"][:], scalar1=-1.0, scalar2=None, op0=ALU.mult), reads=[b_rt], writes=[b_rt])
                        S.op("act", lambda e: e.activation(out=R["ex"][:], in_=R["lg"][:], func=ACT.Exp, bias=R["nm1"][:], scale=1.0), reads=[b_rt], writes=[b_rt])
                        S.op("dve", lambda e: e.tensor_tensor(out=R["w"][:], in0=R["ex"][:], in1=R["sel"][:], op=ALU.mult), reads=[b_rt], writes=[b_rt])
                        S.op("dve", lambda e: e.tensor_reduce(out=R["ss"][:], in_=R["w"][:], axis=AX.X, op=ALU.add), reads=[b_rt], writes=[b_rt])
                        S.op("dve", lambda e: e.reciprocal(out=R["rs"][:], in_=R["ss"][:]), reads=[b_rt], writes=[b_rt])
                        S.op("dve", lambda e: e.tensor_scalar(out=R["g"][:], in0=R["w"][:], scalar1=R["rs"][:], scalar2=None, op0=ALU.mult), reads=[b_rt], writes=[b_rt])
                        S.op("pe", lambda e: e.transpose(out=banks[2][0:NE, 0:128], in_=R["g"][:], identity=cm.ident), reads=[b_rt, cm.b_cstf], writes=[b_bank[2]])
                        S.op("dve", lambda e, off=off, tb=tb: e.tensor_copy(out=gT[:, off + tb * 128:off + (tb + 1) * 128], in_=banks[2][0:NE, 0:128]),
                             reads=[b_bank[2]], writes=[b_gT[ti]])
            first = True
            step = 0
            pend = None
            acount = 0
            for ei, (wg_d, wu_d, wd_d) in enumerate(experts):
                if moe:
                    for ti, t in enumerate(g):
                        N, off = t["N"], offs[ti]
                        S.op("pe", lambda e, ei=ei, off=off, N=N: e.matmul(banks[7][:, :N], lhsT=sel[:, ei, :], rhs=gT[:, off:off + N], start=True, stop=True),
                             reads=[b_sel, b_gT[ti]], writes=[b_bank[7]])
                        S.op("act", lambda e, off=off, N=N: e.activation(out=Gb[:, off:off + N], in_=banks[7][:, :N], func=ACT.Copy),
                             reads=[b_bank[7]], writes=[b_Gb[ti]])
                for s in range(nsl):
                    sl = step % 2
                    step += 1
                    f0 = s * FS
                    S.op("pool", lambda e, sl=sl, f0=f0, wg_d=wg_d: e.dma_start(out=wgu[sl][:, 0, :, :], in_=chunked(wg_d[:, f0:f0 + FS])),
                         writes=[b_wg[sl]], dma_home=b_wg[sl])
                    S.op("pool", lambda e, sl=sl, f0=f0, wu_d=wu_d: e.dma_start(out=wgu[sl][:, 1, :, :], in_=chunked(wu_d[:, f0:f0 + FS])),
                         writes=[b_wu[sl]], dma_home=b_wu[sl])
                    S.op("pool", lambda e, sl=sl, f0=f0, wd_d=wd_d: e.dma_start(out=wdn[sl][:], in_=chunked(wd_d[f0:f0 + FS, :])),
                         writes=[b_wd[sl]], dma_home=b_wd[sl])
                    for ti, t in enumerate(g):
                        N, off = t["N"], offs[ti]
                        asl = acount % 2
                        acount += 1

                        def down(half, sl=sl, asl=asl, N=N, off=off, ti=ti, first=first):
                            for oc in range(half * 4, half * 4 + 4):
                                bk = 4 + oc % 4
                                for fc in range(2):
                                    S.op("pe", lambda e, oc=oc, fc=fc, bk=bk: e.matmul(banks[bk][:, :N], lhsT=wdn[sl][:, fc, oc * 128:(oc + 1) * 128],
                                                                                    rhs=abuf[asl][:, fc, :N], start=(fc == 0), stop=(fc == 1)),
                                         reads=[b_wd[sl], b_a[asl][fc]], writes=[b_bank[bk]])
                                if first:
                                    S.op("dve", lambda e, oc=oc, bk=bk: e.tensor_copy(out=yacc[:, oc, off:off + N], in_=banks[bk][:, :N]),
                                         reads=[b_bank[bk]], writes=[b_y[ti]])
                                else:
                                    S.op("dve", lambda e, oc=oc, bk=bk: e.tensor_tensor(out=yacc[:, oc, off:off + N], in0=yacc[:, oc, off:off + N],
                                                                                     in1=banks[bk][:, :N], op=ALU.add),
                                         reads=[b_bank[bk]], writes=[b_y[ti]])

                        for fc in range(2):
                            gb, ub = fc, 2 + fc
                            for kc in range(KC):
                                S.op("pe", lambda e, kc=kc, fc=fc, gb=gb: e.matmul(banks[gb][:, :N], lhsT=wgu[sl][:, 0, kc, fc * 128:(fc + 1) * 128],
                                                                                rhs=h2[:, kc, off:off + N], start=(kc == 0), stop=(kc == KC - 1)),
                                     reads=[b_wg[sl], b_h2[ti]], writes=[b_bank[gb]])
                            for kc in range(KC):
                                S.op("pe", lambda e, kc=kc, fc=fc, ub=ub: e.matmul(banks[ub][:, :N], lhsT=wgu[sl][:, 1, kc, fc * 128:(fc + 1) * 128],
                                                                                rhs=h2[:, kc, off:off + N], start=(kc == 0), stop=(kc == KC - 1)),
                                     reads=[b_wu[sl], b_h2[ti]], writes=[b_bank[ub]])
                            if pend is not None:
                                pend(fc)
                            S.op("act", lambda e, fc=fc, gb=gb: e.activation(out=sg[fc][:, :N], in_=banks[gb][:, :N], func=ACT.Silu),
                                 reads=[b_bank[gb]], writes=[b_sg[fc]])
                            if moe:
                                S.op("dve", lambda e, fc=fc, ub=ub: e.tensor_tensor(out=tt[fc][:, :N], in0=sg[fc][:, :N], in1=banks[ub][:, :N], op=ALU.mult),
                                     reads=[b_sg[fc], b_bank[ub]], writes=[b_tt[fc]])
                                S.op("dve", lambda e, fc=fc, asl=asl: e.tensor_tensor(out=abuf[asl][:, fc, :N], in0=tt[fc][:, :N], in1=Gb[:, off:off + N], op=ALU.mult),
                                     reads=[b_tt[fc], b_Gb[ti]], writes=[b_a[asl][fc]])
                            else:
                                S.op("dve", lambda e, fc=fc, ub=ub, asl=asl: e.tensor_tensor(out=abuf[asl][:, fc, :N], in0=sg[fc][:, :N], in1=banks[ub][:, :N], op=ALU.mult),
                                     reads=[b_sg[fc], b_bank[ub]], writes=[b_a[asl][fc]])
                        pend = down
                    first = False
            if pend is not None:
                pend(0)
                pend(1)
                pend = None
            for ti, t in enumerate(g):
                N, off, ci = t["N"], offs[ti], t["ci"]
                S.op(DMAQ, lambda e, t=t, N=N: e.dma_start(out=xt[:, :, :N], in_=chunked(t["src"])), writes=[b_xt], dma_home=b_xt)
                for kc in range(KC):
                    S.op("dve", lambda e, kc=kc, N=N, off=off, ci=ci: e.scalar_tensor_tensor(
                        out=xt[:, kc, :N], in0=yacc[:, kc, off:off + N], scalar=cm.gate(ci, 1, kc), in1=xt[:, kc, :N], op0=ALU.mult, op1=ALU.add),
                        reads=[b_y[ti], cm.b_mod], writes=[b_xt])
                oo = S.op(DMAQ, lambda e, t=t, N=N: e.dma_start(out=chunked(t["dst"]), in_=xt[:, :, :N]), reads=[b_xt], dma_home=b_xt)
                out_ops.append(oo)
    S.barrier()
    return out_ops


def conv_phase(kb, cm, tiles, cw_in, cw_out):
    nc, S = kb.nc, kb.S
    with ExitStack() as es:
        win = kb.sb(es, "c_win", [128, KC, 3 * D], BF16)
        wout = kb.sb(es, "c_wout", [128, KC, D], BF16)
        b_win = S.bufs("c_win", 3)
        b_wout = S.buf("c_wout")
        for j in range(3):
            for hh in range(2):
                c0 = j * D + hh * 512
                S.op("pool", lambda e, c0=c0: e.dma_start(out=win[:, :, c0:c0 + 512], in_=chunked(cw_in[:, c0:c0 + 512])),
                     writes=[b_win[j]], dma_home=S.buf("c_win_d"))
        S.op("pool", lambda e: e.dma_start(out=wout[:], in_=chunked(cw_out)), writes=[b_wout], dma_home=b_wout)
        xt = kb.sb(es, "c_xt", [128, KC, NT], F32)
        b_xt = S.buf("c_xt")
        h = kb.sb(es, "c_h", [128, KC, NT], BF16)
        b_h = S.buf("c_h")
        mb = kb.sb(es, "c_m", [128, KC, NT], BF16)
        b_m = S.bufs("c_m", KC)
        nw = NormWork(kb, es, "c_nw")
        bsb = [kb.sb(es, f"c_b{i}", [128, NT], BF16) for i in range(2)]
        csb = [kb.sb(es, f"c_c{i}", [128, NT], F32) for i in range(2)]
        zsb = [kb.sb(es, f"c_z{i}", [128, NT], F32) for i in range(2)]
        acc = [kb.sb(es, f"c_acc{i}", [128, NT], F32) for i in range(2)]
        b_bsb, b_csb, b_zsb, b_acc = S.bufs("c_b", 2), S.bufs("c_c", 2), S.bufs("c_z", 2), S.bufs("c_acc", 2)
        banks = [kb.ps(es, f"c_ps{i}", [128, NT], F32) for i in range(8)]
        b_bank = S.bufs("c_bank", 8)
        vec = cm.vec
        cw = VEC["cw"]
        for t in tiles:
            N, ci = t["N"], t["ci"]
            M = N + 2
            S.op(DMAQ, lambda e, t=t, M=M: e.dma_start(out=xt[:, :, :M], in_=chunked(t["src"])), writes=[b_xt], dma_home=b_xt)
            norm_mod(kb, cm, nw, xt, b_xt, M, ci, 0, banks[6], b_bank[6], lambda kc, M=M: h[:, kc, :M], b_h)
            for kc in range(KC):
                sl = kc % 2
                pb, pc, pu = banks[sl * 3], banks[sl * 3 + 1], banks[sl * 3 + 2]
                bb, bc, bu = b_bank[sl * 3], b_bank[sl * 3 + 1], b_bank[sl * 3 + 2]
                for j, (pp, bpp) in enumerate([(pb, bb), (pc, bc), (pu, bu)]):
                    col = j * D + kc * 128
                    for k2 in range(KC):
                        S.op("pe", lambda e, pp=pp, col=col, k2=k2, M=M: e.matmul(pp[:, :M], lhsT=win[:, k2, col:col + 128], rhs=h[:, k2, :M],
                                                                              start=(k2 == 0), stop=(k2 == KC - 1)),
                             reads=[b_win[j], b_h], writes=[bpp])
                S.op("act", lambda e, sl=sl, pb=pb, M=M: e.activation(out=bsb[sl][:, :M], in_=pb[:, :M], func=ACT.Copy), reads=[bb], writes=[b_bsb[sl]])
                S.op("act", lambda e, sl=sl, pc=pc, M=M: e.activation(out=csb[sl][:, :M], in_=pc[:, :M], func=ACT.Copy), reads=[bc], writes=[b_csb[sl]])
                S.op("dve", lambda e, sl=sl, pu=pu, M=M: e.tensor_tensor(out=zsb[sl][:, :M], in0=csb[sl][:, :M], in1=pu[:, :M], op=ALU.mult),
                     reads=[b_csb[sl], bu], writes=[b_zsb[sl]])
                for side, col in (("lo", 0), ("hi", M - 1)):
                    mode = t[side]
                    if mode == "flag":
                        fcol = VEC["vlo"] if side == "lo" else VEC["vhi"]
                        S.op("dve", lambda e, sl=sl, col=col, fcol=fcol: e.tensor_scalar(out=zsb[sl][:, col:col + 1], in0=zsb[sl][:, col:col + 1],
                                                                                      scalar1=vec[:, fcol:fcol + 1], scalar2=None, op0=ALU.mult),
                             reads=[cm.b_vec], writes=[b_zsb[sl]])
                    elif mode == "zero":
                        S.op("dve", lambda e, sl=sl, col=col: e.memset(zsb[sl][:, col:col + 1], 0.0), writes=[b_zsb[sl]])
                w0 = vec[:, cw + kc:cw + kc + 1]
                w1 = vec[:, cw + 8 + kc:cw + 8 + kc + 1]
                w2 = vec[:, cw + 16 + kc:cw + 16 + kc + 1]
                S.op("dve", lambda e, sl=sl, w0=w0, N=N: e.tensor_scalar(out=acc[sl][:, :N], in0=zsb[sl][:, 0:N], scalar1=w0, scalar2=None, op0=ALU.mult),
                     reads=[b_zsb[sl], cm.b_vec], writes=[b_acc[sl]])
                S.op("dve", lambda e, sl=sl, w1=w1, N=N: e.scalar_tensor_tensor(out=acc[sl][:, :N], in0=zsb[sl][:, 1:N + 1], scalar=w1, in1=acc[sl][:, :N],
                                                                            op0=ALU.mult, op1=ALU.add),
                     reads=[b_zsb[sl]], writes=[b_acc[sl]])
                S.op("dve", lambda e, sl=sl, w2=w2, N=N: e.scalar_tensor_tensor(out=acc[sl][:, :N], in0=zsb[sl][:, 2:N + 2], scalar=w2, in1=acc[sl][:, :N],
                                                                            op0=ALU.mult, op1=ALU.add),
                     reads=[b_zsb[sl]], writes=[b_acc[sl]])
                S.op("pool", lambda e, sl=sl, kc=kc, N=N: e.tensor_tensor(out=mb[:, kc, :N], in0=acc[sl][:, :N], in1=bsb[sl][:, 1:N + 1], op=ALU.mult),
                     reads=[b_acc[sl], b_bsb[sl]], writes=[b_m[kc]])
            for oc in range(KC):
                pk = 6 + oc % 2
                for k2 in range(KC):
                    S.op("pe", lambda e, pk=pk, oc=oc, k2=k2, N=N: e.matmul(banks[pk][:, :N], lhsT=wout[:, k2, oc * 128:(oc + 1) * 128], rhs=mb[:, k2, :N],
                                                                        start=(k2 == 0), stop=(k2 == KC - 1)),
                         reads=[b_wout, b_m[k2]], writes=[b_bank[pk]])
                S.op("dve", lambda e, pk=pk, oc=oc, N=N, ci=ci: e.scalar_tensor_tensor(out=xt[:, oc, 1:N + 1], in0=banks[pk][:, :N], scalar=cm.gate(ci, 0, oc),
                                                                                   in1=xt[:, oc, 1:N + 1], op0=ALU.mult, op1=ALU.add),
                     reads=[b_bank[pk], cm.b_mod], writes=[b_xt])
            S.op(DMAQ, lambda e, t=t, N=N: e.dma_start(out=chunked(t["dst"]), in_=xt[:, :, 1:N + 1]), reads=[b_xt], dma_home=b_xt)
    S.barrier()


def conv_tiles(xpad, xm, ci, ntok, lo, hi):
    tiles = []
    t0 = 0
    while t0 < ntok:
        N = min(510, ntok - t0)
        tiles.append(dict(src=xpad[:, t0:t0 + N + 2], dst=xm[:, t0:t0 + N], N=N, ci=ci,
                          lo=(lo if t0 == 0 else None), hi=(hi if t0 + N == ntok else None)))
        t0 += N
    return tiles


def ffn_tiles(src, dst, ci, ntok):
    return [dict(src=src[:, t0:min(t0 + NT, ntok)], dst=dst[:, t0:min(t0 + NT, ntok)], N=min(NT, ntok - t0), ci=ci) for t0 in range(0, ntok, NT)]


def build_layer_B(with_ctx, n_exp=NE):
    global DMAQ
    DMAQ = "sp"
    kb = KB()
    xpad = kb.din("xpad", [D, HALF + 2])
    vecd = kb.din("vec", [128, NVEC])
    ada_w = kb.din("ada_w", [D, 6 * D])
    consts = dict(cbf=kb.din("cbf", [128, 3, 128]), cf=kb.din("cf", [128, 2, 128]))
    cw_in = kb.din("cw_in", [D, 3 * D])
    cw_out = kb.din("cw_out", [D, D])
    router_w = kb.din("router_w", [D, NE])
    kb.din("selm", [NE, NE, 128])
    mwg = kb.din("mwg", [n_exp, D, DFFE])
    mwu = kb.din("mwu", [n_exp, D, DFFE])
    mwd = kb.din("mwd", [n_exp, DFFE, D])
    yo = kb.dout("yo", [D, HALF])
    xm = kb.dscratch("xm", [D, HALF])
    if with_ctx:
        xcpad = kb.din("xcpad", [D, CTX + 2])
        yc = kb.dout("yc", [D, CTX])
        xcm = kb.dscratch("xcm", [D, CTX])
    cm = Common(kb, vecd, ada_w, consts)
    ct = conv_tiles(xpad, xm, 0, HALF, "flag", "flag")
    ft = ffn_tiles(xm, yo, 0, HALF)
    if with_ctx:
        ct += conv_tiles(xcpad, xcm, 1, CTX, "zero", "zero")
        ft = ft[:4] + ffn_tiles(xcm, yc, 1, CTX) + ft[4:]
    conv_phase(kb, cm, ct, cw_in, cw_out)
    experts = [(mwg[e], mwu[e], mwd[e]) for e in range(n_exp)]
    outs = ffn_phase(kb, cm, ft, experts, DFFE, router_w=router_w)
    stats = kb.S.emit(kb.nc, final_wait_ops=outs)
    kb.es.close()
    return kb.nc, stats


def colpack(v):
    return np.ascontiguousarray(v.reshape(-1, 128).T)


def make_consts():
    cbf = np.zeros((128, 3, 128), np.float32)
    cbf[:, 0, :] = 1.0 / D
    for hh in range(2):
        cbf[hh * 64:(hh + 1) * 64, 1, hh * 64:(hh + 1) * 64] = 1.0 / 64
    for i in range(64):
        cbf[2 * i + 1, 2, 2 * i] = -1.0
        cbf[2 * i, 2, 2 * i + 1] = 1.0
    cf = np.zeros((128, 2, 128), np.float32)
    cf[:, 0, :] = np.eye(128, dtype=np.float32)
    cf[:, 1, :] = 1.0
    selm = np.zeros((NE, NE, 128), np.float32)
    for e in range(NE):
        selm[e, e, :] = 1.0
    return cbf, cf, selm


def make_vec(inp, layer, b, half):
    i = layer // 2
    v = np.zeros((128, NVEC), np.float32)
    v[:, VEC["c"]:VEC["c"] + 8] = colpack(inp["c"][b])
    v[:, VEC["cc"]:VEC["cc"] + 8] = colpack(inp["c_ctx"])
    v[:, VEC["n1"]:VEC["n1"] + 8] = colpack(inp["norm1_g"][layer])
    v[:, VEC["n2"]:VEC["n2"] + 8] = colpack(inp["norm2_g"][layer])
    v[:, VEC["adab"]:VEC["adab"] + 48] = colpack(inp["ada_b"][layer])
    if layer % 2 == 0:
        for n, k in (("qna", "qnorm_a"), ("kna", "knorm_a"), ("qnb", "qnorm_b"), ("knb", "knorm_b")):
            v[:, VEC[n]] = np.tile(inp[k][i], 2)
        v[:, VEC["sink"]:VEC["sink"] + 8] = inp["sink_b"][i][None, :]
    else:
        for j in range(3):
            v[:, VEC["cw"] + 8 * j:VEC["cw"] + 8 * j + 8] = colpack(inp["conv_w"][i][j])
    v[:, VEC["vlo"]] = float(half)
    v[:, VEC["vhi"]] = float(1 - half)
    return v


W_QA, W_QB, W_KA, W_KB, W_V = 0, 512, 1024, 1152, 1280
NWIN = 1536
NTL = SEQ // NT
NB = 2 + SEQ // 128


class QKWork:
    def __init__(self, kb, es):
        S = kb.S
        self.sq = kb.sb(es, "qk_sq", [128, NT], BF16)
        self.sd = kb.sb(es, "qk_sd", [128, NT], F32)
        self.r = kb.sb(es, "qk_r", [128, NT], F32)
        self.qn = kb.sb(es, "qk_qn", [128, NT], BF16)
        self.t1 = kb.sb(es, "qk_t1", [128, NT], F32)
        self.t2 = kb.sb(es, "qk_t2", [128, NT], F32)
        self.b = {n: S.buf("qk_" + n) for n in ("sq", "sd", "r", "qn", "t1", "t2")}


def qk_norm_rope(kb, cm, qw, ps, b_ps, N, gcol, cs, b_cs, ms_ps, b_ms, rot_ps, b_rot, out_ap, b_out):
    S = kb.S
    vec = cm.vec
    S.op("act", lambda e: e.activation(out=qw.sq[:, :N], in_=ps[:, :N], func=ACT.Square), reads=[b_ps], writes=[qw.b["sq"]])
    S.op("pe", lambda e: e.matmul(ms_ps[:, :N], lhsT=cm.bones, rhs=qw.sq[:, :N], start=True, stop=True), reads=[qw.b["sq"], cm.b_cst], writes=[b_ms])
    S.op("act", lambda e: e.activation(out=qw.sd[:, :N], in_=ms_ps[:, :N], func=ACT.Sqrt, bias=cm.eps_col, scale=1.0), reads=[b_ms, cm.b_eps], writes=[qw.b["sd"]])
    S.op("dve", lambda e: e.reciprocal(out=qw.r[:, :N], in_=qw.sd[:, :N]), reads=[qw.b["sd"]], writes=[qw.b["r"]])
    pieces = out_ap if isinstance(out_ap, list) else [(out_ap, 0, 128)]
    if cs is None:
        for (oap, p0, p1) in pieces:
            S.op("dve", lambda e: e.scalar_tensor_tensor(out=oap, in0=ps[p0:p1, :N], scalar=vec[p0:p1, gcol:gcol + 1], in1=qw.r[p0:p1, :N], op0=ALU.mult, op1=ALU.mult),
                 reads=[b_ps, qw.b["r"], cm.b_vec], writes=[b_out])
        return
    cos, sin = cs
    S.op("dve", lambda e: e.scalar_tensor_tensor(out=qw.qn[:, :N], in0=ps[:, :N], scalar=vec[:, gcol:gcol + 1], in1=qw.r[:, :N], op0=ALU.mult, op1=ALU.mult),
         reads=[b_ps, qw.b["r"], cm.b_vec], writes=[qw.b["qn"]])
    S.op("pe", lambda e: e.matmul(rot_ps[:, :N], lhsT=cm.rotm, rhs=qw.qn[:, :N], start=True, stop=True), reads=[qw.b["qn"], cm.b_cst], writes=[b_rot])
    S.op("dve", lambda e: e.tensor_tensor(out=qw.t1[:, :N], in0=qw.qn[:, :N], in1=cos[:, :N], op=ALU.mult), reads=[qw.b["qn"], b_cs], writes=[qw.b["t1"]])
    S.op("dve", lambda e: e.tensor_tensor(out=qw.t2[:, :N], in0=rot_ps[:, :N], in1=sin[:, :N], op=ALU.mult), reads=[b_rot, b_cs], writes=[qw.b["t2"]])
    for (oap, p0, p1) in pieces:
        S.op("pool", lambda e: e.tensor_tensor(out=oap, in0=qw.t1[p0:p1, :N], in1=qw.t2[p0:p1, :N], op=ALU.add), reads=[qw.b["t1"], qw.b["t2"]], writes=[b_out])


def attn_phase(kb, cm, w_in_d, w_out_d, cosd, sind, bandd, xs, xc, xm, xcm, with_ctx_out):
    nc, S = kb.nc, kb.S
    vec = cm.vec
    with ExitStack() as es:
        KA = kb.sb(es, "KA", [128, NB * 128], BF16)
        KBc = kb.sb(es, "KB", [128, NB * 128], BF16)
        VA = kb.sb(es, "VA", [128, NB, 2, 65], BF16)
        VB = kb.sb(es, "VB", [128, NB, 2, 65], BF16)
        b_KA, b_VA, b_KB, b_VB = S.buf("KA"), S.buf("VA"), S.buf("KB"), S.buf("VB")
        win = kb.sb(es, "a_win", [128, KC, NWIN], BF16)
        wout = kb.sb(es, "a_wout", [128, KC, D], BF16)
        b_win, b_wout = S.buf("a_win"), S.buf("a_wout")
        for j in range(3):
            c0 = j * 512
            S.op("pool", lambda e: e.dma_start(out=win[:, :, c0:c0 + 512], in_=chunked(w_in_d[:, c0:c0 + 512])), writes=[b_win], dma_home=S.buf("a_win_d"))
        S.op("pool", lambda e: e.dma_start(out=wout[:], in_=chunked(w_out_d)), writes=[b_wout], dma_home=b_wout)
        band = kb.sb(es, "a_band", [128, 384], BF16)
        b_band = S.buf("a_band")
        S.op("pool", lambda e: e.dma_start(out=band[:], in_=bandd), writes=[b_band], dma_home=b_band)
        esink = kb.sb(es, "a_esink", [128, 8], F32)
        b_esink = S.buf("a_esink")
        S.op("act", lambda e: e.activation(out=esink[:], in_=vec[:, VEC["sink"]:VEC["sink"] + 8], func=ACT.Exp), reads=[cm.b_vec], writes=[b_esink])
        xt = kb.sb(es, "a_xt", [128, KC, NT], F32)
        b_xt = S.buf("a_xt")
        h = kb.sb(es, "a_h", [128, KC, NT], BF16)
        b_h = S.buf("a_h")
        Qlo = kb.sb(es, "a_Qlo", [128, 8, NT], BF16)
        Qhi = kb.sb(es, "a_Qhi", [128, 8, NT], BF16)
        b_Q = S.bufs("a_Q", 8)
        for _c in range(8):
            S.op("pool", lambda e: e.memset(Qlo[:, _c, :], 0.0), writes=[b_Q[_c]])
            S.op("pool", lambda e: e.memset(Qhi[:, _c, :], 0.0), writes=[b_Q[_c]])
        oT = kb.sb(es, "a_oT", [128, 8, NT], BF16)
        b_oT = S.bufs("a_oT", 16)
        nw = NormWork(kb, es, "a_nw")
        qw = QKWork(kb, es)
        cst = kb.sb(es, "a_cos", [128, NT], F32)
        snt = kb.sb(es, "a_sin", [128, NT], F32)
        b_cs = S.buf("a_cs")
        b_cos_d, b_sin_d = S.buf("a_cos_d"), S.buf("a_sin_d")
        psb = [kb.sb(es, f"a_p{i}", [128, NT], BF16) for i in range(3)]
        b_psb = S.bufs("a_p", 3)
        rec = kb.sb(es, "a_rec", [128, NT], F32)
        bcs = kb.sb(es, "a_bc", [64, NT], F32)
        b_rec, b_bcs = S.buf("a_rec"), S.buf("a_bcs")
        banks = [kb.ps(es, f"a_ps{i}", [128, NT], F32) for i in range(8)]
        b_bank = S.bufs("a_bank", 8)

        S.op("dve", lambda e: e.memset(VA[:, :, :, 64:65], 1.0), writes=[b_VA])
        S.op("dve", lambda e: e.memset(VB[:, :, :, 64:65], 1.0), writes=[b_VB])

        def load_tile(src, N, ci, pos0):
            S.op(DMAQ, lambda e: e.dma_start(out=xt[:, :, :N], in_=chunked(src)), writes=[b_xt], dma_home=b_xt)
            if pos0 is not None:
                S.op(DMAQ, lambda e: e.dma_start(out=cst[:, :N], in_=cosd[:, pos0:pos0 + N]), writes=[b_cs], dma_home=b_cos_d)
                S.op(DMAQ, lambda e: e.dma_start(out=snt[:, :N], in_=sind[:, pos0:pos0 + N]), writes=[b_cs], dma_home=b_sin_d)
            norm_mod(kb, cm, nw, xt, b_xt, N, ci, 0, banks[5], b_bank[5], lambda kc: h[:, kc, :N], b_h)

        def project(col, N):
            for kc in range(KC):
                S.op("pe", lambda e: e.matmul(banks[5][:, :N], lhsT=win[:, kc, col:col + 128], rhs=h[:, kc, :N], start=(kc == 0), stop=(kc == KC - 1)),
                     reads=[b_win, b_h], writes=[b_bank[5]])

        def kv_build(src, N, ci, pos0, blk0):
            load_tile(src, N, ci, pos0)
            cs = None if pos0 is None else (cst, snt)
            project(W_KA, N)
            qk_norm_rope(kb, cm, qw, banks[5], b_bank[5], N, VEC["kna"], cs, b_cs, banks[6], b_bank[6], banks[7], b_bank[7],
                         KA[:, blk0 * 128:blk0 * 128 + N], b_KA)
            project(W_KB, N)
            qk_norm_rope(kb, cm, qw, banks[5], b_bank[5], N, VEC["knb"], cs, b_cs, banks[6], b_bank[6], banks[7], b_bank[7],
                         KBc[:, blk0 * 128:blk0 * 128 + N], b_KB)
            for tb in range(N // 128):
                vp = banks[4]
                for kc in range(KC):
                    S.op("pe", lambda e: e.matmul(vp[:, 0:256], lhsT=h[:, kc, tb * 128:(tb + 1) * 128], rhs=win[:, kc, W_V:W_V + 256], start=(kc == 0), stop=(kc == KC - 1)),
                         reads=[b_h, b_win], writes=[b_bank[4]])
                S.op("dve", lambda e: e.tensor_copy(out=VA[:, blk0 + tb, :, 0:64], in_=vp[:, 0:128].rearrange("p (a b) -> p a b", a=2)),
                     reads=[b_bank[4]], writes=[b_VA])
                S.op("dve", lambda e: e.tensor_copy(out=VB[:, blk0 + tb, :, 0:64], in_=vp[:, 128:256].rearrange("p (a b) -> p a b", a=2)),
                     reads=[b_bank[4]], writes=[b_VB])

        kv_build(xc, CTX, 1, None, 0)
        for it in range(NTL):
            kv_build(xs[:, it * NT:(it + 1) * NT], NT, 0, it * NT, 2 + 4 * it)

        def finalize(o_ps, b_o, N, chunk, half, sink_h):
            if sink_h is not None:
                S.op("dve", lambda e: e.tensor_scalar(out=rec[64:65, :N], in0=o_ps[64:65, :N], scalar1=esink[64:65, sink_h:sink_h + 1], scalar2=None, op0=ALU.add),
                     reads=[b_o, b_esink], writes=[b_rec])
                S.op("dve", lambda e: e.reciprocal(out=rec[64:65, :N], in_=rec[64:65, :N]), reads=[b_rec], writes=[b_rec])
            else:
                S.op("dve", lambda e: e.reciprocal(out=rec[64:65, :N], in_=o_ps[64:65, :N]), reads=[b_o], writes=[b_rec])
            S.op("pe", lambda e: e.matmul(banks[7][0:64, :N], lhsT=cm.cst_f[64:65, 1, 0:64], rhs=rec[64:65, :N], start=True, stop=True),
                 reads=[b_rec, cm.b_cstf], writes=[b_bank[7]])
            S.op("dve", lambda e: e.tensor_copy(out=bcs[:, :N], in_=banks[7][0:64, :N]), reads=[b_bank[7]], writes=[b_bcs])
            p0 = half * 64
            S.op("dve", lambda e: e.tensor_tensor(out=oT[p0:p0 + 64, chunk, :N], in0=o_ps[0:64, :N], in1=bcs[:, :N], op=ALU.mult),
                 reads=[b_o, b_bcs], writes=[b_oT[2 * chunk + half]])

        scnt = [0]
        PIPE = 2

        def run_steps(steps):
            n = len(steps)
            sis = []
            for i in range(n + PIPE):
                if i < n:
                    st = steps[i]
                    si = scnt[0] % 3
                    scnt[0] += 1
                    sis.append(si)
                    kv, qc, blk, q0, q1, m0 = st["kv"], st["qc"], st["blk"], st["q0"], st["q1"], st["m0"]
                    Kc = st["Kc"]
                    Qs = Qlo if kv == 0 else Qhi
                    S.op("pe", lambda e: e.matmul(banks[si][:, q0:q1], lhsT=Kc[:, blk * 128:(blk + 1) * 128],
                                                  rhs=Qs[:, qc, q0:q1], start=True, stop=True),
                         reads=[st["b_K"], b_Q[qc]], writes=[b_bank[si]])
                    S.op("act", lambda e: e.activation(out=psb[si][:, q0:q1], in_=banks[si][:, q0:q1], func=ACT.Exp, scale=SCALE),
                         reads=[b_bank[si]], writes=[b_psb[si]])
                    if m0 is not None:
                        S.op("dve", lambda e: e.tensor_tensor(out=psb[si][:, q0:q1], in0=psb[si][:, q0:q1], in1=band[:, m0:m0 + (q1 - q0)], op=ALU.mult),
                             reads=[b_band], writes=[b_psb[si]])
                j = i - PIPE
                if j >= 0:
                    st = steps[j]
                    si = sis[j]
                    kv, blk, q0, q1 = st["kv"], st["blk"], st["q0"], st["q1"]
                    Vc, o_ps = st["Vc"], st["o_ps"]
                    S.op("pe", lambda e: e.matmul(o_ps[0:65, q0:q1], lhsT=Vc[:, blk, kv, 0:65], rhs=psb[si][:, q0:q1], start=st["start"], stop=st["stop"],
                                                  skip_group_check=True),
                         reads=[st["b_V"], b_psb[si]], writes=[st["b_o"]])
                    if st["fin"] is not None:
                        st["fin"]()

        def q_tile(src, dst, N, ci, pos0, it):
            load_tile(src, N, ci, pos0)
            cs = None if pos0 is None else (cst, snt)
            for c in range(8):
                project((W_QA if c < 4 else W_QB) + (c % 4) * 128, N)
                qk_norm_rope(kb, cm, qw, banks[5], b_bank[5], N, VEC["qna"] if c < 4 else VEC["qnb"], cs, b_cs, banks[6], b_bank[6], banks[7], b_bank[7],
                             [(Qlo[0:64, c, :N], 0, 64), (Qhi[64:128, c, :N], 64, 128)], b_Q[c])
            hcnt = 0
            steps = []
            for grp in range(2):
                for hd in range(8):
                    kv, c = hd // 4, hd % 4
                    ob = 3 + hcnt % 2
                    hcnt += 1
                    if grp == 0:
                        blocks = [(0, 0, N, None), (1, 0, N, None)] if it is None else [(b, 0, N, None) for b in range(NB)]
                        Kc, b_K, Vc, b_V, qc = KA, b_KA, VA, b_VA, c
                        fin = (lambda ob=ob, c=c, kv=kv: finalize(banks[ob], b_bank[ob], N, c, kv, None))
                    else:
                        blocks = [(0, 0, N, None), (1, 0, N, None)]
                        if it is not None:
                            for j in range(6):
                                lb = it * 4 - 1 + j
                                if lb < 0 or lb >= SEQ // 128:
                                    continue
                                q0 = max(0, 128 * (j - 2))
                                q1 = min(512, 128 * (j - 2) + 384)
                                blocks.append((2 + lb, q0, q1, q0 - 128 * (j - 2)))
                        Kc, b_K, Vc, b_V, qc = KBc, b_KB, VB, b_VB, 4 + c
                        fin = (lambda ob=ob, c=c, kv=kv, hd=hd: finalize(banks[ob], b_bank[ob], N, 4 + c, kv, hd))
                    nb = len(blocks)
                    for bi, (blk, q0, q1, m0) in enumerate(blocks):
                        steps.append(dict(Kc=Kc, b_K=b_K, Vc=Vc, b_V=b_V, kv=kv, qc=qc, blk=blk, q0=q0, q1=q1, m0=m0, o_ps=banks[ob], b_o=b_bank[ob],
                                          start=(bi == 0), stop=(bi == nb - 1), fin=(fin if bi == nb - 1 else None)))
            run_steps(steps)
            for oc in range(KC):
                pk = 5 + oc % 2
                for c in range(8):
                    S.op("pe", lambda e: e.matmul(banks[pk][:, :N], lhsT=wout[:, c, oc * 128:(oc + 1) * 128], rhs=oT[:, c, :N], start=(c == 0), stop=(c == 7)),
                         reads=[b_wout, b_oT[2 * c], b_oT[2 * c + 1]], writes=[b_bank[pk]])
                S.op("dve", lambda e: e.scalar_tensor_tensor(out=xt[:, oc, :N], in0=banks[pk][:, :N], scalar=cm.gate(ci, 0, oc), in1=xt[:, oc, :N], op0=ALU.mult, op1=ALU.add),
                     reads=[b_bank[pk], cm.b_mod], writes=[b_xt])
            S.op(DMAQ, lambda e: e.dma_start(out=chunked(dst), in_=xt[:, :, :N]), reads=[b_xt], dma_home=b_xt)

        if with_ctx_out:
            q_tile(xc, xcm, CTX, 1, None, None)
        for it in range(NTL):
            q_tile(xs[:, it * NT:(it + 1) * NT], xm[:, it * NT:(it + 1) * NT], NT, 0, it * NT, it)
    S.barrier()


def with_ctx_tiles(ft, fc):
    return ft[:4] + fc + ft[4:]


def build_fused(n_layers=4, n_exp=NE):
    global DMAQ
    DMAQ = "sp"
    kb = KB()
    x0 = kb.din("x0", [D, SEQ])
    xc0 = kb.din("xc0", [D, CTX])
    vecd = kb.din("vec", [4, 128, NVEC])
    ada_w = kb.din("ada_w", [4, D, 6 * D])
    consts = dict(cbf=kb.din("cbf", [128, 3, 128]), cf=kb.din("cf", [128, 2, 128]))
    kb.din("selm", [NE, NE, 128])
    w_in = kb.din("w_in", [2, D, NWIN])
    w_out = kb.din("w_out", [2, D, D])
    cosd = kb.din("rcos", [128, SEQ])
    sind = kb.din("rsin", [128, SEQ])
    bandd = kb.din("bandm", [128, 384])
    fwg = kb.din("fwg", [2, D, DFF])
    fwu = kb.din("fwu", [2, D, DFF])
    fwd = kb.din("fwd", [2, DFF, D])
    cw_in = kb.din("cw_in", [2, D, 3 * D])
    cw_out = kb.din("cw_out", [2, D, D])
    router_w = kb.din("router_w", [2, D, NE])
    mwg = kb.din("mwg", [2, n_exp, D, DFFE])
    mwu = kb.din("mwu", [2, n_exp, D, DFFE])
    mwd = kb.din("mwd", [2, n_exp, DFFE, D])
    yo = kb.dout("yo", [D, SEQ])
    xm = kb.dscratch("xm", [D, SEQ])
    xcm = kb.dscratch("xcm", [D, CTX])
    xp1 = kb.dscratch("xp1", [D, SEQ + 2])
    xcp1 = kb.dscratch("xcp1", [D, CTX + 2])
    x2 = kb.dscratch("x2", [D, SEQ])
    xc2 = kb.dscratch("xc2", [D, CTX])
    xp3 = kb.dscratch("xp3", [D, SEQ + 2])
    outs = []
    zt = kb.sb(kb.es, "zero_col", [128, KC, 1], F32)
    b_zt = kb.S.buf("zero_col")
    kb.S.op("dve", lambda e: e.memset(zt[:], 0.0), writes=[b_zt])
    for buf, n in ((xp1, SEQ), (xcp1, CTX), (xp3, SEQ)):
        for col in (0, n + 1):
            kb.S.op(DMAQ, lambda e: e.dma_start(out=chunked(buf[:, col:col + 1]), in_=zt[:], allow_slow_non_contiguous=True), reads=[b_zt], dma_home=kb.S.buf("zc_d"))
    for layer in range(n_layers):
        i = layer // 2
        with ExitStack() as es:
            cm = Common(kb, vecd[layer], ada_w[layer], consts, es=es)
            if layer == 0:
                attn_phase(kb, cm, w_in[0], w_out[0], cosd, sind, bandd, x0, xc0, xm, xcm, True)
                ft = with_ctx_tiles(ffn_tiles(xm, xp1[:, 1:SEQ + 1], 0, SEQ), ffn_tiles(xcm, xcp1[:, 1:CTX + 1], 1, CTX))
                outs = ffn_phase(kb, cm, ft, [(fwg[0], fwu[0], fwd[0])], DFF)
            elif layer == 1:
                ct = conv_tiles(xp1, xm, 0, SEQ, "zero", "zero") + conv_tiles(xcp1, xcm, 1, CTX, "zero", "zero")
                conv_phase(kb, cm, ct, cw_in[0], cw_out[0])
                ft = with_ctx_tiles(ffn_tiles(xm, x2, 0, SEQ), ffn_tiles(xcm, xc2, 1, CTX))
                outs = ffn_phase(kb, cm, ft, [(mwg[0, e], mwu[0, e], mwd[0, e]) for e in range(n_exp)], DFFE, router_w=router_w[0])
            elif layer == 2:
                attn_phase(kb, cm, w_in[1], w_out[1], cosd, sind, bandd, x2, xc2, xm, None, False)
                outs = ffn_phase(kb, cm, ffn_tiles(xm, xp3[:, 1:SEQ + 1], 0, SEQ), [(fwg[1], fwu[1], fwd[1])], DFF)
            else:
                conv_phase(kb, cm, conv_tiles(xp3, xm, 0, SEQ, "zero", "zero"), cw_in[1], cw_out[1])
                outs = ffn_phase(kb, cm, ffn_tiles(xm, yo, 0, SEQ), [(mwg[1, e], mwu[1, e], mwd[1, e]) for e in range(n_exp)], DFFE, router_w=router_w[1])
    if n_layers < 4:
        pass
    stats = kb.S.emit(kb.nc, final_wait_ops=outs)
    kb.es.close()
    return kb.nc, stats


def rope_tables():
    half = 32
    inv = (10000.0 ** (-np.arange(0, half, 2, dtype=np.float32) / half)).astype(np.float32)
    pos = np.arange(SEQ)
    row = (pos // 64).astype(np.float32)
    col = (pos % 64).astype(np.float32)
    ang = np.concatenate([row[:, None] * inv[None, :], col[:, None] * inv[None, :]], axis=-1)
    cos = np.cos(ang).astype(np.float32)
    sin = np.sin(ang).astype(np.float32)
    pidx = (np.arange(128) % 64) // 2
    return np.ascontiguousarray(cos[:, pidx].T), np.ascontiguousarray(sin[:, pidx].T)


def band_mask():
    kk = np.arange(128)[:, None]
    u = np.arange(384)[None, :] - 128
    return ((u >= kk - 128) & (u <= kk + 128)).astype(np.float32)


def perm_w_in(w):
    cols = []
    for base in (0, 768):
        for c in range(4):
            cols += [w[:, base + c * 64:base + (c + 1) * 64], w[:, base + (4 + c) * 64:base + (5 + c) * 64]]
    cols += [w[:, 512:640], w[:, 1280:1408], w[:, 640:768], w[:, 1408:1536]]
    return np.ascontiguousarray(np.concatenate(cols, axis=1))


def perm_w_out(w):
    rows = []
    for base in (0, 512):
        for c in range(4):
            rows += [w[base + c * 64:base + (c + 1) * 64], w[base + (4 + c) * 64:base + (5 + c) * 64]]
    return np.ascontiguousarray(np.concatenate(rows, axis=0))


_NC = {}


def make_inputs(inp, b):
    cbf, cf, selm = make_consts()
    cos, sin = rope_tables()
    return dict(
        x0=np.ascontiguousarray(inp["x"][b].T), xc0=np.ascontiguousarray(inp["ctx"][b].T),
        vec=np.stack([make_vec(inp, l, b, 0) for l in range(4)]), ada_w=inp["ada_w"], cbf=cbf, cf=cf, selm=selm,
        w_in=np.stack([perm_w_in(inp["attn_w_in"][i]) for i in range(2)]), w_out=np.stack([perm_w_out(inp["attn_w_out"][i]) for i in range(2)]),
        rcos=cos, rsin=sin, bandm=band_mask(), fwg=inp["ffn_w_gate"], fwu=inp["ffn_w_up"], fwd=inp["ffn_w_down"],
        cw_in=inp["conv_w_in"], cw_out=inp["conv_w_out"], router_w=inp["router_w"],
        mwg=inp["moe_w_gate"], mwu=inp["moe_w_up"], mwd=inp["moe_w_down"])


def kernel(**inp):
    inp = {k: np.asarray(v) for k, v in inp.items()}
    B = inp["x"].shape[0]
    if "nc" not in _NC:
        _NC["nc"] = build_fused()[0]
    per_b = [make_inputs(inp, b) for b in range(B)]
    ins = [per_b[c // 2] for c in range(8)]
    res = run_bass_kernel_spmd(_NC["nc"], ins, core_ids=list(range(8)))
    return np.stack([np.ascontiguousarray(res.results[2 * b]["yo"].T) for b in range(B)]).astype(np.float32)
```

```python
import os
import numpy as np
from contextlib import ExitStack
import concourse.bass as bass
import concourse.mybir as mybir
from concourse.bass_utils import run_bass_kernel_spmd

F32 = mybir.dt.float32
BF16 = mybir.dt.bfloat16
ACT = mybir.ActivationFunctionType
ALU = mybir.AluOpType
AX = mybir.AxisListType

D = 1024
KC = 8
SEQ = 8192
HALF = 4096
CTX = 256
NT = 512
DFF = 2816
DFFE = 3584
NE = 8
EPS = 1e-6
SCALE = 0.125
ENG = ("pe", "act", "dve", "pool", "sp")
DMAQ = "pool"


class Buf:
    __slots__ = ("name", "last_w", "readers", "dma_sem_idx")

    def __init__(self, name):
        self.name = name
        self.last_w = None
        self.readers = []
        self.dma_sem_idx = None


class Op:
    __slots__ = ("eng", "fn", "deps", "is_dma", "sem", "signal", "count", "idx")


class _Rec:
    def __init__(self):
        self.call = None

    def __getattr__(self, name):
        def f(*a, **k):
            self.call = (name, a, k)
            return None
        return f


class Sched:
    def __init__(self, same_engine_sync=True):
        self.ops = []
        self.same_engine_sync = same_engine_sync
        self.n_dma_sems = 0
        self.last_eng = {}
        self.dma_since_bar = []

    def buf(self, name):
        return Buf(name)

    def bufs(self, name, n):
        return [Buf(f"{name}{i}") for i in range(n)]

    def op(self, eng, fn, reads=(), writes=(), dma_home=None, extra_deps=()):
        o = Op()
        o.eng = eng
        rec = _Rec()
        fn(rec)
        o.fn = rec.call
        assert o.fn is not None
        o.is_dma = dma_home is not None
        o.idx = len(self.ops)
        o.signal = False
        o.count = None
        deps = set(extra_deps)
        for b in reads:
            if b.last_w is not None:
                deps.add(b.last_w)
        for b in writes:
            if b.last_w is not None:
                deps.add(b.last_w)
            for r in b.readers:
                deps.add(r)
        fdeps = []
        for d in deps:
            dop = self.ops[d]
            if (not dop.is_dma) and (not o.is_dma) and dop.eng == eng:
                if eng == "pe" or not self.same_engine_sync:
                    continue
            fdeps.append(d)
        o.deps = fdeps
        if o.is_dma:
            if dma_home.dma_sem_idx is None:
                dma_home.dma_sem_idx = self.n_dma_sems
                self.n_dma_sems += 1
            o.sem = ("dma", dma_home.dma_sem_idx)
            self.dma_since_bar.append(o.idx)
        else:
            o.sem = ("eng", eng)
            self.last_eng[eng] = o.idx
        for b in reads:
            b.readers.append(o.idx)
        for b in writes:
            b.last_w = o.idx
            b.readers = []
        self.ops.append(o)
        return o

    def barrier(self):
        deps = list(self.last_eng.values()) + list(self.dma_since_bar)
        self.dma_since_bar = []
        for e in ENG:
            o = Op()
            o.eng = e
            o.fn = None
            o.is_dma = False
            o.idx = len(self.ops)
            o.signal = False
            o.count = None
            o.sem = ("eng", e)
            o.deps = [d for d in deps if not (self.ops[d].eng == e and not self.ops[d].is_dma and e == "pe")]
            self.ops.append(o)

    def emit(self, nc, final_wait_ops=()):
        ops = self.ops
        for o in ops:
            for d in o.deps:
                ops[d].signal = True
        for o in final_wait_ops:
            o.signal = True
        counters = {}
        for o in ops:
            if o.fn is None:
                o.signal = False
                continue
            if o.signal or o.is_dma:
                inc = 16 if o.is_dma else 1
                counters[o.sem] = counters.get(o.sem, 0) + inc
                o.count = counters[o.sem]
                o.signal = True
        with ExitStack() as es:
            sems = {}
            for key in counters:
                sems[key] = es.enter_context(nc.semaphore(f"s_{key[0]}_{key[1]}"))
            block = es.enter_context(nc.Block())
            per_eng = {e: [o for o in ops if o.eng == e] for e in ENG}
            n_waits = [0]

            def make(engname):
                def body(engobj):
                    waited = {}
                    for o in per_eng[engname]:
                        need = {}
                        for d in o.deps:
                            dop = ops[d]
                            if dop.count is None:
                                continue
                            if dop.count > need.get(dop.sem, 0):
                                need[dop.sem] = dop.count
                        for sk, v in need.items():
                            if waited.get(sk, 0) >= v:
                                continue
                            engobj.wait_ge(sems[sk], v)
                            n_waits[0] += 1
                            waited[sk] = v
                        if o.fn is None:
                            continue
                        ins = getattr(engobj, o.fn[0])(*o.fn[1], **o.fn[2])
                        if o.signal:
                            ins.then_inc(sems[o.sem], 16 if o.is_dma else 1)
                    if engname == "sp":
                        for fo in final_wait_ops:
                            engobj.wait_ge(sems[fo.sem], fo.count)
                return body

            block.tensor(make("pe"))
            block.scalar(make("act"))
            block.vector(make("dve"))
            block.gpsimd(make("pool"))
            block.sync(make("sp"))
        self.stats = dict(n_ops=len(ops), n_sems=len(counters), n_waits=n_waits[0],
                          per_eng={e: len(per_eng[e]) for e in ENG})
        return self.stats


VEC = {}
_c = 0
for _n, _w in [("c", 8), ("cc", 8), ("n1", 8), ("n2", 8), ("adab", 48), ("qna", 1), ("kna", 1),
               ("qnb", 1), ("knb", 1), ("sink", 8), ("vlo", 1), ("vhi", 1), ("cw", 24)]:
    VEC[_n] = _c
    _c += _w
NVEC = _c


class KB:
    def __init__(self):
        self.nc = bass.Bass("TRN2", target_bir_lowering=False)
        self.S = Sched()
        self.es = ExitStack()
        self.dram_in = {}
        self.uid = 0
        self.dump = None

    def din(self, name, shape, dt=F32):
        t = self.nc.dram_tensor(name, list(shape), dt, kind="ExternalInput").ap()
        self.dram_in[name] = t
        return t

    def dout(self, name, shape, dt=F32):
        return self.nc.dram_tensor(name, list(shape), dt, kind="ExternalOutput").ap()

    def dscratch(self, name, shape, dt=F32):
        return self.nc.dram_tensor(name, list(shape), dt).ap()

    def sb(self, es, name, shape, dt):
        self.uid += 1
        return es.enter_context(self.nc.sbuf_tensor(f"sb{self.uid}_{name}", list(shape), dt))

    def ps(self, es, name, shape, dt=F32):
        self.uid += 1
        return es.enter_context(self.nc.psum_tensor(f"ps{self.uid}_{name}", list(shape), dt))

    def name(self, p):
        self.uid += 1
        return f"{p}{self.uid}"


def chunked(ap):
    return ap.rearrange("(k p) n -> p k n", p=128)


class Common:
    def __init__(self, kb, layer_vec, ada_w, consts, es=None):
        self.kb = kb
        nc, S = kb.nc, kb.S
        es = es if es is not None else kb.es
        self.vec = kb.sb(es, "vec", [128, NVEC], F32)
        self.b_vec = S.buf("vec")
        S.op(DMAQ, lambda e: e.dma_start(out=self.vec[:], in_=layer_vec), writes=[self.b_vec], dma_home=self.b_vec)
        self.cst_bf = kb.sb(es, "cst_bf", [128, 3, 128], BF16)
        self.b_cst = S.buf("cst")
        S.op("pool", lambda e: e.dma_start(out=self.cst_bf[:], in_=consts["cbf"]), writes=[self.b_cst], dma_home=self.b_cst)
        self.cst_f = kb.sb(es, "cst_f", [128, 2, 128], F32)
        self.b_cstf = S.buf("cstf")
        S.op(DMAQ, lambda e: e.dma_start(out=self.cst_f[:], in_=consts["cf"]), writes=[self.b_cstf], dma_home=self.b_cstf)
        self.onesm = self.cst_bf[:, 0, :]
        self.bones = self.cst_bf[:, 1, :]
        self.rotm = self.cst_bf[:, 2, :]
        self.ident = self.cst_f[:, 0, :]
        self.ones_f = self.cst_f[:, 1, :]
        self.eps_t = kb.sb(es, "eps_t", [128, 1], F32)
        self.b_eps = S.buf("eps")
        S.op("dve", lambda e: e.memset(self.eps_t[:], EPS), writes=[self.b_eps])
        self.eps_col = self.eps_t[:, 0:1]
        self.mod = kb.sb(es, "mod", [128, 2, 48], F32)
        self.geff = kb.sb(es, "geff", [128, 2, 2, 8], F32)
        self.b_mod = S.buf("mod")
        self._build_mod(ada_w)

    def _build_mod(self, ada_w):
        kb = self.kb
        nc, S = kb.nc, kb.S
        with ExitStack() as es:
            sc = kb.sb(es, "silu_c", [128, 8, 2], BF16)
            sc32 = kb.sb(es, "silu_c32", [128, 2, 8], F32)
            b_sc = S.buf("silu_c")
            wsl = [kb.sb(es, f"adaw{i}", [128, 8, 1024], BF16) for i in range(2)]
            b_w = S.bufs("adaw", 2)
            mps = kb.ps(es, "mod_ps", [128, 48, 2], F32)
            b_mps = S.buf("mod_ps")
            vec = self.vec
            S.op("act", lambda e: e.activation(out=sc32[:, 0, :], in_=vec[:, VEC["c"]:VEC["c"] + 8], func=ACT.Silu),
                 reads=[self.b_vec], writes=[b_sc])
            S.op("act", lambda e: e.activation(out=sc32[:, 1, :], in_=vec[:, VEC["cc"]:VEC["cc"] + 8], func=ACT.Silu),
                 reads=[self.b_vec], writes=[b_sc])
            for ci in range(2):
                S.op("dve", lambda e, ci=ci: e.tensor_copy(out=sc[:, :, ci], in_=sc32[:, ci, :]), reads=[b_sc], writes=[b_sc])
            for j in range(6):
                sl = j % 2
                S.op("pool", lambda e, j=j, sl=sl: e.dma_start(out=wsl[sl][:], in_=chunked(ada_w[:, j * 1024:(j + 1) * 1024])),
                     writes=[b_w[sl]], dma_home=b_w[sl])
                for oc in range(8):
                    for kc in range(8):
                        S.op("pe", lambda e, j=j, sl=sl, oc=oc, kc=kc: e.matmul(
                            mps[:, j * 8 + oc, :], lhsT=wsl[sl][:, kc, oc * 128:(oc + 1) * 128], rhs=sc[:, kc, :],
                            start=(kc == 0), stop=(kc == 7)), reads=[b_w[sl], b_sc], writes=[b_mps])
            ab = VEC["adab"]
            for ci in range(2):
                S.op("dve", lambda e, ci=ci: e.tensor_tensor(out=self.mod[:, ci, :], in0=mps[:, :, ci], in1=vec[:, ab:ab + 48], op=ALU.add),
                     reads=[b_mps, self.b_vec], writes=[self.b_mod])
            for ci in range(2):
                for ni in range(2):
                    gcol = VEC["n1"] if ni == 0 else VEC["n2"]
                    scj = 1 if ni == 0 else 4
                    S.op("dve", lambda e, ci=ci, ni=ni, gcol=gcol, scj=scj: e.scalar_tensor_tensor(
                        out=self.geff[:, ci, ni, :], in0=self.mod[:, ci, scj * 8:scj * 8 + 8], scalar=1.0,
                        in1=vec[:, gcol:gcol + 8], op0=ALU.add, op1=ALU.mult),
                        reads=[self.b_mod, self.b_vec], writes=[self.b_mod])
        S.barrier()

    def sh(self, ci, ni, kc):
        j = 0 if ni == 0 else 3
        return self.mod[:, ci, j * 8 + kc:j * 8 + kc + 1]

    def gate(self, ci, ni, kc):
        j = 2 if ni == 0 else 5
        return self.mod[:, ci, j * 8 + kc:j * 8 + kc + 1]

    def ge(self, ci, ni, kc):
        return self.geff[:, ci, ni, kc:kc + 1]


class NormWork:
    def __init__(self, kb, es, tag):
        S = kb.S
        self.sq = [kb.sb(es, f"{tag}_sq{i}", [128, NT], BF16) for i in range(2)]
        self.b_sq = S.bufs(f"{tag}_sq", 2)
        self.sd = kb.sb(es, f"{tag}_sd", [128, NT], F32)
        self.rstd = kb.sb(es, f"{tag}_rstd", [128, NT], F32)
        self.b_sd = S.buf(f"{tag}_sd")
        self.b_rstd = S.buf(f"{tag}_rstd")
        self.tmp = [kb.sb(es, f"{tag}_tmp{i}", [128, NT], F32) for i in range(2)]
        self.b_tmp = S.bufs(f"{tag}_tmp", 2)


def norm_mod(kb, cm, nw, xt, b_xt, N, ci, ni, ms_ps, b_ms, h_out, b_h, inplace=False):
    S = kb.S
    for kc in range(KC):
        sl = kc % 2
        S.op("act", lambda e, kc=kc, sl=sl: e.activation(out=nw.sq[sl][:, :N], in_=xt[:, kc, :N], func=ACT.Square),
             reads=[b_xt], writes=[nw.b_sq[sl]])
        S.op("pe", lambda e, kc=kc, sl=sl: e.matmul(ms_ps[:, :N], lhsT=cm.onesm, rhs=nw.sq[sl][:, :N], start=(kc == 0), stop=(kc == KC - 1)),
             reads=[nw.b_sq[sl], cm.b_cst], writes=[b_ms])
    S.op("act", lambda e: e.activation(out=nw.sd[:, :N], in_=ms_ps[:, :N], func=ACT.Sqrt, bias=cm.eps_col, scale=1.0),
         reads=[b_ms, cm.b_eps], writes=[nw.b_sd])
    S.op("dve", lambda e: e.reciprocal(out=nw.rstd[:, :N], in_=nw.sd[:, :N]), reads=[nw.b_sd], writes=[nw.b_rstd])
    for kc in range(KC):
        sl = kc % 2
        if inplace:
            S.op("dve", lambda e, kc=kc: e.scalar_tensor_tensor(out=xt[:, kc, :N], in0=xt[:, kc, :N], scalar=cm.ge(ci, ni, kc),
                                                                 in1=nw.rstd[:, :N], op0=ALU.mult, op1=ALU.mult),
                 reads=[nw.b_rstd, cm.b_mod], writes=[b_xt])
            S.op("act", lambda e, kc=kc: e.activation(out=xt[:, kc, :N], in_=xt[:, kc, :N], func=ACT.Identity, bias=cm.sh(ci, ni, kc), scale=1.0),
                 reads=[cm.b_mod], writes=[b_xt])
            S.op("pool", lambda e, kc=kc: e.tensor_copy(out=h_out(kc), in_=xt[:, kc, :N]), reads=[b_xt], writes=[b_h])
        else:
            S.op("dve", lambda e, kc=kc, sl=sl: e.scalar_tensor_tensor(out=nw.tmp[sl][:, :N], in0=xt[:, kc, :N], scalar=cm.ge(ci, ni, kc),
                                                                        in1=nw.rstd[:, :N], op0=ALU.mult, op1=ALU.mult),
                 reads=[b_xt, nw.b_rstd, cm.b_mod], writes=[nw.b_tmp[sl]])
            S.op("act", lambda e, kc=kc, sl=sl: e.activation(out=h_out(kc), in_=nw.tmp[sl][:, :N], func=ACT.Identity, bias=cm.sh(ci, ni, kc), scale=1.0),
                 reads=[nw.b_tmp[sl], cm.b_mod], writes=[b_h])


def ffn_phase(kb, cm, tiles, experts, dff, router_w=None):
    nc, S = kb.nc, kb.S
    moe = router_w is not None
    FS = 256
    nsl = dff // FS
    groups = []
    cur, tot = [], 0
    for t in tiles:
        if tot + t["N"] > 2304:
            groups.append(cur)
            cur, tot = [], 0
        cur.append(t)
        tot += t["N"]
    if cur:
        groups.append(cur)
    TMAX = max(sum(t["N"] for t in g) for g in groups)
    out_ops = []
    with ExitStack() as es:
        h2 = kb.sb(es, "f_h2", [128, KC, TMAX], BF16)
        yacc = kb.sb(es, "f_yacc", [128, KC, TMAX], F32)
        wgu = [kb.sb(es, f"f_wgu{i}", [128, 2, KC, FS], BF16) for i in range(2)]
        wdn = [kb.sb(es, f"f_wdn{i}", [128, FS // 128, D], BF16) for i in range(2)]
        b_wg = S.bufs("f_wg", 2)
        b_wu = S.bufs("f_wu", 2)
        b_wd = S.bufs("f_wd", 2)
        xt = kb.sb(es, "f_xt", [128, KC, NT], F32)
        b_xt = S.buf("f_xt")
        nw = NormWork(kb, es, "f_nw")
        sg = [kb.sb(es, f"f_sg{i}", [128, NT], F32) for i in range(2)]
        b_sg = S.bufs("f_sg", 2)
        tt = [kb.sb(es, f"f_tt{i}", [128, NT], F32) for i in range(2)]
        b_tt = S.bufs("f_tt", 2)
        abuf = [kb.sb(es, f"f_a{i}", [128, 2, NT], BF16) for i in range(2)]
        b_a = [S.bufs(f"f_a{i}_", 2) for i in range(2)]
        banks = [kb.ps(es, f"f_ps{i}", [128, NT], F32) for i in range(8)]
        b_bank = S.bufs("f_bank", 8)
        if moe:
            rw = kb.sb(es, "f_rw", [128, KC, NE], F32)
            b_rw = S.buf("f_rw")
            S.op(DMAQ, lambda e: e.dma_start(out=rw[:], in_=chunked(router_w)), writes=[b_rw], dma_home=b_rw)
            gT = kb.sb(es, "f_gT", [NE, TMAX], F32)
            Gb = kb.sb(es, "f_Gb", [128, TMAX], F32)
            sel = kb.sb(es, "f_sel", [NE, NE, 128], F32)
            b_sel = S.buf("f_sel")
            S.op(DMAQ, lambda e: e.dma_start(out=sel[:], in_=kb.dram_in["selm"]), writes=[b_sel], dma_home=b_sel)
            rt = {n: kb.sb(es, f"f_rt_{n}", [128, w], F32) for n, w in
                  [("lg", 8), ("m1", 1), ("eq", 8), ("lg2", 8), ("m2", 1), ("sel", 8), ("nm1", 1), ("ex", 8), ("w", 8), ("ss", 1), ("rs", 1), ("g", 8)]}
            b_rt = S.buf("f_rt")

        for g in groups:
            offs = []
            o = 0
            for t in g:
                offs.append(o)
                o += t["N"]
            b_h2 = S.bufs("f_h2_", len(g))
            b_y = S.bufs("f_y_", len(g))
            b_gT = S.bufs("f_gT_", len(g))
            b_Gb = S.bufs("f_Gb_", len(g))
            for ti, t in enumerate(g):
                N, off, ci = t["N"], offs[ti], t["ci"]
                S.op(DMAQ, lambda e, t=t, N=N: e.dma_start(out=xt[:, :, :N], in_=chunked(t["src"])), writes=[b_xt], dma_home=b_xt)
                norm_mod(kb, cm, nw, xt, b_xt, N, ci, 1, banks[0], b_bank[0],
                         lambda kc, off=off, N=N: h2[:, kc, off:off + N], b_h2[ti], inplace=moe)
                if moe:
                    for tb in range(N // 128):
                        lgp = banks[1]
                        for kc in range(KC):
                            S.op("pe", lambda e, kc=kc, tb=tb: e.matmul(lgp[:, 0:NE], lhsT=xt[:, kc, tb * 128:(tb + 1) * 128], rhs=rw[:, kc, :],
                                                                       start=(kc == 0), stop=(kc == KC - 1)),
                                 reads=[b_xt, b_rw], writes=[b_bank[1]])
                        R = rt
                        S.op("dve", lambda e: e.tensor_copy(out=R["lg"][:], in_=lgp[:, 0:NE]), reads=[b_bank[1]], writes=[b_rt])
                        S.op("dve", lambda e: e.tensor_reduce(out=R["m1"][:], in_=R["lg"][:], axis=AX.X, op=ALU.max), reads=[b_rt], writes=[b_rt])
                        S.op("dve", lambda e: e.tensor_scalar(out=R["eq"][:], in0=R["lg"][:], scalar1=R["m1"][:], scalar2=None, op0=ALU.is_ge), reads=[b_rt], writes=[b_rt])
                        S.op("dve", lambda e: e.scalar_tensor_tensor(out=R["lg2"][:], in0=R["eq"][:], scalar=-1e30, in1=R["lg"][:], op0=ALU.mult, op1=ALU.add), reads=[b_rt], writes=[b_rt])
                        S.op("dve", lambda e: e.tensor_reduce(out=R["m2"][:], in_=R["lg2"][:], axis=AX.X, op=ALU.max), reads=[b_rt], writes=[b_rt])
                        S.op("dve", lambda e: e.tensor_scalar(out=R["sel"][:], in0=R["lg"][:], scalar1=R["m2"][:], scalar2=None, op0=ALU.is_ge), reads=[b_rt], writes=[b_rt])
                        S.op("dve", lambda e: e.tensor_scalar(out=R["nm1"][:], in0=R["m1"][:], scalar1=-1.0, scalar2=None, op0=ALU.mult), reads=[b_rt], writes=[b_rt])
                        S.op("act", lambda e: e.activation(out=R["ex"][:], in_=R["lg"][:], func=ACT.Exp, bias=R["nm1"][:], scale=1.0), reads=[b_rt], writes=[b_rt])
                        S.op("dve", lambda e: e.tensor_tensor(out=R["w"][:], in0=R["ex"][:], in1=R["sel"][:], op=ALU.mult), reads=[b_rt], writes=[b_rt])
                        S.op("dve", lambda e: e.tensor_reduce(out=R["ss"][:], in_=R["w"][:], axis=AX.X, op=ALU.add), reads=[b_rt], writes=[b_rt])
                        S.op("dve", lambda e: e.reciprocal(out=R["rs"][:], in_=R["ss"][:]), reads=[b_rt], writes=[b_rt])
                        S.op("dve", lambda e: e.tensor_scalar(out=R["g"][:], in0=R["w"][:], scalar1=R["rs"][:], scalar2=None, op0=ALU.mult), reads=[b_rt], writes=[b_rt])
                        S.op("pe", lambda e: e.transpose(out=banks[2][0:NE, 0:128], in_=R["g"][:], identity=cm.ident), reads=[b_rt, cm.b_cstf], writes=[b_bank[2]])
                        S.op("dve", lambda e, off=off, tb=tb: e.tensor_copy(out=gT[:, off + tb * 128:off + (tb + 1) * 128], in_=banks[2][0:NE, 0:128]),
                             reads=[b_bank[2]], writes=[b_gT[ti]])
            first = True
            step = 0
            pend = None
            acount = 0
            for ei, (wg_d, wu_d, wd_d) in enumerate(experts):
                if moe:
                    for ti, t in enumerate(g):
                        N, off = t["N"], offs[ti]
                        S.op("pe", lambda e, ei=ei, off=off, N=N: e.matmul(banks[7][:, :N], lhsT=sel[:, ei, :], rhs=gT[:, off:off + N], start=True, stop=True),
                             reads=[b_sel, b_gT[ti]], writes=[b_bank[7]])
                        S.op("act", lambda e, off=off, N=N: e.activation(out=Gb[:, off:off + N], in_=banks[7][:, :N], func=ACT.Copy),
                             reads=[b_bank[7]], writes=[b_Gb[ti]])
                for s in range(nsl):
                    sl = step % 2
                    step += 1
                    f0 = s * FS
                    S.op("pool", lambda e, sl=sl, f0=f0, wg_d=wg_d: e.dma_start(out=wgu[sl][:, 0, :, :], in_=chunked(wg_d[:, f0:f0 + FS])),
                         writes=[b_wg[sl]], dma_home=b_wg[sl])
                    S.op("pool", lambda e, sl=sl, f0=f0, wu_d=wu_d: e.dma_start(out=wgu[sl][:, 1, :, :], in_=chunked(wu_d[:, f0:f0 + FS])),
                         writes=[b_wu[sl]], dma_home=b_wu[sl])
                    S.op("pool", lambda e, sl=sl, f0=f0, wd_d=wd_d: e.dma_start(out=wdn[sl][:], in_=chunked(wd_d[f0:f0 + FS, :])),
                         writes=[b_wd[sl]], dma_home=b_wd[sl])
                    for ti, t in enumerate(g):
                        N, off = t["N"], offs[ti]
                        asl = acount % 2
                        acount += 1

                        def down(half, sl=sl, asl=asl, N=N, off=off, ti=ti, first=first):
                            for oc in range(half * 4, half * 4 + 4):
                                bk = 4 + oc % 4
                                for fc in range(2):
                                    S.op("pe", lambda e, oc=oc, fc=fc, bk=bk: e.matmul(banks[bk][:, :N], lhsT=wdn[sl][:, fc, oc * 128:(oc + 1) * 128],
                                                                                    rhs=abuf[asl][:, fc, :N], start=(fc == 0), stop=(fc == 1)),
                                         reads=[b_wd[sl], b_a[asl][fc]], writes=[b_bank[bk]])
                                if first:
                                    S.op("dve", lambda e, oc=oc, bk=bk: e.tensor_copy(out=yacc[:, oc, off:off + N], in_=banks[bk][:, :N]),
                                         reads=[b_bank[bk]], writes=[b_y[ti]])
                                else:
                                    S.op("dve", lambda e, oc=oc, bk=bk: e.tensor_tensor(out=yacc[:, oc, off:off + N], in0=yacc[:, oc, off:off + N],
                                                                                     in1=banks[bk][:, :N], op=ALU.add),
                                         reads=[b_bank[bk]], writes=[b_y[ti]])

                        for fc in range(2):
                            gb, ub = fc, 2 + fc
                            for kc in range(KC):
                                S.op("pe", lambda e, kc=kc, fc=fc, gb=gb: e.matmul(banks[gb][:, :N], lhsT=wgu[sl][:, 0, kc, fc * 128:(fc + 1) * 128],
                                                                                rhs=h2[:, kc, off:off + N], start=(kc == 0), stop=(kc == KC - 1)),
                                     reads=[b_wg[sl], b_h2[ti]], writes=[b_bank[gb]])
                            for kc in range(KC):
                                S.op("pe", lambda e, kc=kc, fc=fc, ub=ub: e.matmul(banks[ub][:, :N], lhsT=wgu[sl][:, 1, kc, fc * 128:(fc + 1) * 128],
                                                                                rhs=h2[:, kc, off:off + N], start=(kc == 0), stop=(kc == KC - 1)),
                                     reads=[b_wu[sl], b_h2[ti]], writes=[b_bank[ub]])
                            if pend is not None:
                                pend(fc)
                            S.op("act", lambda e, fc=fc, gb=gb: e.activation(out=sg[fc][:, :N], in_=banks[gb][:, :N], func=ACT.Silu),
                                 reads=[b_bank[gb]], writes=[b_sg[fc]])
                            if moe:
                                S.op("dve", lambda e, fc=fc, ub=ub: e.tensor_tensor(out=tt[fc][:, :N], in0=sg[fc][:, :N], in1=banks[ub][:, :N], op=ALU.mult),
                                     reads=[b_sg[fc], b_bank[ub]], writes=[b_tt[fc]])
                                S.op("dve", lambda e, fc=fc, asl=asl: e.tensor_tensor(out=abuf[asl][:, fc, :N], in0=tt[fc][:, :N], in1=Gb[:, off:off + N], op=ALU.mult),
                                     reads=[b_tt[fc], b_Gb[ti]], writes=[b_a[asl][fc]])
                            else:
                                S.op("dve", lambda e, fc=fc, ub=ub, asl=asl: e.tensor_tensor(out=abuf[asl][:, fc, :N], in0=sg[fc][:, :N], in1=banks[ub][:, :N], op=ALU.mult),
                                     reads=[b_sg[fc], b_bank[ub]], writes=[b_a[asl][fc]])
                        pend = down
                    first = False
            if pend is not None:
                pend(0)
                pend(1)
                pend = None
            for ti, t in enumerate(g):
                N, off, ci = t["N"], offs[ti], t["ci"]
                S.op(DMAQ, lambda e, t=t, N=N: e.dma_start(out=xt[:, :, :N], in_=chunked(t["src"])), writes=[b_xt], dma_home=b_xt)
                for kc in range(KC):
                    S.op("dve", lambda e, kc=kc, N=N, off=off, ci=ci: e.scalar_tensor_tensor(
                        out=xt[:, kc, :N], in0=yacc[:, kc, off:off + N], scalar=cm.gate(ci, 1, kc), in1=xt[:, kc, :N], op0=ALU.mult, op1=ALU.add),
                        reads=[b_y[ti], cm.b_mod], writes=[b_xt])
                oo = S.op(DMAQ, lambda e, t=t, N=N: e.dma_start(out=chunked(t["dst"]), in_=xt[:, :, :N]), reads=[b_xt], dma_home=b_xt)
                out_ops.append(oo)
    S.barrier()
    return out_ops


def conv_phase(kb, cm, tiles, cw_in, cw_out):
    nc, S = kb.nc, kb.S
    with ExitStack() as es:
        win = kb.sb(es, "c_win", [128, KC, 3 * D], BF16)
        wout = kb.sb(es, "c_wout", [128, KC, D], BF16)
        b_win = S.bufs("c_win", 3)
        b_wout = S.buf("c_wout")
        for j in range(3):
            for hh in range(2):
                c0 = j * D + hh * 512
                S.op("pool", lambda e, c0=c0: e.dma_start(out=win[:, :, c0:c0 + 512], in_=chunked(cw_in[:, c0:c0 + 512])),
                     writes=[b_win[j]], dma_home=S.buf("c_win_d"))
        S.op("pool", lambda e: e.dma_start(out=wout[:], in_=chunked(cw_out)), writes=[b_wout], dma_home=b_wout)
        xt = kb.sb(es, "c_xt", [128, KC, NT], F32)
        b_xt = S.buf("c_xt")
        h = kb.sb(es, "c_h", [128, KC, NT], BF16)
        b_h = S.buf("c_h")
        mb = kb.sb(es, "c_m", [128, KC, NT], BF16)
        b_m = S.bufs("c_m", KC)
        nw = NormWork(kb, es, "c_nw")
        bsb = [kb.sb(es, f"c_b{i}", [128, NT], BF16) for i in range(2)]
        csb = [kb.sb(es, f"c_c{i}", [128, NT], F32) for i in range(2)]
        zsb = [kb.sb(es, f"c_z{i}", [128, NT], F32) for i in range(2)]
        acc = [kb.sb(es, f"c_acc{i}", [128, NT], F32) for i in range(2)]
        b_bsb, b_csb, b_zsb, b_acc = S.bufs("c_b", 2), S.bufs("c_c", 2), S.bufs("c_z", 2), S.bufs("c_acc", 2)
        banks = [kb.ps(es, f"c_ps{i}", [128, NT], F32) for i in range(8)]
        b_bank = S.bufs("c_bank", 8)
        vec = cm.vec
        cw = VEC["cw"]
        for t in tiles:
            N, ci = t["N"], t["ci"]
            M = N + 2
            S.op(DMAQ, lambda e, t=t, M=M: e.dma_start(out=xt[:, :, :M], in_=chunked(t["src"])), writes=[b_xt], dma_home=b_xt)
            norm_mod(kb, cm, nw, xt, b_xt, M, ci, 0, banks[6], b_bank[6], lambda kc, M=M: h[:, kc, :M], b_h)
            for kc in range(KC):
                sl = kc % 2
                pb, pc, pu = banks[sl * 3], banks[sl * 3 + 1], banks[sl * 3 + 2]
                bb, bc, bu = b_bank[sl * 3], b_bank[sl * 3 + 1], b_bank[sl * 3 + 2]
                for j, (pp, bpp) in enumerate([(pb, bb), (pc, bc), (pu, bu)]):
                    col = j * D + kc * 128
                    for k2 in range(KC):
                        S.op("pe", lambda e, pp=pp, col=col, k2=k2, M=M: e.matmul(pp[:, :M], lhsT=win[:, k2, col:col + 128], rhs=h[:, k2, :M],
                                                                              start=(k2 == 0), stop=(k2 == KC - 1)),
                             reads=[b_win[j], b_h], writes=[bpp])
                S.op("act", lambda e, sl=sl, pb=pb, M=M: e.activation(out=bsb[sl][:, :M], in_=pb[:, :M], func=ACT.Copy), reads=[bb], writes=[b_bsb[sl]])
                S.op("act", lambda e, sl=sl, pc=pc, M=M: e.activation(out=csb[sl][:, :M], in_=pc[:, :M], func=ACT.Copy), reads=[bc], writes=[b_csb[sl]])
                S.op("dve", lambda e, sl=sl, pu=pu, M=M: e.tensor_tensor(out=zsb[sl][:, :M], in0=csb[sl][:, :M], in1=pu[:, :M], op=ALU.mult),
                     reads=[b_csb[sl], bu], writes=[b_zsb[sl]])
                for side, col in (("lo", 0), ("hi", M - 1)):
                    mode = t[side]
                    if mode == "flag":
                        fcol = VEC["vlo"] if side == "lo" else VEC["vhi"]
                        S.op("dve", lambda e, sl=sl, col=col, fcol=fcol: e.tensor_scalar(out=zsb[sl][:, col:col + 1], in0=zsb[sl][:, col:col + 1],
                                                                                      scalar1=vec[:, fcol:fcol + 1], scalar2=None, op0=ALU.mult),
                             reads=[cm.b_vec], writes=[b_zsb[sl]])
                    elif mode == "zero":
                        S.op("dve", lambda e, sl=sl, col=col: e.memset(zsb[sl][:, col:col + 1], 0.0), writes=[b_zsb[sl]])
                w0 = vec[:, cw + kc:cw + kc + 1]
                w1 = vec[:, cw + 8 + kc:cw + 8 + kc + 1]
                w2 = vec[:, cw + 16 + kc:cw + 16 + kc + 1]
                S.op("dve", lambda e, sl=sl, w0=w0, N=N: e.tensor_scalar(out=acc[sl][:, :N], in0=zsb[sl][:, 0:N], scalar1=w0, scalar2=None, op0=ALU.mult),
                     reads=[b_zsb[sl], cm.b_vec], writes=[b_acc[sl]])
                S.op("dve", lambda e, sl=sl, w1=w1, N=N: e.scalar_tensor_tensor(out=acc[sl][:, :N], in0=zsb[sl][:, 1:N + 1], scalar=w1, in1=acc[sl][:, :N],
                                                                            op0=ALU.mult, op1=ALU.add),
                     reads=[b_zsb[sl]], writes=[b_acc[sl]])
                S.op("dve", lambda e, sl=sl, w2=w2, N=N: e.scalar_tensor_tensor(out=acc[sl][:, :N], in0=zsb[sl][:, 2:N + 2], scalar=w2, in1=acc[sl][:, :N],
                                                                            op0=ALU.mult, op1=ALU.add),
                     reads=[b_zsb[sl]], writes=[b_acc[sl]])
                S.op("pool", lambda e, sl=sl, kc=kc, N=N: e.tensor_tensor(out=mb[:, kc, :N], in0=acc[sl][:, :N], in1=bsb[sl][:, 1:N + 1], op=ALU.mult),
                     reads=[b_acc[sl], b_bsb[sl]], writes=[b_m[kc]])
            for oc in range(KC):
                pk = 6 + oc % 2
                for k2 in range(KC):
                    S.op("pe", lambda e, pk=pk, oc=oc, k2=k2, N=N: e.matmul(banks[pk][:, :N], lhsT=wout[:, k2, oc * 128:(oc + 1) * 128], rhs=mb[:, k2, :N],
                                                                        start=(k2 == 0), stop=(k2 == KC - 1)),
                         reads=[b_wout, b_m[k2]], writes=[b_bank[pk]])
                S.op("dve", lambda e, pk=pk, oc=oc, N=N, ci=ci: e.scalar_tensor_tensor(out=xt[:, oc, 1:N + 1], in0=banks[pk][:, :N], scalar=cm.gate(ci, 0, oc),
                                                                                   in1=xt[:, oc, 1:N + 1], op0=ALU.mult, op1=ALU.add),
                     reads=[b_bank[pk], cm.b_mod], writes=[b_xt])
            S.op(DMAQ, lambda e, t=t, N=N: e.dma_start(out=chunked(t["dst"]), in_=xt[:, :, 1:N + 1]), reads=[b_xt], dma_home=b_xt)
    S.barrier()


def conv_tiles(xpad, xm, ci, ntok, lo, hi):
    tiles = []
    t0 = 0
    while t0 < ntok:
        N = min(510, ntok - t0)
        tiles.append(dict(src=xpad[:, t0:t0 + N + 2], dst=xm[:, t0:t0 + N], N=N, ci=ci,
                          lo=(lo if t0 == 0 else None), hi=(hi if t0 + N == ntok else None)))
        t0 += N
    return tiles


def ffn_tiles(src, dst, ci, ntok):
    return [dict(src=src[:, t0:min(t0 + NT, ntok)], dst=dst[:, t0:min(t0 + NT, ntok)], N=min(NT, ntok - t0), ci=ci) for t0 in range(0, ntok, NT)]


def build_layer_B(with_ctx, n_exp=NE):
    global DMAQ
    DMAQ = "sp"
    kb = KB()
    xpad = kb.din("xpad", [D, HALF + 2])
    vecd = kb.din("vec", [128, NVEC])
    ada_w = kb.din("ada_w", [D, 6 * D])
    consts = dict(cbf=kb.din("cbf", [128, 3, 128]), cf=kb.din("cf", [128, 2, 128]))
    cw_in = kb.din("cw_in", [D, 3 * D])
    cw_out = kb.din("cw_out", [D, D])
    router_w = kb.din("router_w", [D, NE])
    kb.din("selm", [NE, NE, 128])
    mwg = kb.din("mwg", [n_exp, D, DFFE])
    mwu = kb.din("mwu", [n_exp, D, DFFE])
    mwd = kb.din("mwd", [n_exp, DFFE, D])
    yo = kb.dout("yo", [D, HALF])
    xm = kb.dscratch("xm", [D, HALF])
    if with_ctx:
        xcpad = kb.din("xcpad", [D, CTX + 2])
        yc = kb.dout("yc", [D, CTX])
        xcm = kb.dscratch("xcm", [D, CTX])
    cm = Common(kb, vecd, ada_w, consts)
    ct = conv_tiles(xpad, xm, 0, HALF, "flag", "flag")
    ft = ffn_tiles(xm, yo, 0, HALF)
    if with_ctx:
        ct += conv_tiles(xcpad, xcm, 1, CTX, "zero", "zero")
        ft = ft[:4] + ffn_tiles(xcm, yc, 1, CTX) + ft[4:]
    conv_phase(kb, cm, ct, cw_in, cw_out)
    experts = [(mwg[e], mwu[e], mwd[e]) for e in range(n_exp)]
    outs = ffn_phase(kb, cm, ft, experts, DFFE, router_w=router_w)
    stats = kb.S.emit(kb.nc, final_wait_ops=outs)
    kb.es.close()
    return kb.nc, stats


def colpack(v):
    return np.ascontiguousarray(v.reshape(-1, 128).T)


def make_consts():
    cbf = np.zeros((128, 3, 128), np.float32)
    cbf[:, 0, :] = 1.0 / D
    for hh in range(2):
        cbf[hh * 64:(hh + 1) * 64, 1, hh * 64:(hh + 1) * 64] = 1.0 / 64
    for i in range(64):
        cbf[2 * i + 1, 2, 2 * i] = -1.0
        cbf[2 * i, 2, 2 * i + 1] = 1.0
    cf = np.zeros((128, 2, 128), np.float32)
    cf[:, 0, :] = np.eye(128, dtype=np.float32)
    cf[:, 1, :] = 1.0
    selm = np.zeros((NE, NE, 128), np.float32)
    for e in range(NE):
        selm[e, e, :] = 1.0
    return cbf, cf, selm


def make_vec(inp, layer, b, half):
    i = layer // 2
    v = np.zeros((128, NVEC), np.float32)
    v[:, VEC["c"]:VEC["c"] + 8] = colpack(inp["c"][b])
    v[:, VEC["cc"]:VEC["cc"] + 8] = colpack(inp["c_ctx"])
    v[:, VEC["n1"]:VEC["n1"] + 8] = colpack(inp["norm1_g"][layer])
    v[:, VEC["n2"]:VEC["n2"] + 8] = colpack(inp["norm2_g"][layer])
    v[:, VEC["adab"]:VEC["adab"] + 48] = colpack(inp["ada_b"][layer])
    if layer % 2 == 0:
        for n, k in (("qna", "qnorm_a"), ("kna", "knorm_a"), ("qnb", "qnorm_b"), ("knb", "knorm_b")):
            v[:, VEC[n]] = np.tile(inp[k][i], 2)
        v[:, VEC["sink"]:VEC["sink"] + 8] = inp["sink_b"][i][None, :]
    else:
        for j in range(3):
            v[:, VEC["cw"] + 8 * j:VEC["cw"] + 8 * j + 8] = colpack(inp["conv_w"][i][j])
    v[:, VEC["vlo"]] = float(half)
    v[:, VEC["vhi"]] = float(1 - half)
    return v


W_QA, W_QB, W_KA, W_KB, W_V = 0, 512, 1024, 1152, 1280
NWIN = 1536
NTL = SEQ // NT
NB = 2 + SEQ // 128


class QKWork:
    def __init__(self, kb, es):
        S = kb.S
        self.sq = kb.sb(es, "qk_sq", [128, NT], BF16)
        self.sd = kb.sb(es, "qk_sd", [128, NT], F32)
        self.r = kb.sb(es, "qk_r", [128, NT], F32)
        self.qn = kb.sb(es, "qk_qn", [128, NT], BF16)
        self.t1 = kb.sb(es, "qk_t1", [128, NT], F32)
        self.t2 = kb.sb(es, "qk_t2", [128, NT], F32)
        self.b = {n: S.buf("qk_" + n) for n in ("sq", "sd", "r", "qn", "t1", "t2")}


def qk_norm_rope(kb, cm, qw, ps, b_ps, N, gcol, cs, b_cs, ms_ps, b_ms, rot_ps, b_rot, out_ap, b_out):
    S = kb.S
    vec = cm.vec
    S.op("act", lambda e: e.activation(out=qw.sq[:, :N], in_=ps[:, :N], func=ACT.Square), reads=[b_ps], writes=[qw.b["sq"]])
    S.op("pe", lambda e: e.matmul(ms_ps[:, :N], lhsT=cm.bones, rhs=qw.sq[:, :N], start=True, stop=True), reads=[qw.b["sq"], cm.b_cst], writes=[b_ms])
    S.op("act", lambda e: e.activation(out=qw.sd[:, :N], in_=ms_ps[:, :N], func=ACT.Sqrt, bias=cm.eps_col, scale=1.0), reads=[b_ms, cm.b_eps], writes=[qw.b["sd"]])
    S.op("dve", lambda e: e.reciprocal(out=qw.r[:, :N], in_=qw.sd[:, :N]), reads=[qw.b["sd"]], writes=[qw.b["r"]])
    pieces = out_ap if isinstance(out_ap, list) else [(out_ap, 0, 128)]
    if cs is None:
        for (oap, p0, p1) in pieces:
            S.op("dve", lambda e: e.scalar_tensor_tensor(out=oap, in0=ps[p0:p1, :N], scalar=vec[p0:p1, gcol:gcol + 1], in1=qw.r[p0:p1, :N], op0=ALU.mult, op1=ALU.mult),
                 reads=[b_ps, qw.b["r"], cm.b_vec], writes=[b_out])
        return
    cos, sin = cs
    S.op("dve", lambda e: e.scalar_tensor_tensor(out=qw.qn[:, :N], in0=ps[:, :N], scalar=vec[:, gcol:gcol + 1], in1=qw.r[:, :N], op0=ALU.mult, op1=ALU.mult),
         reads=[b_ps, qw.b["r"], cm.b_vec], writes=[qw.b["qn"]])
    S.op("pe", lambda e: e.matmul(rot_ps[:, :N], lhsT=cm.rotm, rhs=qw.qn[:, :N], start=True, stop=True), reads=[qw.b["qn"], cm.b_cst], writes=[b_rot])
    S.op("dve", lambda e: e.tensor_tensor(out=qw.t1[:, :N], in0=qw.qn[:, :N], in1=cos[:, :N], op=ALU.mult), reads=[qw.b["qn"], b_cs], writes=[qw.b["t1"]])
    S.op("dve", lambda e: e.tensor_tensor(out=qw.t2[:, :N], in0=rot_ps[:, :N], in1=sin[:, :N], op=ALU.mult), reads=[b_rot, b_cs], writes=[qw.b["t2"]])
    for (oap, p0, p1) in pieces:
        S.op("pool", lambda e: e.tensor_tensor(out=oap, in0=qw.t1[p0:p1, :N], in1=qw.t2[p0:p1, :N], op=ALU.add), reads=[qw.b["t1"], qw.b["t2"]], writes=[b_out])


def attn_phase(kb, cm, w_in_d, w_out_d, cosd, sind, bandd, xs, xc, xm, xcm, with_ctx_out):
    nc, S = kb.nc, kb.S
    vec = cm.vec
    with ExitStack() as es:
        KA = kb.sb(es, "KA", [128, NB * 128], BF16)
        KBc = kb.sb(es, "KB", [128, NB * 128], BF16)
        VA = kb.sb(es, "VA", [128, NB, 2, 65], BF16)
        VB = kb.sb(es, "VB", [128, NB, 2, 65], BF16)
        b_KA, b_VA, b_KB, b_VB = S.buf("KA"), S.buf("VA"), S.buf("KB"), S.buf("VB")
        win = kb.sb(es, "a_win", [128, KC, NWIN], BF16)
        wout = kb.sb(es, "a_wout", [128, KC, D], BF16)
        b_win, b_wout = S.buf("a_win"), S.buf("a_wout")
        for j in range(3):
            c0 = j * 512
            S.op("pool", lambda e: e.dma_start(out=win[:, :, c0:c0 + 512], in_=chunked(w_in_d[:, c0:c0 + 512])), writes=[b_win], dma_home=S.buf("a_win_d"))
        S.op("pool", lambda e: e.dma_start(out=wout[:], in_=chunked(w_out_d)), writes=[b_wout], dma_home=b_wout)
        band = kb.sb(es, "a_band", [128, 384], BF16)
        b_band = S.buf("a_band")
        S.op("pool", lambda e: e.dma_start(out=band[:], in_=bandd), writes=[b_band], dma_home=b_band)
        esink = kb.sb(es, "a_esink", [128, 8], F32)
        b_esink = S.buf("a_esink")
        S.op("act", lambda e: e.activation(out=esink[:], in_=vec[:, VEC["sink"]:VEC["sink"] + 8], func=ACT.Exp), reads=[cm.b_vec], writes=[b_esink])
        xt = kb.sb(es, "a_xt", [128, KC, NT], F32)
        b_xt = S.buf("a_xt")
        h = kb.sb(es, "a_h", [128, KC, NT], BF16)
        b_h = S.buf("a_h")
        Qlo = kb.sb(es, "a_Qlo", [128, 8, NT], BF16)
        Qhi = kb.sb(es, "a_Qhi", [128, 8, NT], BF16)
        b_Q = S.bufs("a_Q", 8)
        for _c in range(8):
            S.op("pool", lambda e: e.memset(Qlo[:, _c, :], 0.0), writes=[b_Q[_c]])
            S.op("pool", lambda e: e.memset(Qhi[:, _c, :], 0.0), writes=[b_Q[_c]])
        oT = kb.sb(es, "a_oT", [128, 8, NT], BF16)
        b_oT = S.bufs("a_oT", 16)
        nw = NormWork(kb, es, "a_nw")
        qw = QKWork(kb, es)
        cst = kb.sb(es, "a_cos", [128, NT], F32)
        snt = kb.sb(es, "a_sin", [128, NT], F32)
        b_cs = S.buf("a_cs")
        b_cos_d, b_sin_d = S.buf("a_cos_d"), S.buf("a_sin_d")
        psb = [kb.sb(es, f"a_p{i}", [128, NT], BF16) for i in range(3)]
        b_psb = S.bufs("a_p", 3)
        rec = kb.sb(es, "a_rec", [128, NT], F32)
        bcs = kb.sb(es, "a_bc", [64, NT], F32)
        b_rec, b_bcs = S.buf("a_rec"), S.buf("a_bcs")
        banks = [kb.ps(es, f"a_ps{i}", [128, NT], F32) for i in range(8)]
        b_bank = S.bufs("a_bank", 8)

        S.op("dve", lambda e: e.memset(VA[:, :, :, 64:65], 1.0), writes=[b_VA])
        S.op("dve", lambda e: e.memset(VB[:, :, :, 64:65], 1.0), writes=[b_VB])

        def load_tile(src, N, ci, pos0):
            S.op(DMAQ, lambda e: e.dma_start(out=xt[:, :, :N], in_=chunked(src)), writes=[b_xt], dma_home=b_xt)
            if pos0 is not None:
                S.op(DMAQ, lambda e: e.dma_start(out=cst[:, :N], in_=cosd[:, pos0:pos0 + N]), writes=[b_cs], dma_home=b_cos_d)
                S.op(DMAQ, lambda e: e.dma_start(out=snt[:, :N], in_=sind[:, pos0:pos0 + N]), writes=[b_cs], dma_home=b_sin_d)
            norm_mod(kb, cm, nw, xt, b_xt, N, ci, 0, banks[5], b_bank[5], lambda kc: h[:, kc, :N], b_h)

        def project(col, N):
            for kc in range(KC):
                S.op("pe", lambda e: e.matmul(banks[5][:, :N], lhsT=win[:, kc, col:col + 128], rhs=h[:, kc, :N], start=(kc == 0), stop=(kc == KC - 1)),
                     reads=[b_win, b_h], writes=[b_bank[5]])

        def kv_build(src, N, ci, pos0, blk0):
            load_tile(src, N, ci, pos0)
            cs = None if pos0 is None else (cst, snt)
            project(W_KA, N)
            qk_norm_rope(kb, cm, qw, banks[5], b_bank[5], N, VEC["kna"], cs, b_cs, banks[6], b_bank[6], banks[7], b_bank[7],
                         KA[:, blk0 * 128:blk0 * 128 + N], b_KA)
            project(W_KB, N)
            qk_norm_rope(kb, cm, qw, banks[5], b_bank[5], N, VEC["knb"], cs, b_cs, banks[6], b_bank[6], banks[7], b_bank[7],
                         KBc[:, blk0 * 128:blk0 * 128 + N], b_KB)
            for tb in range(N // 128):
                vp = banks[4]
                for kc in range(KC):
                    S.op("pe", lambda e: e.matmul(vp[:, 0:256], lhsT=h[:, kc, tb * 128:(tb + 1) * 128], rhs=win[:, kc, W_V:W_V + 256], start=(kc == 0), stop=(kc == KC - 1)),
                         reads=[b_h, b_win], writes=[b_bank[4]])
                S.op("dve", lambda e: e.tensor_copy(out=VA[:, blk0 + tb, :, 0:64], in_=vp[:, 0:128].rearrange("p (a b) -> p a b", a=2)),
                     reads=[b_bank[4]], writes=[b_VA])
                S.op("dve", lambda e: e.tensor_copy(out=VB[:, blk0 + tb, :, 0:64], in_=vp[:, 128:256].rearrange("p (a b) -> p a b", a=2)),
                     reads=[b_bank[4]], writes=[b_VB])

        kv_build(xc, CTX, 1, None, 0)
        for it in range(NTL):
            kv_build(xs[:, it * NT:(it + 1) * NT], NT, 0, it * NT, 2 + 4 * it)

        def finalize(o_ps, b_o, N, chunk, half, sink_h):
            if sink_h is not None:
                S.op("dve", lambda e: e.tensor_scalar(out=rec[64:65, :N], in0=o_ps[64:65, :N], scalar1=esink[64:65, sink_h:sink_h + 1], scalar2=None, op0=ALU.add),
                     reads=[b_o, b_esink], writes=[b_rec])
                S.op("dve", lambda e: e.reciprocal(out=rec[64:65, :N], in_=rec[64:65, :N]), reads=[b_rec], writes=[b_rec])
            else:
                S.op("dve", lambda e: e.reciprocal(out=rec[64:65, :N], in_=o_ps[64:65, :N]), reads=[b_o], writes=[b_rec])
            S.op("pe", lambda e: e.matmul(banks[7][0:64, :N], lhsT=cm.cst_f[64:65, 1, 0:64], rhs=rec[64:65, :N], start=True, stop=True),
                 reads=[b_rec, cm.b_cstf], writes=[b_bank[7]])
            S.op("dve", lambda e: e.tensor_copy(out=bcs[:, :N], in_=banks[7][0:64, :N]), reads=[b_bank[7]], writes=[b_bcs])
            p0 = half * 64
            S.op("dve", lambda e: e.tensor_tensor(out=oT[p0:p0 + 64, chunk, :N], in0=o_ps[0:64, :N], in1=bcs[:, :N], op=ALU.mult),
                 reads=[b_o, b_bcs], writes=[b_oT[2 * chunk + half]])

        scnt = [0]
        PIPE = 2

        def run_steps(steps):
            n = len(steps)
            sis = []
            pend = {}
            for i in range(n + PIPE):
                for k in [k for k, (due, _) in pend.items() if due <= i]:
                    pend.pop(k)[1]()
                if i < n:
                    st = steps[i]
                    si = scnt[0] % 3
                    scnt[0] += 1
                    sis.append(si)
                    kv, qc, blk, q0, q1, m0 = st["kv"], st["qc"], st["blk"], st["q0"], st["q1"], st["m0"]
                    Kc = st["Kc"]
                    Qs = Qlo if kv == 0 else Qhi
                    S.op("pe", lambda e: e.matmul(banks[si][:, q0:q1], lhsT=Kc[:, blk * 128:(blk + 1) * 128],
                                                  rhs=Qs[:, qc, q0:q1], start=True, stop=True),
                         reads=[st["b_K"], b_Q[qc]], writes=[b_bank[si]])
                    S.op("act", lambda e: e.activation(out=psb[si][:, q0:q1], in_=banks[si][:, q0:q1], func=ACT.Exp, scale=SCALE),
                         reads=[b_bank[si]], writes=[b_psb[si]])
                    if m0 is not None:
                        S.op("dve", lambda e: e.tensor_tensor(out=psb[si][:, q0:q1], in0=psb[si][:, q0:q1], in1=band[:, m0:m0 + (q1 - q0)], op=ALU.mult),
                             reads=[b_band], writes=[b_psb[si]])
                j = i - PIPE
                if j >= 0:
                    st = steps[j]
                    si = sis[j]
                    kv, blk, q0, q1 = st["kv"], st["blk"], st["q0"], st["q1"]
                    Vc, o_ps = st["Vc"], st["o_ps"]
                    if st["start"] and id(o_ps) in pend:
                        pend.pop(id(o_ps))[1]()
                    S.op("pe", lambda e: e.matmul(o_ps[0:65, q0:q1], lhsT=Vc[:, blk, kv, 0:65], rhs=psb[si][:, q0:q1], start=st["start"], stop=st["stop"],
                                                  skip_group_check=True),
                         reads=[st["b_V"], b_psb[si]], writes=[st["b_o"]])
                    if st["fin"] is not None:
                        pend[id(o_ps)] = (i + 6, st["fin"])
            for k in list(pend):
                pend.pop(k)[1]()

        def q_tile(src, dst, N, ci, pos0, it):
            load_tile(src, N, ci, pos0)
            cs = None if pos0 is None else (cst, snt)
            for c in range(8):
                project((W_QA if c < 4 else W_QB) + (c % 4) * 128, N)
                qk_norm_rope(kb, cm, qw, banks[5], b_bank[5], N, VEC["qna"] if c < 4 else VEC["qnb"], cs, b_cs, banks[6], b_bank[6], banks[7], b_bank[7],
                             [(Qlo[0:64, c, :N], 0, 64), (Qhi[64:128, c, :N], 64, 128)], b_Q[c])
            hcnt = 0
            steps = []
            for grp in range(2):
                for hd in range(8):
                    kv, c = hd // 4, hd % 4
                    ob = 3 + hcnt % 2
                    hcnt += 1
                    if grp == 0:
                        blocks = [(0, 0, N, None), (1, 0, N, None)] if it is None else [(b, 0, N, None) for b in range(NB)]
                        Kc, b_K, Vc, b_V, qc = KA, b_KA, VA, b_VA, c
                        fin = (lambda ob=ob, c=c, kv=kv: finalize(banks[ob], b_bank[ob], N, c, kv, None))
                    else:
                        blocks = [(0, 0, N, None), (1, 0, N, None)]
                        if it is not None:
                            for j in range(6):
                                lb = it * 4 - 1 + j
                                if lb < 0 or lb >= SEQ // 128:
                                    continue
                                q0 = max(0, 128 * (j - 2))
                                q1 = min(512, 128 * (j - 2) + 384)
                                blocks.append((2 + lb, q0, q1, q0 - 128 * (j - 2)))
                        Kc, b_K, Vc, b_V, qc = KBc, b_KB, VB, b_VB, 4 + c
                        fin = (lambda ob=ob, c=c, kv=kv, hd=hd: finalize(banks[ob], b_bank[ob], N, 4 + c, kv, hd))
                    nb = len(blocks)
                    for bi, (blk, q0, q1, m0) in enumerate(blocks):
                        steps.append(dict(Kc=Kc, b_K=b_K, Vc=Vc, b_V=b_V, kv=kv, qc=qc, blk=blk, q0=q0, q1=q1, m0=m0, o_ps=banks[ob], b_o=b_bank[ob],
                                          start=(bi == 0), stop=(bi == nb - 1), fin=(fin if bi == nb - 1 else None)))
            run_steps(steps)
            for oc in range(KC):
                pk = 5 + oc % 2
                for c in range(8):
                    S.op("pe", lambda e: e.matmul(banks[pk][:, :N], lhsT=wout[:, c, oc * 128:(oc + 1) * 128], rhs=oT[:, c, :N], start=(c == 0), stop=(c == 7)),
                         reads=[b_wout, b_oT[2 * c], b_oT[2 * c + 1]], writes=[b_bank[pk]])
                S.op("dve", lambda e: e.scalar_tensor_tensor(out=xt[:, oc, :N], in0=banks[pk][:, :N], scalar=cm.gate(ci, 0, oc), in1=xt[:, oc, :N], op0=ALU.mult, op1=ALU.add),
                     reads=[b_bank[pk], cm.b_mod], writes=[b_xt])
            S.op(DMAQ, lambda e: e.dma_start(out=chunked(dst), in_=xt[:, :, :N]), reads=[b_xt], dma_home=b_xt)

        if with_ctx_out:
            q_tile(xc, xcm, CTX, 1, None, None)
        for it in range(NTL):
            q_tile(xs[:, it * NT:(it + 1) * NT], xm[:, it * NT:(it + 1) * NT], NT, 0, it * NT, it)
    S.barrier()


def with_ctx_tiles(ft, fc):
    return ft[:4] + fc + ft[4:]


def build_fused(n_layers=4, n_exp=NE):
    global DMAQ
    DMAQ = "sp"
    kb = KB()
    x0 = kb.din("x0", [D, SEQ])
    xc0 = kb.din("xc0", [D, CTX])
    vecd = kb.din("vec", [4, 128, NVEC])
    ada_w = kb.din("ada_w", [4, D, 6 * D])
    consts = dict(cbf=kb.din("cbf", [128, 3, 128]), cf=kb.din("cf", [128, 2, 128]))
    kb.din("selm", [NE, NE, 128])
    w_in = kb.din("w_in", [2, D, NWIN])
    w_out = kb.din("w_out", [2, D, D])
    cosd = kb.din("rcos", [128, SEQ])
    sind = kb.din("rsin", [128, SEQ])
    bandd = kb.din("bandm", [128, 384])
    fwg = kb.din("fwg", [2, D, DFF])
    fwu = kb.din("fwu", [2, D, DFF])
    fwd = kb.din("fwd", [2, DFF, D])
    cw_in = kb.din("cw_in", [2, D, 3 * D])
    cw_out = kb.din("cw_out", [2, D, D])
    router_w = kb.din("router_w", [2, D, NE])
    mwg = kb.din("mwg", [2, n_exp, D, DFFE])
    mwu = kb.din("mwu", [2, n_exp, D, DFFE])
    mwd = kb.din("mwd", [2, n_exp, DFFE, D])
    yo = kb.dout("yo", [D, SEQ])
    xm = kb.dscratch("xm", [D, SEQ])
    xcm = kb.dscratch("xcm", [D, CTX])
    xp1 = kb.dscratch("xp1", [D, SEQ + 2])
    xcp1 = kb.dscratch("xcp1", [D, CTX + 2])
    x2 = kb.dscratch("x2", [D, SEQ])
    xc2 = kb.dscratch("xc2", [D, CTX])
    xp3 = kb.dscratch("xp3", [D, SEQ + 2])
    outs = []
    zt = kb.sb(kb.es, "zero_col", [128, KC, 1], F32)
    b_zt = kb.S.buf("zero_col")
    kb.S.op("dve", lambda e: e.memset(zt[:], 0.0), writes=[b_zt])
    for buf, n in ((xp1, SEQ), (xcp1, CTX), (xp3, SEQ)):
        for col in (0, n + 1):
            kb.S.op(DMAQ, lambda e: e.dma_start(out=chunked(buf[:, col:col + 1]), in_=zt[:], allow_slow_non_contiguous=True), reads=[b_zt], dma_home=kb.S.buf("zc_d"))
    for layer in range(n_layers):
        i = layer // 2
        with ExitStack() as es:
            cm = Common(kb, vecd[layer], ada_w[layer], consts, es=es)
            if layer == 0:
                attn_phase(kb, cm, w_in[0], w_out[0], cosd, sind, bandd, x0, xc0, xm, xcm, True)
                ft = with_ctx_tiles(ffn_tiles(xm, xp1[:, 1:SEQ + 1], 0, SEQ), ffn_tiles(xcm, xcp1[:, 1:CTX + 1], 1, CTX))
                outs = ffn_phase(kb, cm, ft, [(fwg[0], fwu[0], fwd[0])], DFF)
            elif layer == 1:
                ct = conv_tiles(xp1, xm, 0, SEQ, "zero", "zero") + conv_tiles(xcp1, xcm, 1, CTX, "zero", "zero")
                conv_phase(kb, cm, ct, cw_in[0], cw_out[0])
                ft = with_ctx_tiles(ffn_tiles(xm, x2, 0, SEQ), ffn_tiles(xcm, xc2, 1, CTX))
                outs = ffn_phase(kb, cm, ft, [(mwg[0, e], mwu[0, e], mwd[0, e]) for e in range(n_exp)], DFFE, router_w=router_w[0])
            elif layer == 2:
                attn_phase(kb, cm, w_in[1], w_out[1], cosd, sind, bandd, x2, xc2, xm, None, False)
                outs = ffn_phase(kb, cm, ffn_tiles(xm, xp3[:, 1:SEQ + 1], 0, SEQ), [(fwg[1], fwu[1], fwd[1])], DFF)
            else:
                conv_phase(kb, cm, conv_tiles(xp3, xm, 0, SEQ, "zero", "zero"), cw_in[1], cw_out[1])
                outs = ffn_phase(kb, cm, ffn_tiles(xm, yo, 0, SEQ), [(mwg[1, e], mwu[1, e], mwd[1, e]) for e in range(n_exp)], DFFE, router_w=router_w[1])
    if n_layers < 4:
        pass
    stats = kb.S.emit(kb.nc, final_wait_ops=outs)
    kb.es.close()
    return kb.nc, stats


def rope_tables():
    half = 32
    inv = (10000.0 ** (-np.arange(0, half, 2, dtype=np.float32) / half)).astype(np.float32)
    pos = np.arange(SEQ)
    row = (pos // 64).astype(np.float32)
    col = (pos % 64).astype(np.float32)
    ang = np.concatenate([row[:, None] * inv[None, :], col[:, None] * inv[None, :]], axis=-1)
    cos = np.cos(ang).astype(np.float32)
    sin = np.sin(ang).astype(np.float32)
    pidx = (np.arange(128) % 64) // 2
    return np.ascontiguousarray(cos[:, pidx].T), np.ascontiguousarray(sin[:, pidx].T)


def band_mask():
    kk = np.arange(128)[:, None]
    u = np.arange(384)[None, :] - 128
    return ((u >= kk - 128) & (u <= kk + 128)).astype(np.float32)


def perm_w_in(w):
    cols = []
    for base in (0, 768):
        for c in range(4):
            cols += [w[:, base + c * 64:base + (c + 1) * 64], w[:, base + (4 + c) * 64:base + (5 + c) * 64]]
    cols += [w[:, 512:640], w[:, 1280:1408], w[:, 640:768], w[:, 1408:1536]]
    return np.ascontiguousarray(np.concatenate(cols, axis=1))


def perm_w_out(w):
    rows = []
    for base in (0, 512):
        for c in range(4):
            rows += [w[base + c * 64:base + (c + 1) * 64], w[base + (4 + c) * 64:base + (5 + c) * 64]]
    return np.ascontiguousarray(np.concatenate(rows, axis=0))


_NC = {}


def make_inputs(inp, b):
    cbf, cf, selm = make_consts()
    cos, sin = rope_tables()
    return dict(
        x0=np.ascontiguousarray(inp["x"][b].T), xc0=np.ascontiguousarray(inp["ctx"][b].T),
        vec=np.stack([make_vec(inp, l, b, 0) for l in range(4)]), ada_w=inp["ada_w"], cbf=cbf, cf=cf, selm=selm,
        w_in=np.stack([perm_w_in(inp["attn_w_in"][i]) for i in range(2)]), w_out=np.stack([perm_w_out(inp["attn_w_out"][i]) for i in range(2)]),
        rcos=cos, rsin=sin, bandm=band_mask(), fwg=inp["ffn_w_gate"], fwu=inp["ffn_w_up"], fwd=inp["ffn_w_down"],
        cw_in=inp["conv_w_in"], cw_out=inp["conv_w_out"], router_w=inp["router_w"],
        mwg=inp["moe_w_gate"], mwu=inp["moe_w_up"], mwd=inp["moe_w_down"])


def kernel(**inp):
    inp = {k: np.asarray(v) for k, v in inp.items()}
    B = inp["x"].shape[0]
    if "nc" not in _NC:
        _NC["nc"] = build_fused()[0]
    per_b = [make_inputs(inp, b) for b in range(B)]
    ins = [per_b[c // 2] for c in range(8)]
    res = run_bass_kernel_spmd(_NC["nc"], ins, core_ids=list(range(8)))
    return np.stack([np.ascontiguousarray(res.results[2 * b]["yo"].T) for b in range(B)]).astype(np.float32)
```
